# Optimizing a Trainium2 kernel written in Bass

```python
import jax, jax.numpy as jnp
from jax import lax
import numpy as np

D_MODEL = 1024
BATCH = 4
SEQ = 8192
DEPTH = 2

GRID_W = 64
CTX_LEN = 256
NORM_EPS = 1e-6
F32 = jnp.float32

D_CONV = 512
D_RWKV = 512
RWKV_HEAD = 64
RWKV_HEADS = D_RWKV // RWKV_HEAD
RWKV_DECAY_RANK = 64
RWKV_ICLR_RANK = 64
RWKV_GATE_RANK = 128
RWKV_LN_EPS = 64e-5
D_NAT = 512
NAT_HEAD = 64
NAT_HEADS = D_NAT // NAT_HEAD
NAT_ROWS = 8
NAT_COLS = 16
GLA_HEADS = 4
GLA_DK = 64
GLA_DV = 128
D_GLA_K = GLA_HEADS * GLA_DK
D_GLA_V = GLA_HEADS * GLA_DV
GLA_GATE_RANK = 16
GLA_GATE_TEMP = 16.0
GLA_CHUNK = 64
ROPE_BASE = 10000.0
N_EXPERTS = 16
D_EXPERT = 1536
EC_CAPACITY_FACTOR = 2

EVEN_SPLIT = [D_CONV, D_CONV, D_CONV, D_RWKV, D_RWKV, D_RWKV, RWKV_DECAY_RANK, RWKV_ICLR_RANK, RWKV_GATE_RANK]
ODD_SPLIT = [D_NAT, D_NAT, D_NAT, D_GLA_K, D_GLA_K, D_GLA_V, D_GLA_V, GLA_GATE_RANK]
EVEN_IN = sum(EVEN_SPLIT)
ODD_IN = sum(ODD_SPLIT)

kernel_name = 'hybrid_diffusion_conv_rwkv7_natten_gla_ecmoe'


def _split(p, sizes):
    return jnp.split(p, np.cumsum(sizes)[:-1].tolist(), axis=-1)


def rmsnorm(x, g, eps=NORM_EPS):
    xf = x.astype(F32)
    y = xf * lax.rsqrt(jnp.mean(xf * xf, axis=-1, keepdims=True) + eps)
    return (y * g.astype(F32)).astype(x.dtype)


def modulate(x, g, shift, scale):
    return rmsnorm(x, g) * (1 + scale) + shift


def conv3(u, w):
    up = jnp.pad(u, ((0, 0), (1, 1), (0, 0)))
    return up[:, :-2] * w[0] + up[:, 1:-1] * w[1] + up[:, 2:] * w[2]


def rope_2d(x):
    L, d = x.shape[1], x.shape[-1]
    half, nf = d // 2, d // 4
    t = jnp.arange(L)
    inv = ROPE_BASE ** (-jnp.arange(nf, dtype=F32) / nf)

    def rot(u, pos):
        ang = pos.astype(F32)[:, None] * inv[None, :]
        cos, sin = jnp.cos(ang)[None, :, None, :], jnp.sin(ang)[None, :, None, :]
        u1, u2 = u[..., :nf].astype(F32), u[..., nf:].astype(F32)
        return jnp.concatenate([u1 * cos - u2 * sin, u1 * sin + u2 * cos], axis=-1)

    return jnp.concatenate([rot(x[..., :half], t // GRID_W), rot(x[..., half:], t % GRID_W)], axis=-1).astype(x.dtype)


def ec_moe(h, router, w1, w3, w2):
    B, T, D = h.shape
    cap = EC_CAPACITY_FACTOR * T // N_EXPERTS
    aff = jax.nn.softmax((h @ router).astype(F32), axis=-1)
    gate, idx = lax.top_k(jnp.swapaxes(aff, 1, 2), cap)
    xin = jax.vmap(lambda hb, ib: hb[ib])(h, idx)
    hid = jax.nn.silu(jnp.einsum('becd,edf->becf', xin, w1)) * jnp.einsum('becd,edf->becf', xin, w3)
    y = jnp.einsum('becf,efd->becd', hid, w2) * gate[..., None].astype(h.dtype)
    out = jax.vmap(lambda ib, yb: jnp.zeros((T, D), yb.dtype).at[ib.reshape(-1)].add(yb.reshape(-1, D)))(idx, y)
    return out.astype(h.dtype)


def rwkv_scan(r, w, k, v, kk, a, S0, reverse, emit):
    xs = tuple(jnp.moveaxis(t, 1, 0) for t in (r, w, k, v, kk, a))

    def step(S, inp):
        rt, wt, kt, vt, kkt, at = inp
        sa = jnp.einsum('bhij,bhj->bhi', S, -kkt)
        S = S * wt[:, :, None, :] + sa[..., None] * (kkt * at)[:, :, None, :] + vt[..., :, None] * kt[:, :, None, :]
        if emit:
            return S, jnp.einsum('bhij,bhj->bhi', S, rt)
        return S, None

    S, ys = lax.scan(step, S0, xs, reverse=reverse)
    return (jnp.moveaxis(ys, 0, 1) if emit else None), S


def rwkv_readout(y, r, k, v, xg, r_k, g_up, ln_g, ln_b):
    B, L = r.shape[:2]
    hs = lambda t: t.reshape(B, L, RWKV_HEADS, RWKV_HEAD)
    yf = y.astype(F32)
    mu = jnp.mean(yf, axis=-1, keepdims=True)
    var = jnp.mean(jnp.square(yf - mu), axis=-1, keepdims=True)
    yn = ((yf - mu) * lax.rsqrt(var + RWKV_LN_EPS)).reshape(B, L, D_RWKV) * ln_g + ln_b
    bonus = (jnp.sum(hs(r) * hs(k) * r_k, axis=-1, keepdims=True) * hs(v)).reshape(B, L, D_RWKV)
    g = jax.nn.sigmoid(xg) @ g_up
    return ((yn + bonus) * g).astype(r.dtype)


def even_mixer(h_lat, h_ctx, w_in, w_out, conv_w, k_k, k_a, r_k, w0, w_up, a0, a_up, g_up, ln_g, ln_b, need_ctx):
    B = h_lat.shape[0]
    p_lat = _split(h_lat @ w_in, EVEN_SPLIT)
    p_ctx = _split(h_ctx @ w_in, EVEN_SPLIT)

    def conv_branch(u, gate_b, gate_c):
        return gate_b * conv3(gate_c * u, conv_w)

    def dir_inputs(r, k, v, xw, xa, d):
        Bq, L = r.shape[:2]
        hs = lambda t: t.reshape(Bq, L, RWKV_HEADS, RWKV_HEAD)
        w_raw = (w0[d] + jnp.tanh(xw) @ w_up[d]).astype(F32)
        decay = jnp.exp(-jnp.exp(-jax.nn.softplus(-w_raw) - 0.5))
        a = jax.nn.sigmoid(a0[d] + xa @ a_up[d])
        kkf = hs((k * k_k).astype(F32))
        kk = kkf * lax.rsqrt(jnp.sum(kkf * kkf, axis=-1, keepdims=True) + 1e-12)
        k_eff = k * (1 + (a - 1) * k_a)
        return hs(r), hs(decay), hs(k_eff), hs(v), kk, hs(a)

    S0 = jnp.zeros((B, RWKV_HEADS, RWKV_HEAD, RWKV_HEAD), F32)
    y_lat, y_ctx = 0.0, 0.0
    for d, rev in ((0, False), (1, True)):
        yc, Sc = rwkv_scan(*dir_inputs(*p_ctx[3:8], d), S0, rev, need_ctx)
        yl, _ = rwkv_scan(*dir_inputs(*p_lat[3:8], d), Sc, rev, True)
        y_lat = y_lat + yl
        if need_ctx:
            y_ctx = y_ctx + yc
    out_lat = jnp.concatenate([conv_branch(*p_lat[0:3]),
                               rwkv_readout(y_lat, *p_lat[3:6], p_lat[8], r_k, g_up, ln_g, ln_b)], axis=-1) @ w_out
    out_ctx = None
    if need_ctx:
        out_ctx = jnp.concatenate([conv_branch(*p_ctx[0:3]),
                                   rwkv_readout(y_ctx, *p_ctx[3:6], p_ctx[8], r_k, g_up, ln_g, ln_b)], axis=-1) @ w_out
    return out_lat, out_ctx


def nat_latent(q, k, v, kc, vc, rpb):
    B, H, L, dh = q.shape
    rows = L // GRID_W
    kr = min(NAT_ROWS, rows)
    kw = NAT_COLS
    qg = q.reshape(B, H, rows, GRID_W, dh)
    kg = k.reshape(B, H, rows, GRID_W, dh)
    vg = v.reshape(B, H, rows, GRID_W, dh)
    col = jnp.arange(GRID_W)
    col_idx = jnp.clip(col - kw // 2, 0, GRID_W - kw)[:, None] + jnp.arange(kw)[None, :]
    col_bias_idx = col_idx - col[:, None] + (NAT_COLS - 1)
    scale = dh ** -0.5

    def row_block(r):
        rs = jnp.clip(r - kr // 2, 0, rows - kr)
        q_r = lax.dynamic_index_in_dim(qg, r, axis=2, keepdims=False)
        k_r = lax.dynamic_slice_in_dim(kg, rs, kr, axis=2)
        v_r = lax.dynamic_slice_in_dim(vg, rs, kr, axis=2)
        k_w = k_r[:, :, :, col_idx]
        v_w = v_r[:, :, :, col_idx]
        row_bias_idx = rs + jnp.arange(kr) - r + (NAT_ROWS - 1)
        bias = rpb[:, row_bias_idx[None, :, None], col_bias_idx[:, None, :]]
        s_loc = jnp.einsum('bhqd,bhrqwd->bhqrw', q_r, k_w) * scale + bias
        s_ctx = jnp.einsum('bhqd,bhcd->bhqc', q_r, kc) * scale
        s = jnp.concatenate([s_loc.reshape(B, H, GRID_W, kr * kw), s_ctx], axis=-1).astype(F32)
        p = jax.nn.softmax(s, axis=-1).astype(v.dtype)
        p_loc = p[..., :kr * kw].reshape(B, H, GRID_W, kr, kw)
        p_ctx = p[..., kr * kw:]
        return jnp.einsum('bhqrw,bhrqwd->bhqd', p_loc, v_w) + jnp.einsum('bhqc,bhcd->bhqd', p_ctx, vc)

    out = lax.map(row_block, jnp.arange(rows))
    return jnp.moveaxis(out, 0, 2).reshape(B, H, L, dh)


def ctx_attention(q, k, v):
    s = jnp.einsum('bhqd,bhkd->bhqk', q, k).astype(F32) * (q.shape[-1] ** -0.5)
    p = jax.nn.softmax(s, axis=-1).astype(v.dtype)
    return jnp.einsum('bhqk,bhkd->bhqd', p, v)


def gla_chunked(q, k, v, logg, S0, emit):
    B, L, H, dk = q.shape
    dv = v.shape[-1]
    C = GLA_CHUNK
    n = L // C
    to_chunks = lambda t: jnp.moveaxis(t.reshape(B, n, C, H, t.shape[-1]), 1, 0)
    xs = tuple(to_chunks(t) for t in (q, k, v, logg))
    mask = jnp.tril(jnp.ones((C, C), bool))[None, :, :, None, None]

    def step(S, inp):
        qc, kc, vc, gc = inp
        b = jnp.cumsum(gc.astype(F32), axis=1)
        bC = b[:, -1]
        S_new = S * jnp.exp(bC)[..., None] + jnp.einsum('bjhd,bjhv->bhdv', kc * jnp.exp(bC[:, None] - b), vc)
        if not emit:
            return S_new, None
        decay = jnp.exp(jnp.where(mask, b[:, :, None] - b[:, None, :], -jnp.inf))
        A = jnp.einsum('bihd,bjhd,bijhd->bhij', qc, kc, decay)
        o = jnp.einsum('bhij,bjhv->bihv', A, vc) + jnp.einsum('bihd,bhdv->bihv', qc * jnp.exp(b), S)
        return S_new, o

    S, ys = lax.scan(step, S0, xs)
    o = jnp.moveaxis(ys, 0, 1).reshape(B, L, H, dv) if emit else None
    return o, S


def gla_log_gate(ga, a_up_d, a_b_d):
    B, L = ga.shape[:2]
    lg = jax.nn.log_sigmoid((ga @ a_up_d + a_b_d).astype(F32)) / GLA_GATE_TEMP
    return lg.reshape(B, L, GLA_HEADS, GLA_DK)


def gla_readout(o, gr, ln_g):
    B, L = gr.shape[:2]
    return (rmsnorm(o, ln_g).reshape(B, L, D_GLA_V) * jax.nn.silu(gr)).astype(gr.dtype)


def odd_mixer(h_lat, h_ctx, w_in, w_out, qn_g, kn_g, rpb, a_up, a_b, gla_ln_g, need_ctx):
    B, L = h_lat.shape[:2]
    Lc = h_ctx.shape[1]
    nq, nk, nv, gq, gk, gv, gr, ga = _split(h_lat @ w_in, ODD_SPLIT)
    cnq, cnk, cnv, cgq, cgk, cgv, cgr, cga = _split(h_ctx @ w_in, ODD_SPLIT)

    def nat_heads(t, g=None):
        t = t.reshape(t.shape[0], t.shape[1], NAT_HEADS, NAT_HEAD)
        if g is not None:
            t = rmsnorm(t, g)
        return jnp.swapaxes(t, 1, 2)

    kc, vc = nat_heads(cnk, kn_g), nat_heads(cnv)
    nat_lat = nat_latent(nat_heads(nq, qn_g), nat_heads(nk, kn_g), nat_heads(nv), kc, vc, rpb)
    nat_lat = jnp.swapaxes(nat_lat, 1, 2).reshape(B, L, D_NAT)

    gh = lambda t, d: t.reshape(t.shape[0], t.shape[1], GLA_HEADS, d)
    qscale = GLA_DK ** -0.5
    q = rope_2d(gh(gq, GLA_DK)) * qscale
    k = rope_2d(gh(gk, GLA_DK))
    v = gh(gv, GLA_DV)
    qc, kcg, vcg = gh(cgq, GLA_DK) * qscale, gh(cgk, GLA_DK), gh(cgv, GLA_DV)
    S0 = jnp.zeros((B, GLA_HEADS, GLA_DK, GLA_DV), F32)
    o_lat, o_ctx = 0.0, 0.0
    for d in range(2):
        f = (lambda t: t) if d == 0 else (lambda t: jnp.flip(t, axis=1))
        oc, Sc = gla_chunked(f(qc), f(kcg), f(vcg), f(gla_log_gate(cga, a_up[d], a_b[d])), S0, need_ctx)
        ol, _ = gla_chunked(f(q), f(k), f(v), f(gla_log_gate(ga, a_up[d], a_b[d])), Sc, True)
        o_lat = o_lat + f(ol)
        if need_ctx:
            o_ctx = o_ctx + f(oc)
    out_lat = jnp.concatenate([nat_lat, gla_readout(o_lat, gr, gla_ln_g)], axis=-1) @ w_out
    out_ctx = None
    if need_ctx:
        nat_ctx = jnp.swapaxes(ctx_attention(nat_heads(cnq, qn_g), kc, vc), 1, 2).reshape(B, Lc, D_NAT)
        out_ctx = jnp.concatenate([nat_ctx, gla_readout(o_ctx, cgr, gla_ln_g)], axis=-1) @ w_out
    return out_lat, out_ctx


def setup_inputs(seed: int = 0) -> dict:
    key = jax.random.key(seed)
    keys = iter(jax.random.split(key, 48))
    ne, no = (DEPTH + 1) // 2, DEPTH // 2

    def normal(shape, std):
        return std * jax.random.normal(next(keys), shape, F32)

    def lin(shape, fan_in, gain=1.0):
        return normal(shape, gain * fan_in ** -0.5)

    def gain(shape):
        return 1.0 + normal(shape, 0.02)

    return {
        'x': normal((BATCH, SEQ, D_MODEL), 1.0),
        'c': normal((BATCH, D_MODEL), 1.0),
        'ctx': normal((BATCH, CTX_LEN, D_MODEL), 1.0),
        'c_ctx': normal((D_MODEL,), 1.0),
        'ada_w': lin((DEPTH, D_MODEL, 6 * D_MODEL), D_MODEL, 0.5),
        'ada_b': normal((DEPTH, 6 * D_MODEL), 0.02),
        'norm1_g': gain((DEPTH, D_MODEL)),
        'norm2_g': gain((DEPTH, D_MODEL)),
        'ev_w_in': lin((ne, D_MODEL, EVEN_IN), D_MODEL),
        'ev_w_out': lin((ne, D_MODEL, D_MODEL), D_MODEL),
        'conv_w': lin((ne, 3, D_CONV), 3),
        'rw_k_k': 0.85 + normal((ne, D_RWKV), 0.02),
        'rw_k_a': gain((ne, D_RWKV)),
        'rw_r_k': normal((ne, RWKV_HEADS, RWKV_HEAD), 0.1),
        'rw_w0': normal((ne, 2, D_RWKV), 1.0),
        'rw_w_up': lin((ne, 2, RWKV_DECAY_RANK, D_RWKV), RWKV_DECAY_RANK, 0.5),
        'rw_a0': normal((ne, 2, D_RWKV), 0.5),
        'rw_a_up': lin((ne, 2, RWKV_ICLR_RANK, D_RWKV), RWKV_ICLR_RANK, 0.5),
        'rw_g_up': lin((ne, RWKV_GATE_RANK, D_RWKV), RWKV_GATE_RANK),
        'rw_ln_g': gain((ne, D_RWKV)),
        'rw_ln_b': normal((ne, D_RWKV), 0.02),
        'od_w_in': lin((no, D_MODEL, ODD_IN), D_MODEL),
        'od_w_out': lin((no, D_MODEL, D_MODEL), D_MODEL),
        'nat_qn_g': gain((no, NAT_HEAD)),
        'nat_kn_g': gain((no, NAT_HEAD)),
        'nat_rpb': normal((no, NAT_HEADS, 2 * NAT_ROWS - 1, 2 * NAT_COLS - 1), 0.1),
        'gla_a_up': lin((no, 2, GLA_GATE_RANK, D_GLA_K), GLA_GATE_RANK),
        'gla_a_b': normal((no, 2, D_GLA_K), 0.5),
        'gla_ln_g': gain((no, GLA_DV)),
        'moe_router': lin((DEPTH, D_MODEL, N_EXPERTS), D_MODEL),
        'moe_w1': lin((DEPTH, N_EXPERTS, D_MODEL, D_EXPERT), D_MODEL),
        'moe_w3': lin((DEPTH, N_EXPERTS, D_MODEL, D_EXPERT), D_MODEL),
        'moe_w2': lin((DEPTH, N_EXPERTS, D_EXPERT, D_MODEL), D_EXPERT),
    }


def reference(x, c, ctx, c_ctx, ada_w, ada_b, norm1_g, norm2_g,
              ev_w_in, ev_w_out, conv_w, rw_k_k, rw_k_a, rw_r_k, rw_w0, rw_w_up, rw_a0, rw_a_up, rw_g_up,
              rw_ln_g, rw_ln_b, od_w_in, od_w_out, nat_qn_g, nat_kn_g, nat_rpb, gla_a_up, gla_a_b, gla_ln_g,
              moe_router, moe_w1, moe_w3, moe_w2):
    ctx_s = ctx
    silu_c = jax.nn.silu(c)
    silu_cc = jax.nn.silu(c_ctx)
    for l in range(DEPTH):
        last = l == DEPTH - 1
        j = l // 2
        sh1, sc1, gt1, sh2, sc2, gt2 = _split((silu_c @ ada_w[l] + ada_b[l])[:, None, :], [D_MODEL] * 6)
        csh1, csc1, cgt1, csh2, csc2, cgt2 = _split(silu_cc @ ada_w[l] + ada_b[l], [D_MODEL] * 6)
        h_lat = modulate(x, norm1_g[l], sh1, sc1)
        h_ctx = modulate(ctx_s, norm1_g[l], csh1, csc1)
        if l % 2 == 0:
            y_lat, y_ctx = even_mixer(h_lat, h_ctx, ev_w_in[j], ev_w_out[j], conv_w[j], rw_k_k[j], rw_k_a[j], rw_r_k[j],
                                      rw_w0[j], rw_w_up[j], rw_a0[j], rw_a_up[j], rw_g_up[j], rw_ln_g[j], rw_ln_b[j],
                                      not last)
        else:
            y_lat, y_ctx = odd_mixer(h_lat, h_ctx, od_w_in[j], od_w_out[j], nat_qn_g[j], nat_kn_g[j], nat_rpb[j],
                                     gla_a_up[j], gla_a_b[j], gla_ln_g[j], not last)
        x = x + gt1 * y_lat
        x = x + gt2 * ec_moe(modulate(x, norm2_g[l], sh2, sc2), moe_router[l], moe_w1[l], moe_w3[l], moe_w2[l])
        if not last:
            ctx_s = ctx_s + cgt1 * y_ctx
            ctx_s = ctx_s + cgt2 * ec_moe(modulate(ctx_s, norm2_g[l], csh2, csc2),
                                          moe_router[l], moe_w1[l], moe_w3[l], moe_w2[l])
    return x
```

```python
import ml_dtypes
import contextlib
import numpy as np
import concourse.bass as bass
import concourse.mybir as mybir
from concourse.bass_utils import run_bass_kernel_spmd

F32 = mybir.dt.float32
BF16 = mybir.dt.bfloat16
I32 = mybir.dt.int32
AF = mybir.ActivationFunctionType
ALU = mybir.AluOpType
AX = mybir.AxisListType

ENGS = ("tensor", "vector", "scalar", "gpsimd", "sync")
N_DMA_SEM = 8
import os as _os
REORDER = _os.environ.get('REORDER', '1') == '1'
PE_WIN = int(_os.environ.get('PE_WIN', '1'))


class Prog:
    def __init__(self, same_engine_sync=True, reorder=None):
        self.reorder = REORDER if reorder is None else reorder
        self.seg_noreorder = set()
        self.nc = bass.Bass("TRN2", target_bir_lowering=False)
        self.stack = contextlib.ExitStack()
        self.ops = []
        self.same_engine_sync = same_engine_sync
        self.n_names = 0

    def dram(self, name, shape, dt, kind):
        k = {"in": "ExternalInput", "out": "ExternalOutput", "tmp": "Internal"}[kind]
        return self.nc.dram_tensor(name, list(shape), dt, kind=k).ap()

    def sb(self, name, shape, dt):
        return self.stack.enter_context(self.nc.sbuf_tensor(name, list(shape), dt))

    def ps(self, name, shape, dt=F32):
        return self.stack.enter_context(self.nc.psum_tensor(name, list(shape), dt))

    @staticmethod
    def _keys(lst):
        out = []
        for x in lst:
            if x is None:
                continue
            if isinstance(x, tuple):
                t, sub = x
                out.append((t if isinstance(t, str) else t.name, sub))
            elif isinstance(x, str):
                out.append((x, None))
            else:
                out.append((x.name, None))
        return out

    def op(self, eng, meth, R=None, W=None, **kw):
        if W is None:
            W = [v for k, v in kw.items() if k in ("out", "accum_out") and hasattr(v, "name")]
        if R is None:
            R = [v for k, v in kw.items() if k not in ("out", "accum_out") and hasattr(v, "name") and hasattr(v, "partition_size")]
        self.ops.append((eng, "c", (meth, kw), self._keys(R), self._keys(W)))

    def dma(self, eng, out, in_, R=None, W=None, **kw):
        if W is None:
            W = [out]
        if R is None:
            R = [in_]
        self.ops.append((eng, "d", ("dma_start", dict(out=out, in_=in_, **kw)), self._keys(R), self._keys(W)))

    def idma(self, R, W, **kw):
        self.ops.append(("gpsimd", "d", ("indirect_dma_start", kw), self._keys(R), self._keys(W)))

    def barrier(self):
        self.ops.append(("*", "b", None, [], []))

    def mark_no_reorder(self):
        self.seg_noreorder.add(sum(1 for o in self.ops if o[1] == "b"))

    @contextlib.contextmanager
    def scope(self):
        outer, self.stack = self.stack, contextlib.ExitStack()
        try:
            yield
        finally:
            self.barrier()
            self.stack.close()
            self.stack = outer

    def V(self, meth, **kw):
        self.op("vector", meth, **kw)

    def S(self, meth, **kw):
        self.op("scalar", meth, **kw)

    def G(self, meth, **kw):
        self.op("gpsimd", meth, **kw)

    def T(self, meth, **kw):
        self.op("tensor", meth, **kw)

    @staticmethod
    def _cost(eng, kind, fn):
        meth, kw = fn
        o = kw.get("out", None)
        if o is None:
            o = kw.get("ap", None)
        n = 1
        if o is not None and hasattr(o, "shape"):
            for d in o.shape[1:]:
                n *= d
        if kind == "d":
            if meth == "indirect_dma_start":
                return 1.6, 3.0
            return (0.6 if eng == "gpsimd" else 0.15), 2.0 + n * 128 * 4 / 150e3
        if eng == "tensor":
            return 0.18 + 0.0005 * n, 0.0
        if eng == "vector":
            return 0.08 + 0.00105 * n, 0.0
        if eng == "scalar":
            return 0.22 + 0.00105 * n, 0.0
        return 0.12 + 0.0022 * n, 0.0

    def build(self):
        nc = self.nc
        st = self.stack
        ops = self.ops
        NOPS = len(ops)
        preds = [None] * NOPS
        seg_of = [0] * NOPS
        state = {}
        seg = 0

        def subs_of(name, sub):
            d = state.get(name)
            if d is None:
                return []
            if sub is None:
                return list(d.values())
            res = []
            if None in d:
                res.append(d[None])
            if sub in d:
                res.append(d[sub])
            return res

        for i, (eng, kind, fn, R, W) in enumerate(ops):
            if kind == "b":
                seg += 1
                state.clear()
                seg_of[i] = seg
                continue
            seg_of[i] = seg
            ps = set()
            for (n, sb_) in R:
                for en in subs_of(n, sb_):
                    if en["w"] is not None:
                        ps.add(en["w"])
            for (n, sb_) in W:
                for en in subs_of(n, sb_):
                    if en["w"] is not None:
                        ps.add(en["w"])
                    ps.update(en["r"])
            ps.discard(i)
            preds[i] = sorted(ps)
            for (n, sb_) in R:
                d = state.setdefault(n, {})
                if sb_ not in d:
                    d[sb_] = dict(w=None, r=[])
                d[sb_]["r"].append(i)
            for (n, sb_) in W:
                if sb_ is None:
                    state[n] = {None: dict(w=i, r=[])}
                else:
                    d = state.setdefault(n, {})
                    d[sb_] = dict(w=i, r=[])
        WIN = 24
        order = {e: [] for e in ENGS}
        fin = [0.0] * NOPS
        done = [False] * NOPS
        i0 = 0
        tnow = {e: 0.0 for e in ENGS}
        while i0 < NOPS:
            if ops[i0][1] == "b":
                for e in ENGS:
                    order[e].append(-1)
                tb = max(tnow.values())
                for e in ENGS:
                    tnow[e] = tb
                i0 += 1
                continue
            i1 = i0
            while i1 < NOPS and ops[i1][1] != "b":
                i1 += 1
            q = {e: [] for e in ENGS}
            for i in range(i0, i1):
                q[ops[i][0]].append(i)
            head = {e: 0 for e in ENGS}
            remaining = i1 - i0
            if (not self.reorder) or (seg_of[i0] in self.seg_noreorder):
                for i in range(i0, i1):
                    order[ops[i][0]].append(i)
                remaining = 0
            pe_forced = None
            pos_in_q = {i: j for j, i in enumerate(q["tensor"])}
            while remaining:
                best = None
                for e in ENGS:
                    lst = q[e]
                    h = head[e]
                    if e == "tensor" and pe_forced is not None:
                        i = lst[pe_forced]
                        tr = tnow[e]
                        ok = True
                        for p in preds[i]:
                            if not done[p]:
                                ok = False
                                break
                            lat = 0.0 if ops[p][0] == e else 0.9
                            tr = max(tr, fin[p] + lat)
                        if ok:
                            key = (tr, i)
                            if best is None or key < best[0]:
                                best = (key, e, i)
                        continue
                    while h < len(lst) and done[lst[h]]:
                        h += 1
                    head[e] = h
                    cnt = 0
                    j = h
                    while j < len(lst) and cnt < (PE_WIN if e == "tensor" else WIN):
                        i = lst[j]
                        j += 1
                        if done[i]:
                            continue
                        cnt += 1
                        ok = True
                        tr = tnow[e]
                        for p in preds[i]:
                            if not done[p]:
                                ok = False
                                break
                            lat = 0.0 if (ops[p][0] == e and e == "tensor") else (0.35 if ops[p][0] == e else 0.9)
                            if fin[p] + lat > tr:
                                tr = fin[p] + lat
                        if not ok:
                            continue
                        key = (tr, i)
                        if best is None or key < best[0]:
                            best = (key, e, i)
                        if tr <= tnow[e]:
                            break
                assert best is not None, "scheduler deadlock"
                (tr, _), e, i = best
                iss, lat = self._cost(e, ops[i][1], ops[i][2])
                tnow[e] = tr + iss
                fin[i] = tr + iss + lat
                done[i] = True
                order[e].append(i)
                remaining -= 1
                if e == "tensor":
                    kw_ = ops[i][2][1]
                    if ops[i][2][0] == "matmul" and kw_.get("stop", True) is False:
                        pe_forced = pos_in_q[i] + 1
                    else:
                        pe_forced = None
            i0 = i1
        self.sim_time_us = max(tnow.values())
        EPOCH = 20000
        ntot = {e: sum(1 for i in order[e] if i >= 0 and ops[i][1] == "c") for e in ENGS if e != "sync"}
        esem = {e: [st.enter_context(nc.semaphore(f"sem_{e}_{i}")) for i in range(ntot[e] // EPOCH + 1)] for e in ntot}
        dsem = {e: [st.enter_context(nc.semaphore(f"dsem_{e}_{i}")) for i in range(N_DMA_SEM)]
                for e in ("sync", "gpsimd", "scalar")}
        handle = [None] * NOPS
        ecount = {e: 0 for e in esem}
        dcount = {e: 0 for e in dsem}
        for e in ENGS:
            for i in order[e]:
                if i < 0:
                    continue
                if ops[i][1] == "c":
                    ecount[e] += 1
                    handle[i] = ("e", e, ecount[e])
                else:
                    k = dcount[e]
                    dcount[e] += 1
                    handle[i] = ("d", e, k % N_DMA_SEM, 16 * (k // N_DMA_SEM + 1))
        streams = {e: [] for e in ENGS}
        waited = {e: {} for e in ENGS}

        def need_wait(eng, h):
            if h is None:
                return
            if h[0] == "e":
                _, e2, cnt = h
                if e2 == eng and (eng == "tensor" or not self.same_engine_sync):
                    return
                key = ("e", e2)
            else:
                _, q_, idx, cnt = h
                key = ("d", q_, idx)
            if waited[eng].get(key, 0) >= cnt:
                return
            waited[eng][key] = cnt
            streams[eng].append(("wait", h))

        for e in ENGS:
            ec = {x: 0 for x in esem}
            dc = {x: 0 for x in dsem}
            bar_targets = []
        cum = {e: [] for e in ENGS}
        for e in ENGS:
            ce, cd = 0, 0
            for i in order[e]:
                if i < 0:
                    cum[e].append((ce, cd))
                elif ops[i][1] == "c":
                    ce += 1
                else:
                    cd += 1
            cum[e].append((ce, cd))
        def dma_latest(q_, n, idx):
            if n <= idx:
                return None
            last = n - 1 - ((n - 1 - idx) % N_DMA_SEM)
            return ("d", q_, idx, 16 * (last // N_DMA_SEM + 1))
        for e in ENGS:
            bi = 0
            for i in order[e]:
                if i < 0:
                    for e2 in ENGS:
                        ce, cd = cum[e2][bi]
                        if e2 in esem and ce and e2 != e:
                            need_wait(e, ("e", e2, ce))
                        if e2 in dsem:
                            for idx in range(N_DMA_SEM):
                                need_wait(e, dma_latest(e2, cd, idx))
                    bi += 1
                    continue
                for p in preds[i]:
                    need_wait(e, handle[p])
                h = handle[i]
                if h[0] == "d" and h[3] > 16:
                    need_wait(e, ("d", h[1], h[2], h[3] - 16))
                streams[e].append(("op", ops[i][1], ops[i][2], h))
        for q_ in dsem:
            for idx in range(N_DMA_SEM):
                need_wait(q_, dma_latest(q_, dcount[q_], idx))
        for e in esem:
            if ecount[e]:
                need_wait("sync", ("e", e, ecount[e]))

        def emit(engname, engobj):
            for item in streams[engname]:
                if item[0] == "wait":
                    h = item[1]
                    if h[0] == "e":
                        engobj.wait_ge(esem[h[1]][(h[2] - 1) // EPOCH], (h[2] - 1) % EPOCH + 1)
                    else:
                        engobj.wait_ge(dsem[h[1]][h[2]], h[3])
                else:
                    _, kind, (meth, kw), h = item
                    try:
                        ins = getattr(engobj, meth)(**kw)
                    except Exception:
                        print("EMIT FAIL", engname, meth, {k: (getattr(v, "shape", v), getattr(v, "name", "")) for k, v in kw.items()})
                        raise
                    if h[0] == "e":
                        ins.then_inc(esem[h[1]][(h[2] - 1) // EPOCH], 1)
                    else:
                        ins.then_inc(dsem[h[1]][h[2]], 16)

        with nc.Block() as block:
            @block.sync
            def _(e):
                emit("sync", e)

            @block.tensor
            def _(e):
                emit("tensor", e)

            @block.vector
            def _(e):
                emit("vector", e)

            @block.scalar
            def _(e):
                emit("scalar", e)

            @block.gpsimd
            def _(e):
                emit("gpsimd", e)
        self.n_instr = {e: len(streams[e]) for e in ENGS}
        st.close()
        return nc


BF = ml_dtypes.bfloat16
NCORE = 8
_CACHE = {}


def run_prog(key, builder, in_maps):
    if key not in _CACHE:
        _CACHE[key] = builder()
    res = run_bass_kernel_spmd(_CACHE[key], in_maps, core_ids=list(range(NCORE)))
    return res.results


def kmajor(w):
    K, N = w.shape
    return np.ascontiguousarray(w.reshape(K // 128, 128, N).transpose(1, 0, 2))


def build_p0():
    P = Prog()
    cT_d = P.dram("cT", [128, 8, 5], F32, "in")
    aw_d = P.dram("aw", [128, 8, 1536], F32, "in")
    ab_d = P.dram("ab", [5, 1536], F32, "in")
    ng_d = P.dram("ng", [5, 2, 256], F32, "in")
    o_d = P.dram("o", [5, 6, 256], F32, "out")
    cT = P.sb("cTs", [128, 8, 5], F32)
    sc = P.sb("sc", [128, 8, 5], F32)
    aw = P.sb("aws", [128, 8, 1536], F32)
    ab = P.sb("abs", [5, 1536], F32)
    ng = P.sb("ngs", [5, 2, 256], F32)
    m = P.sb("m", [5, 1536], F32)
    o = P.sb("os", [5, 6, 256], F32)
    pms = [P.ps(f"pm{i}", [5, 512]) for i in range(3)]
    P.dma("sync", cT[:], cT_d[:, :, :])
    for k in range(8):
        P.dma("sync", aw[:, k, :], aw_d[:, k, :], W=[(aw, k)])
    P.dma("sync", ab[:], ab_d[:, :])
    P.dma("sync", ng[:], ng_d[:, :, :])
    P.S("activation", out=sc[:], in_=cT[:], func=AF.Silu)
    for cb in range(3):
        for k in range(8):
            P.T("matmul", out=pms[cb][:], lhsT=sc[:, k, :], rhs=aw[:, k, cb * 512:(cb + 1) * 512],
                start=(k == 0), stop=(k == 7), R=[sc, (aw, k)])
        P.V("tensor_tensor", out=m[:, cb * 512:(cb + 1) * 512], in0=pms[cb][:], in1=ab[:, cb * 512:(cb + 1) * 512], op=ALU.add,
            W=[(m, cb)])
    P.V("tensor_copy", out=o[:].rearrange("p a b -> p (a b)"), in_=m[:])
    P.V("scalar_tensor_tensor", out=o[:, 1, :], in0=m[:, 256:512], scalar=1.0, in1=ng[:, 0, :], op0=ALU.add, op1=ALU.mult)
    P.V("scalar_tensor_tensor", out=o[:, 4, :], in0=m[:, 1024:1280], scalar=1.0, in1=ng[:, 1, :], op0=ALU.add, op1=ALU.mult)
    P.dma("sync", o_d[:, :, :], o[:])
    return P.build()


def run_p0(inp):
    c5 = np.concatenate([inp["c"], inp["c_ctx"][None, :]], 0)
    cT = kmajor(np.ascontiguousarray(c5.T))
    maps = []
    for c in range(NCORE):
        l, q = c // 4, c % 4
        cols = np.concatenate([np.arange(k * 1024 + q * 256, k * 1024 + q * 256 + 256) for k in range(6)])
        aw = kmajor(np.ascontiguousarray(inp["ada_w"][l][:, cols]))
        ab = np.ascontiguousarray(np.broadcast_to(inp["ada_b"][l][cols][None, :], (5, 1536)))
        ng = np.stack([inp["norm1_g"][l][q * 256:(q + 1) * 256], inp["norm2_g"][l][q * 256:(q + 1) * 256]], 0)
        ng = np.ascontiguousarray(np.broadcast_to(ng[None], (5, 2, 256)))
        maps.append(dict(cT=cT, aw=aw, ab=ab, ng=ng))
    res = run_prog("p0", build_p0, maps)
    mods = np.zeros((2, 5, 6, 1024), np.float32)
    for c in range(NCORE):
        l, q = c // 4, c % 4
        mods[l, :, :, q * 256:(q + 1) * 256] = res[c]["o"]
    return mods


def bc128(v):
    return np.ascontiguousarray(np.broadcast_to(v[None], (128,) + v.shape))


import os
RW_CUT = int(os.environ.get('RW_CUT', '0'))
SAME_ENGINE_SYNC = os.environ.get('SES', '1') == '1'
NO_REORDER = set(os.environ.get('NO_REORDER', 'gla,nat').split(','))
T_TOK = 8448
NTILE = 66
C0 = 0.6065306597126334


def emit_weight_bf16(P, w_d, w_sb, K8, N, tag, stg=None):
    if stg is None:
        stg = [P.sb(f"wstg{tag}{i}", [128, N], F32) for i in range(2)]
    for k in range(K8):
        s = stg[k % 2]
        P.dma("sync", s[:, 0:N], w_d[:, k, :])
        if k % 2 == 0:
            P.V("tensor_copy", out=w_sb[:, k, :], in_=s[:, 0:N], W=[(w_sb, k)])
        else:
            P.G("tensor_copy", out=w_sb[:, k, :], in_=s[:, 0:N], W=[(w_sb, k)])
    return stg


def emit_norm_mod(P, xt, gs, sh, hb, tmp, ss, rs, epsb, hf, hf_out=None):
    P.S("activation", out=tmp[:], in_=xt[:], func=AF.Square, accum_out=ss[:])
    P.S("activation", out=rs[:], in_=ss[:], func=AF.Sqrt, scale=1.0 / 1024, bias=epsb[:, 0:1])
    P.V("reciprocal", out=rs[:], in_=rs[:])
    P.V("scalar_tensor_tensor", out=hf[:], in0=xt[:], scalar=rs[:, 0:1], in1=gs, op0=ALU.mult, op1=ALU.mult)
    P.G("tensor_tensor", out=hb[:], in0=hf[:], in1=sh, op=ALU.add)
    if hf_out is not None:
        P.G("tensor_tensor", out=hf_out[:], in0=hf[:], in1=sh, op=ALU.add)


class Ctx:
    pass


def stage_pre(P, G, N, w_d, mod_d, lyr, x_src, p_scr, post=None):
    modS = P.sb(f"s1{lyr}_modS", [128, 2, 1024], F32)
    modC = P.sb(f"s1{lyr}_modC", [128, 2, 1024], F32)
    w_sb = P.sb(f"s1{lyr}_w", [128, 8, N], BF16)
    P.dma("sync", modS[:], mod_d[lyr, 0, 0:2].rearrange("v p f -> p v f"))
    P.dma("sync", modC[:], mod_d[lyr, 1, 0:2].rearrange("v p f -> p v f"))
    emit_weight_bf16(P, w_d, w_sb, 8, N, f"s1{lyr}")
    xts = [P.sb(f"s1{lyr}_xt{i}", [128, 1024], F32) for i in range(2)]
    tmp = P.sb(f"s1{lyr}_tmp", [128, 1024], F32)
    hf = P.sb(f"s1{lyr}_hf", [128, 1024], F32)
    hb = P.sb(f"s1{lyr}_hb", [128, 1024], BF16)
    hT = P.sb(f"s1{lyr}_hT", [128, 8, 128], BF16)
    ss = P.sb(f"s1{lyr}_ss", [128, 1], F32)
    rs = P.sb(f"s1{lyr}_rs", [128, 1], F32)
    pos = [P.sb(f"s1{lyr}_po{i}", [128, N], F32) for i in range(2)]
    pT = P.ps(f"s1{lyr}_pT", [128, 8, 128], BF16)
    pms = [P.ps(f"s1{lyr}_pm{i}", [128, 512]) for i in range(4)]
    nblk = (N + 511) // 512
    ev = 0
    for t in range(NTILE):
        xt = xts[t % 2]
        po = pos[t % 2]
        mod = modC if t < 2 else modS
        P.dma("sync", xt[:], x_src[t * 128:(t + 1) * 128, :])
        emit_norm_mod(P, xt, mod[:, 1, :], mod[:, 0, :], hb, tmp, ss, rs, G.epsb, hf)
        for k in range(8):
            P.T("transpose", out=pT[:, k, :], in_=hb[:, k * 128:(k + 1) * 128], identity=G.identB[:])
        P.S("copy", out=hT[:], in_=pT[:])
        for nb in range(nblk):
            c0, c1 = nb * 512, min(N, nb * 512 + 512)
            pm = pms[ev % 4]
            for k in range(8):
                P.T("matmul", out=pm[:, 0:c1 - c0], lhsT=hT[:, k, :], rhs=w_sb[:, k, c0:c1], start=(k == 0), stop=(k == 7),
                    R=[hT, (w_sb, k)])
            if ev % 2 == 0:
                P.V("tensor_copy", out=po[:, c0:c1], in_=pm[:, 0:c1 - c0], W=[(po, nb)])
            else:
                P.S("copy", out=po[:, c0:c1], in_=pm[:, 0:c1 - c0], W=[(po, nb)])
            ev += 1
        P.dma("sync", p_scr[t * 128:(t + 1) * 128, :], po[:])
        if post is not None:
            post(t, po)


def rwkv_consts(inp):
    def two(v):
        return np.concatenate([np.broadcast_to(v[0][None], (64, 512)), np.broadcast_to(v[1][None], (64, 512))], 0)
    s = np.arange(64)
    early = (s[:, None] < s[None, :]).astype(np.float32)
    earlyeq = (s[:, None] <= s[None, :]).astype(np.float32)
    MA = np.concatenate([early, early.T], 0)
    MB = np.concatenate([earlyeq, earlyeq.T], 0)
    MC = np.concatenate([early.T, early], 0)
    eye = np.concatenate([np.eye(64, dtype=np.float32)] * 2, 0)
    t8 = lambda m: np.tile(m, (1, 8))
    rwc = np.stack([two(inp["rw_w0"][0]), two(inp["rw_a0"][0]), bc128(inp["rw_k_k"][0]), bc128(inp["rw_k_a"][0]),
                    t8(MA), t8(MB), t8(MC), t8(eye)], 1).astype(np.float32)
    wup = np.stack([inp["rw_w_up"][0], inp["rw_a_up"][0]], 0).transpose(2, 0, 1, 3)
    cumM = np.zeros((128, 128), np.float32)
    cumM[:64, :64] = earlyeq
    cumM[64:, 64:] = earlyeq.T
    sel = np.zeros((128, 2), np.float32)
    sel[63, 0] = 1.0
    sel[64, 1] = 1.0
    return dict(rwc=np.ascontiguousarray(rwc), wup=np.ascontiguousarray(wup.astype(np.float32)), cumM=cumM, sel=sel)


def stage_rwkv(P, G, p_scr, yf_scr, yr_scr, rwc_d, wup_d, cumM_d, sel_d, nsteps=132):
    NH = 8
    f32t = lambda n, shp=(128, 512): P.sb("rw_" + n, list(shp), F32)
    rwc = f32t("rwc", (128, 8, 512))
    wup = f32t("wup", (64, 2, 2, 512))
    cumM = f32t("cumM", (128, 128))
    sel = f32t("sel", (128, 2))
    P.dma("sync", rwc[:], rwc_d[:, :, :])
    P.dma("sync", wup[:], wup_d[:, :, :, :])
    P.dma("sync", cumM[:], cumM_d[:, :])
    P.dma("sync", sel[:], sel_d[:, :])
    w0b, a0b, kkb, kab, MA, MB, MC, EYE = [rwc[:, i, :] for i in range(8)]
    rkvs = [f32t(f"rkv{i}", (128, 1536)) for i in range(2)]
    xwas = [f32t(f"xwa{i}", (128, 128)) for i in range(2)]
    txw = f32t("txw", (128, 64))
    xT = f32t("xT", (64, 2, 128))
    wr, sg, ar, av = f32t("wr"), f32t("sg"), f32t("ar"), f32t("av")
    E, Einv, Eprev, Lp = f32t("E"), f32t("Einv"), f32t("Eprev"), f32t("Lp")
    kk0, sq, kk, t1, keff, bb = f32t("kk0"), f32t("sq"), f32t("kk"), f32t("t1"), f32t("keff"), f32t("bb")
    n2, rn = f32t("n2", (128, 8)), f32t("rn", (128, 8))
    tiny = f32t("tiny", (128, 1))
    P.G("memset", ap=tiny[:], constant=1e-12, R=[], W=[tiny])
    bft = lambda n, shp=(128, 512): P.sb("rw_" + n, list(shp), BF16)
    kap_t, kt_b, bt_b, rt_t, vb = bft("kap_t"), bft("kt_b"), bft("bt_b"), bft("rt_t"), bft("vb")
    kapT, ktT, btT, rtT = [bft(n, (64, 8, 128)) for n in ("kapT", "ktT", "btT", "rtT")]
    AkkT_s, BrkT_s, BrbT_s, negU = bft("AkkT_s"), bft("BrkT_s"), bft("BrbT_s"), bft("negU")
    Xs = [bft(f"X{i}") for i in range(2)]
    Xts = [bft(f"Xt{i}") for i in range(2)]
    Tts = [bft(f"Tt{i}") for i in range(2)]
    W1a, y_sb = f32t("W1a"), f32t("y_sb")
    W1 = bft("W1")
    Qf = f32t("Qf", (64, 1024))
    Qb = bft("Qb", (64, 1024))
    tmpq = f32t("tmpq", (64, 1024))
    cC = f32t("cC", (64, 16))
    P.V("memset", ap=Qf[:], constant=0.0, R=[], W=[Qf])
    P.V("memset", ap=Qb[:], constant=0.0, R=[], W=[Qb])
    pf = [P.ps(f"rpf{i}", [128, 512]) for i in range(6)]
    pb = [P.ps(f"rpb{i}", [64, 8, 128], BF16) for i in range(2)]
    pfi = [0]

    def nxt():
        t = pf[pfi[0] % 6]
        pfi[0] += 1
        return t

    def unit_mm(out_t, lhs_t, rhs_t, **kw):
        for h in range(NH):
            for d in range(2):
                r0, c0 = 64 * d, 64 * h
                P.T("matmul", out=out_t[r0:r0 + 64, c0:c0 + 64], lhsT=lhs_t[r0:r0 + 64, c0:c0 + 64],
                    rhs=rhs_t[r0:r0 + 64, c0:c0 + 64], start=True, stop=True, R=[lhs_t, rhs_t], W=[out_t])

    def feat_mm(out_t, lT, rT_):
        for h in range(NH):
            for d in range(2):
                P.T("matmul", out=out_t[64 * d:64 * d + 64, 64 * h:64 * h + 64], lhsT=lT[:, h, 64 * d:64 * d + 64],
                    rhs=rT_[:, h, 64 * d:64 * d + 64], start=True, stop=True, R=[lT, rT_], W=[out_t])

    for n in range(nsteps):
        m0 = n
        m1 = (3 - n) if n < 4 else (135 - n)
        rkv, xwa = rkvs[n % 2], xwas[n % 2]
        for d, m in ((0, m0), (1, m1)):
            P.dma("sync", rkv[64 * d:64 * d + 64, :], p_scr[64 * m:64 * m + 64, 1536:3072], W=[(rkv, d)])
            P.dma("sync", xwa[64 * d:64 * d + 64, :], p_scr[64 * m:64 * m + 64, 3072:3200], W=[(xwa, d)])
        r_, k_, v_ = rkv[:, 0:512], rkv[:, 512:1024], rkv[:, 1024:1536]
        P.S("activation", out=txw[:], in_=xwa[:, 0:64], func=AF.Tanh)
        pt = nxt()
        P.T("transpose", out=pt[0:64, 0:128], in_=txw[:], identity=G.identF[:])
        P.T("transpose", out=pt[0:64, 128:256], in_=xwa[:, 64:128], identity=G.identF[:])
        P.V("tensor_copy", out=xT[:].rearrange("p a b -> p (a b)"), in_=pt[0:64, 0:256])
        pw, pa = nxt(), nxt()
        for d in range(2):
            P.T("matmul", out=pw[64 * d:64 * d + 64, :], lhsT=xT[:, 0, 64 * d:64 * d + 64], rhs=wup[:, 0, d, :], start=True, stop=True)
            P.T("matmul", out=pa[64 * d:64 * d + 64, :], lhsT=xT[:, 1, 64 * d:64 * d + 64], rhs=wup[:, 1, d, :], start=True, stop=True)
        P.V("tensor_tensor", out=wr[:], in0=pw[:], in1=w0b, op=ALU.add)
        P.S("activation", out=sg[:], in_=wr[:], func=AF.Sigmoid)
        P.V("tensor_tensor", out=ar[:], in0=pa[:], in1=a0b, op=ALU.add)
        P.S("activation", out=av[:], in_=ar[:], func=AF.Sigmoid)
        if RW_CUT == 1:
            continue
        pL = nxt()
        P.T("matmul", out=pL[:], lhsT=cumM[:], rhs=sg[:], start=True, stop=True)
        P.S("activation", out=E[:], in_=pL[:], func=AF.Exp, scale=-C0)
        P.S("activation", out=Einv[:], in_=pL[:], func=AF.Exp, scale=C0)
        P.V("tensor_tensor", out=Lp[:], in0=pL[:], in1=sg[:], op=ALU.subtract)
        P.S("activation", out=Eprev[:], in_=Lp[:], func=AF.Exp, scale=-C0)
        if RW_CUT == 2:
            continue
        P.G("tensor_tensor", out=kk0[:], in0=k_, in1=kkb, op=ALU.mult, R=[rkv, rwc])
        P.G("tensor_tensor", out=sq[:], in0=kk0[:], in1=kk0[:], op=ALU.mult)
        P.V("tensor_reduce", out=n2[:], in_=sq[:].rearrange("p (h j) -> p h j", h=NH), axis=AX.X, op=ALU.add)
        P.S("activation", out=rn[:], in_=n2[:], func=AF.Sqrt, bias=tiny[:, 0:1])
        P.V("reciprocal", out=rn[:], in_=rn[:])
        P.V("tensor_tensor", out=kk[:].rearrange("p (h j) -> p h j", h=NH), in0=kk0[:].rearrange("p (h j) -> p h j", h=NH),
            in1=rn[:].unsqueeze(2).to_broadcast([128, NH, 64]), op=ALU.mult)
        P.V("scalar_tensor_tensor", out=t1[:], in0=av[:], scalar=-1.0, in1=kab, op0=ALU.add, op1=ALU.mult, R=[av, rwc])
        P.V("scalar_tensor_tensor", out=keff[:], in0=t1[:], scalar=1.0, in1=k_, op0=ALU.add, op1=ALU.mult, R=[t1, rkv])
        P.G("tensor_tensor", out=bb[:], in0=kk[:], in1=av[:], op=ALU.mult)
        if RW_CUT == 3:
            continue
        P.V("tensor_tensor", out=kap_t[:], in0=kk[:], in1=Eprev[:], op=ALU.mult)
        P.G("tensor_tensor", out=kt_b[:], in0=keff[:], in1=Einv[:], op=ALU.mult)
        P.V("tensor_tensor", out=bt_b[:], in0=bb[:], in1=Einv[:], op=ALU.mult)
        P.G("tensor_tensor", out=rt_t[:], in0=r_, in1=E[:], op=ALU.mult, R=[rkv, E])
        P.S("copy", out=vb[:], in_=v_, R=[rkv])
        if RW_CUT == 4:
            continue
        pc = nxt()
        for h in range(NH):
            P.T("matmul", out=pc[0:64, 2 * h:2 * h + 2], lhsT=E[:, 64 * h:64 * h + 64], rhs=sel[:], start=True, stop=True)
        P.V("tensor_copy", out=cC[:], in_=pc[0:64, 0:16])
        if RW_CUT == 5:
            continue
        for qi, (src, dst) in enumerate(((kap_t, kapT), (kt_b, ktT), (bt_b, btT), (rt_t, rtT))):
            pbt = pb[qi % 2]
            for h in range(NH):
                P.T("transpose", out=pbt[:, h, :], in_=src[:, 64 * h:64 * h + 64], identity=G.identB[:])
            if qi % 2 == 0:
                P.S("copy", out=dst[:], in_=pbt[:])
            else:
                P.V("tensor_copy", out=dst[:], in_=pbt[:])
        if RW_CUT == 6:
            continue
        X, Xt, Tt = Xs[0], Xts[0], Tts[0]
        p1 = nxt(); feat_mm(p1, ktT, kapT)
        P.V("tensor_tensor", out=AkkT_s[:], in0=p1[:], in1=MA, op=ALU.mult, R=[p1, rwc])
        p2 = nxt(); feat_mm(p2, btT, kapT)
        P.V("tensor_tensor", out=X[:], in0=p2[:], in1=MA, op=ALU.mult, R=[p2, rwc])
        p3 = nxt(); feat_mm(p3, kapT, btT)
        P.V("tensor_tensor", out=Xt[:], in0=p3[:], in1=MC, op=ALU.mult, R=[p3, rwc])
        p4 = nxt(); feat_mm(p4, ktT, rtT)
        P.V("tensor_tensor", out=BrkT_s[:], in0=p4[:], in1=MB, op=ALU.mult, R=[p4, rwc])
        p5 = nxt(); feat_mm(p5, btT, rtT)
        P.V("tensor_tensor", out=BrbT_s[:], in0=p5[:], in1=MB, op=ALU.mult, R=[p5, rwc])
        if RW_CUT == 7:
            continue
        P.G("tensor_tensor", out=Tt[:], in0=EYE, in1=X[:], op=ALU.subtract, R=[rwc, X])
        cur = 0
        for lev in range(1, 6):
            X, Xt, Tt = Xs[cur], Xts[cur], Tts[cur]
            Xn, Xtn, Ttn = Xs[1 - cur], Xts[1 - cur], Tts[1 - cur]
            pxt = nxt(); unit_mm(pxt, X, Xt)
            P.S("copy", out=Xtn[:], in_=pxt[:])
            if lev < 5:
                px = nxt(); unit_mm(px, Xt, X)
                P.V("tensor_copy", out=Xn[:], in_=px[:])
            ptt = nxt(); unit_mm(ptt, Xtn, Tt)
            P.V("tensor_tensor", out=Ttn[:], in0=ptt[:], in1=Tt[:], op=ALU.add)
            cur = 1 - cur
        Tt = Tts[cur]
        if RW_CUT == 8:
            continue
        pwa = nxt()
        for h in range(NH):
            for d in range(2):
                u = 8 * d + h
                P.T("matmul", out=pwa[64 * d:64 * d + 64, 64 * h:64 * h + 64], lhsT=kapT[:, h, 64 * d:64 * d + 64],
                    rhs=Qb[:, 64 * u:64 * u + 64], start=True, stop=True, R=[kapT, Qb], W=[pwa])
        P.S("copy", out=W1a[:], in_=pwa[:])
        pwb = nxt(); unit_mm(pwb, AkkT_s, vb)
        P.V("tensor_tensor", out=W1[:], in0=pwb[:], in1=W1a[:], op=ALU.add)
        pu = nxt(); unit_mm(pu, Tt, W1)
        P.S("mul", out=negU[:], in_=pu[:], mul=-1.0)
        if RW_CUT == 9:
            continue
        pya = nxt()
        for h in range(NH):
            for d in range(2):
                u = 8 * d + h
                P.T("matmul", out=pya[64 * d:64 * d + 64, 64 * h:64 * h + 64], lhsT=rtT[:, h, 64 * d:64 * d + 64],
                    rhs=Qb[:, 64 * u:64 * u + 64], start=True, stop=True, R=[rtT, Qb], W=[pya])
        P.S("copy", out=W1a[:], in_=pya[:])
        pyb = nxt()
        for h in range(NH):
            for d in range(2):
                r0, c0 = 64 * d, 64 * h
                o = pyb[r0:r0 + 64, c0:c0 + 64]
                P.T("matmul", out=o, lhsT=BrkT_s[r0:r0 + 64, c0:c0 + 64], rhs=vb[r0:r0 + 64, c0:c0 + 64], start=True, stop=False,
                    R=[BrkT_s, vb], W=[pyb])
                P.T("matmul", out=o, lhsT=BrbT_s[r0:r0 + 64, c0:c0 + 64], rhs=negU[r0:r0 + 64, c0:c0 + 64], start=False, stop=True,
                    R=[BrbT_s, negU], W=[pyb])
        P.V("tensor_tensor", out=y_sb[:], in0=pyb[:], in1=W1a[:], op=ALU.add)
        P.dma("sync", yf_scr[64 * m0:64 * m0 + 64, :], y_sb[0:64, :])
        P.dma("sync", yr_scr[64 * m1:64 * m1 + 64, :], y_sb[64:128, :])
        if RW_CUT == 10:
            continue
        for d in range(2):
            pq = nxt()
            for h in range(NH):
                r0, c0 = 64 * d, 64 * h
                o = pq[0:64, c0:c0 + 64]
                P.T("matmul", out=o, lhsT=kt_b[r0:r0 + 64, c0:c0 + 64], rhs=vb[r0:r0 + 64, c0:c0 + 64], start=True, stop=False,
                    R=[kt_b, vb], W=[pq])
                P.T("matmul", out=o, lhsT=bt_b[r0:r0 + 64, c0:c0 + 64], rhs=negU[r0:r0 + 64, c0:c0 + 64], start=False, stop=True,
                    R=[bt_b, negU], W=[pq])
            P.V("tensor_tensor", out=tmpq[:, 512 * d:512 * d + 512], in0=pq[0:64, :], in1=Qf[:, 512 * d:512 * d + 512],
                op=ALU.add, W=[(tmpq, d)], R=[pq, Qf])
        P.V("tensor_tensor", out=Qf[:].rearrange("p (d h i) -> p d h i", d=2, h=NH), in0=tmpq[:].rearrange("p (d h i) -> p d h i", d=2, h=NH),
            in1=cC[:].rearrange("p (h d) -> p d h", d=2).unsqueeze(3).to_broadcast([64, 2, NH, 64]), op=ALU.mult)
        P.S("copy", out=Qb[:], in_=Qf[:])


def emit_globals(P, G, ident_d, identf_d):
    G.identB = P.sb("g_identB", [128, 128], BF16)
    G.identF = P.sb("g_identF", [128, 128], F32)
    G.epsb = P.sb("g_epsb", [128, 1], F32)
    P.dma("sync", G.identB[:], ident_d[:, :])
    P.dma("sync", G.identF[:], identf_d[:, :])
    P.G("memset", ap=G.epsb[:], constant=1e-6, R=[], W=[G.epsb])
    G.oneb = P.sb("g_oneb", [128, 1], F32)
    P.G("memset", ap=G.oneb[:], constant=1.0, R=[], W=[G.oneb])


def build_main(upto="all", dbg=(), rw_steps=132, n_exp=16, n_moe_layers=2, fuse_adaln=False):
    P = Prog(same_engine_sync=SAME_ENGINE_SYNC)
    G = Ctx()
    scr = lambda name, shape, dt=F32: P.dram(name, shape, dt, "out" if name in dbg else "tmp")
    xin_d = P.dram("xin", [T_TOK, 1024], F32, "in")
    if fuse_adaln:
        mod_d = scr("mods_scr", [2, 2, 6, 128, 1024])
        cT_d = P.dram("cT2", [128, 8, 2], F32, "in")
        G.selr_d = P.dram("selr", [2, 2, 128], F32, "in")
        adaw_d = P.dram("adaw", [2, 128, 8, 6144], F32, "in")
        adab_d = P.dram("adab", [2, 128, 6144], F32, "in")
        ng_d = P.dram("ng", [2, 128, 2, 1024], F32, "in")
    else:
        mod_d = P.dram("mods", [2, 2, 6, 128, 1024], F32, "in")
    ident_d = P.dram("identb", [128, 128], BF16, "in")
    identf_d = P.dram("identf", [128, 128], F32, "in")
    ew_in_d = P.dram("ev_w_in", [128, 8, 3328], F32, "in")
    rwc_d = P.dram("rwc", [128, 8, 512], F32, "in")
    wup_d = P.dram("wup", [64, 2, 2, 512], F32, "in")
    cumM_d = P.dram("cumM", [128, 128], F32, "in")
    sel_d = P.dram("sel", [128, 2], F32, "in")
    p_scr = scr("p_scr", [T_TOK, 3328])
    yf_scr = scr("yf_scr", [T_TOK, 512])
    yr_scr = scr("yr_scr", [T_TOK, 512])
    emit_globals(P, G, ident_d, identf_d)
    if fuse_adaln:
        with P.scope():
            stage_adaln(P, G, cT_d, adaw_d, adab_d, ng_d, mod_d)
    with P.scope():
        stage_pre(P, G, 3328, ew_in_d, mod_d, 0, xin_d, p_scr)
    if upto == "pre":
        return P.build(), P
    with P.scope():
        stage_rwkv(P, G, p_scr, yf_scr, yr_scr, rwc_d, wup_d, cumM_d, sel_d, nsteps=rw_steps)
    if upto == "rwkv":
        return P.build(), P
    evc_d = P.dram("evc", [128, 6, 512], F32, "in")
    gup_d = P.dram("g_up", [128, 512], F32, "in")
    ewout_d = P.dram("ev_w_out", [128, 8, 1024], F32, "in")
    router_d = P.dram("router", [2, 128, 8, 16], F32, "in")
    xmid_scr = scr("xmid_scr", [T_TOK, 1024])
    h2_scr = scr("h2_scr", [T_TOK, 1024], BF16)
    aff_scr = scr("aff_scr", [T_TOK, 16])
    with P.scope():
        stage_post(P, G, 0, "even", xin_d, p_scr, 3328, yf_scr, yr_scr, None, evc_d, gup_d, ewout_d, router_d[0], mod_d,
                   xmid_scr, h2_scr, aff_scr, list(range(NTILE)))
    if upto == "post0":
        return P.build(), P
    tric_d = P.dram("tric", [128, 2, 128], F32, "in")
    moe_w = [[P.dram(f"moe_{n}_{l}", [16, 128, k8, nn], F32, "in") for (n, k8, nn) in (("w1", 8, 1536), ("w3", 8, 1536), ("w2", 12, 1024))]
             for l in range(n_moe_layers)]
    xl_scr = [scr(f"xl_scr{e}", [1025, 1024], BF16) for e in range(16)]
    xc_scr = [scr(f"xc_scr{e}", [33, 1024], BF16) for e in range(16)]
    yl_scr = [scr(f"yl_scr{e}", [1025, 1024]) for e in range(16)]
    yc_scr = [scr(f"yc_scr{e}", [33, 1024]) for e in range(16)]
    x1_scr = scr("x1_scr", [T_TOK, 1024])
    with P.scope():
        stage_moe(P, G, 0, moe_w[0][0], moe_w[0][1], moe_w[0][2], mod_d, tric_d, xmid_scr, h2_scr, aff_scr, xl_scr, xc_scr, yl_scr, yc_scr,
                  x1_scr, None, True, n_exp=n_exp)
    if upto == "moe0":
        return P.build(), P
    ow_in_d = P.dram("od_w_in", [128, 8, ODD_N], F32, "in")
    owout_d = P.dram("od_w_out", [128, 8, 1024], F32, "in")
    glc_d = P.dram("glc", [128, 2, 256], F32, "in")
    aup_d = P.dram("aup", [16, 2, 256], F32, "in")
    rope_d = P.dram("rope", [T_TOK, 2, 64], F32, "in")
    qkg_d = P.dram("qkg", [128, 2, 512], F32, "in")
    natb_d = P.dram("natb", [8, 128, 8, 4, 64], F32, "in")
    glng_d = P.dram("glng", [128, 1, 512], F32, "in")
    out_d = P.dram("out", [8192, 1024], F32, "out")
    p1_scr = scr("p1_scr", [T_TOK, ODD_N])
    of_scr = scr("of_scr", [T_TOK, 512])
    or_scr = scr("or_scr", [T_TOK, 512])
    nat_scr = scr("nat_scr", [T_TOK, 512])
    qT_scr = scr("qT_scr", [8, 64, T_TOK], BF16)
    kT_scr = scr("kT_scr", [8, 64, T_TOK], BF16)
    va_scr = scr("va_scr", [T_TOK, 8, 65], BF16)
    xmid1_scr = scr("xmid1_scr", [T_TOK, 1024])
    with P.scope():
        stage_pre(P, G, ODD_N, ow_in_d, mod_d, 1, x1_scr, p1_scr)
    with P.scope():
        if "gla" in NO_REORDER:
            P.mark_no_reorder()
        stage_gla(P, G, p1_scr, of_scr, or_scr, glc_d, aup_d, rope_d, cumM_d, sel_d, nsteps=rw_steps)
    if upto == "gla":
        return P.build(), P
    with P.scope():
        stage_nat(P, G, p1_scr, nat_scr, qkg_d, natb_d, qT_scr, kT_scr, va_scr)
    if upto == "nat":
        return P.build(), P
    lat = list(range(2, NTILE))
    with P.scope():
        stage_post(P, G, 1, "odd", x1_scr, p1_scr, ODD_N, of_scr, or_scr, nat_scr, glng_d, None, owout_d, router_d[1], mod_d,
                   xmid1_scr, h2_scr, aff_scr, lat)
    if upto == "post1":
        return P.build(), P
    with P.scope():
        stage_moe(P, G, 1, moe_w[1][0], moe_w[1][1], moe_w[1][2], mod_d, tric_d, xmid1_scr, h2_scr, aff_scr, xl_scr, xc_scr, yl_scr, yc_scr,
                  out_d, 256, False, n_exp=n_exp)
    return P.build(), P


def mods_bcast(mods, b):
    m = np.stack([mods[:, b], mods[:, 4]], 1)
    return np.ascontiguousarray(np.broadcast_to(m[:, :, :, None, :], (2, 2, 6, 128, 1024)))


def main_inputs(inp, mods, b, n_moe_layers=2):
    d = dict(xin=np.ascontiguousarray(np.concatenate([inp["ctx"][b], inp["x"][b]], 0)),
             identb=IDENT_BF, identf=IDENT_F, ev_w_in=kmajor(inp["ev_w_in"][0]))
    if mods is not None:
        d["mods"] = mods_bcast(mods, b)
    else:
        c2 = np.stack([inp["c"][b], inp["c_ctx"]], 1)
        d["cT2"] = kmajor(np.ascontiguousarray(c2))
        d["selr"] = np.ascontiguousarray(np.broadcast_to(np.eye(2, dtype=np.float32)[:, :, None], (2, 2, 128)))
        d["adaw"] = np.stack([kmajor(inp["ada_w"][l]) for l in range(2)], 0)
        d["adab"] = np.stack([bc128(inp["ada_b"][l]) for l in range(2)], 0)
        d["ng"] = np.stack([bc128(np.stack([inp["norm1_g"][l], inp["norm2_g"][l]], 0)) for l in range(2)], 0)
    d.update(rwkv_consts(inp))
    d.update(post_consts_even(inp))
    d.update(odd_consts(inp))
    d["router"] = np.stack([kmajor(inp["moe_router"][l]) for l in range(2)], 0)
    tri = np.zeros((128, 2, 128), np.float32)
    tri[:, 0, :] = 1.0
    tri[:, 1, :] = (np.arange(128)[:, None] < np.arange(128)[None, :])
    d["tric"] = tri
    for l in range(n_moe_layers):
        d[f"moe_w1_{l}"] = np.ascontiguousarray(inp["moe_w1"][l].reshape(16, 8, 128, 1536).transpose(0, 2, 1, 3))
        d[f"moe_w3_{l}"] = np.ascontiguousarray(inp["moe_w3"][l].reshape(16, 8, 128, 1536).transpose(0, 2, 1, 3))
        d[f"moe_w2_{l}"] = np.ascontiguousarray(inp["moe_w2"][l].reshape(16, 12, 128, 1024).transpose(0, 2, 1, 3))
    return d


IDENT_BF = np.eye(128, dtype=np.float32).astype(BF)
IDENT_F = np.eye(128, dtype=np.float32)


def post_consts_even(inp):
    ev = np.stack([bc128(inp["conv_w"][0][0]), bc128(inp["conv_w"][0][1]), bc128(inp["conv_w"][0][2]),
                   bc128(inp["rw_r_k"][0].reshape(512)), bc128(inp["rw_ln_g"][0]), bc128(inp["rw_ln_b"][0])], 1)
    return dict(evc=np.ascontiguousarray(ev.astype(np.float32)), g_up=np.ascontiguousarray(inp["rw_g_up"][0]),
                ev_w_out=kmajor(inp["ev_w_out"][0]))


def stage_post(P, G, lyr, kind, x_src, p_scr, N, ya_scr, yb_scr, nat_scr, cst_d, gup_d, wout_d, router_d, mod_d,
               xmid_scr, h2_scr, aff_scr, tiles):
    pre = f"po{lyr}_"
    f32t = lambda n, shp=(128, 512): P.sb(pre + n, list(shp), F32)
    bft = lambda n, shp=(128, 512): P.sb(pre + n, list(shp), BF16)
    modS = f32t("modS", (128, 3, 1024))
    modC = f32t("modC", (128, 3, 1024))
    for dst, si in ((modS, 0), (modC, 1)):
        P.dma("sync", dst[:], mod_d[lyr, si, 2:5].rearrange("v p f -> p v f"))
    wout = bft("wout", (128, 8, 1024))
    stg = emit_weight_bf16(P, wout_d, wout, 8, 1024, pre + "w")
    router = f32t("router", (128, 8, 16))
    P.dma("sync", router[:], router_d[:, :, :])
    ncst = 6 if kind == "even" else 1
    cst = f32t("cst", (128, ncst, 512))
    P.dma("sync", cst[:], cst_d[:, :, :])
    if kind == "even":
        gup_f = f32t("gup_f", (128, 512))
        gup = bft("gup", (128, 512))
        P.dma("sync", gup_f[:], gup_d[:, :])
        P.V("tensor_copy", out=gup[:], in_=gup_f[:])
        cw0, cw1, cw2, rkb, lng, lnb = [cst[:, i, :] for i in range(6)]
    pt_ = [f32t(f"pt{i}", (128, N)) for i in range(2)]
    xts = [f32t(f"xt{i}", (128, 1024)) for i in range(2)]
    yas = [f32t(f"ya{i}") for i in range(2)]
    ybs = [f32t(f"yb{i}") for i in range(2)]
    if kind == "even":
        pu_, pg_, nu_, ng_ = f32t("pu"), f32t("pg"), f32t("nu"), f32t("ng")
    else:
        natt = [f32t(f"nat{i}") for i in range(2)]
    TS = [dict(a1=f32t(f"a1{i}"), a2=f32t(f"a2{i}"), a3=f32t(f"a3{i}"), a4=f32t(f"a4{i}"), s8=f32t(f"s8{i}", (128, 8)),
               m8=f32t(f"m8{i}", (128, 8)), v8=f32t(f"v8{i}", (128, 8)), sgx=bft(f"sgx{i}", (128, 128)), sgT=bft(f"sgT{i}", (128, 128)),
               cat=bft(f"cat{i}", (128, 1024)), catT=bft(f"catT{i}", (128, 8, 128)), xmid=f32t(f"xmid{i}", (128, 1024)),
               h2f=f32t(f"h2f{i}", (128, 1024)), h2b=bft(f"h2b{i}", (128, 1024)), h2T=f32t(f"h2T{i}", (128, 8, 128)),
               ss=f32t(f"ss{i}", (128, 1)), rs=f32t(f"rs{i}", (128, 1)), mx=f32t(f"mx{i}", (128, 1)), se=f32t(f"se{i}", (128, 1)),
               ex=f32t(f"ex{i}", (128, 16)), aff=f32t(f"aff{i}", (128, 16))) for i in range(2)]
    lneps = f32t("lneps", (128, 1))
    P.G("memset", ap=lneps[:], constant=(64e-5 if kind == "even" else 1e-6), R=[], W=[lneps])
    tmp, hf = f32t("tmp", (128, 1024)), f32t("hf", (128, 1024))
    pTb = P.ps(pre + "pTb", [128, 8, 128], BF16)
    pTf = [P.ps(pre + f"pTf{i}", [128, 4, 128]) for i in range(2)]
    pms = [P.ps(pre + f"pm{i}", [128, 512]) for i in range(3)]
    pl = P.ps(pre + "pl", [128, 16])
    pgT = P.ps(pre + "pgT", [128, 128], BF16)
    v3 = lambda ap, h: ap.rearrange("p (h j) -> p h j", h=h)
    for it, t in enumerate(tiles):
        r0 = t * 128
        first = t in (0, 2)
        last = t in (1, NTILE - 1)
        pt, xt, ya, yb = pt_[it % 2], xts[it % 2], yas[it % 2], ybs[it % 2]
        mod = modC if t < 2 else modS
        ts_ = TS[it % 2]
        a1, a2, a3, a4, s8, m8, v8, sgx, sgT, cat, catT, xmid = [ts_[k] for k in ("a1", "a2", "a3", "a4", "s8", "m8", "v8", "sgx", "sgT", "cat", "catT", "xmid")]
        h2f, h2b, h2T, ss, rs, mx, se, ex, aff = [ts_[k] for k in ("h2f", "h2b", "h2T", "ss", "rs", "mx", "se", "ex", "aff")]
        P.dma("sync", pt[:], p_scr[r0:r0 + 128, :])
        P.dma("sync", xt[:], x_src[r0:r0 + 128, :])
        P.dma("sync", ya[:], ya_scr[r0:r0 + 128, :])
        P.dma("sync", yb[:], yb_scr[r0:r0 + 128, :])
        if kind == "even":
            if first:
                P.V("memset", ap=pu_[:], constant=0.0, R=[], W=[pu_])
                P.V("memset", ap=pg_[:], constant=0.0, R=[], W=[pg_])
                P.dma("sync", pu_[1:128, :], p_scr[r0:r0 + 127, 0:512])
                P.dma("sync", pg_[1:128, :], p_scr[r0:r0 + 127, 1024:1536])
            else:
                P.dma("sync", pu_[:], p_scr[r0 - 1:r0 + 127, 0:512])
                P.dma("sync", pg_[:], p_scr[r0 - 1:r0 + 127, 1024:1536])
            if last:
                P.V("memset", ap=nu_[:], constant=0.0, R=[], W=[nu_])
                P.V("memset", ap=ng_[:], constant=0.0, R=[], W=[ng_])
                P.dma("sync", nu_[0:127, :], p_scr[r0 + 1:r0 + 128, 0:512])
                P.dma("sync", ng_[0:127, :], p_scr[r0 + 1:r0 + 128, 1024:1536])
            else:
                P.dma("sync", nu_[:], p_scr[r0 + 1:r0 + 129, 0:512])
                P.dma("sync", ng_[:], p_scr[r0 + 1:r0 + 129, 1024:1536])
            P.G("tensor_tensor", out=a1[:], in0=pu_[:], in1=pg_[:], op=ALU.mult)
            P.G("tensor_tensor", out=a1[:], in0=a1[:], in1=cw0, op=ALU.mult)
            P.V("tensor_tensor", out=a2[:], in0=pt[:, 0:512], in1=pt[:, 1024:1536], op=ALU.mult)
            P.V("tensor_tensor", out=a2[:], in0=a2[:], in1=cw1, op=ALU.mult)
            P.G("tensor_tensor", out=a3[:], in0=nu_[:], in1=ng_[:], op=ALU.mult)
            P.G("tensor_tensor", out=a3[:], in0=a3[:], in1=cw2, op=ALU.mult)
            P.V("tensor_tensor", out=a2[:], in0=a2[:], in1=a1[:], op=ALU.add)
            P.V("tensor_tensor", out=a2[:], in0=a2[:], in1=a3[:], op=ALU.add)
            P.V("tensor_tensor", out=cat[:, 0:512], in0=a2[:], in1=pt[:, 512:1024], op=ALU.mult, W=[(cat, 0)])
            r_, k_, v_, xg_ = pt[:, 1536:2048], pt[:, 2048:2560], pt[:, 2560:3072], pt[:, 3200:3328]
            P.V("tensor_tensor", out=a1[:], in0=ya[:], in1=yb[:], op=ALU.add)
            P.V("tensor_reduce", out=s8[:], in_=v3(a1[:], 8), axis=AX.X, op=ALU.add)
            P.V("tensor_scalar", out=m8[:], in0=s8[:], scalar1=-1.0 / 64, scalar2=None, op0=ALU.mult)
            P.V("tensor_tensor", out=v3(a2[:], 8), in0=v3(a1[:], 8), in1=m8[:].unsqueeze(2).to_broadcast([128, 8, 64]), op=ALU.add)
            P.G("tensor_tensor", out=a3[:], in0=a2[:], in1=a2[:], op=ALU.mult)
            P.V("tensor_reduce", out=v8[:], in_=v3(a3[:], 8), axis=AX.X, op=ALU.add)
            P.S("activation", out=v8[:], in_=v8[:], func=AF.Sqrt, scale=1.0 / 64, bias=lneps[:, 0:1])
            P.V("reciprocal", out=v8[:], in_=v8[:])
            P.V("tensor_tensor", out=v3(a2[:], 8), in0=v3(a2[:], 8), in1=v8[:].unsqueeze(2).to_broadcast([128, 8, 64]), op=ALU.mult)
            P.G("tensor_tensor", out=a2[:], in0=a2[:], in1=lng, op=ALU.mult)
            P.G("tensor_tensor", out=a2[:], in0=a2[:], in1=lnb, op=ALU.add)
            P.V("tensor_tensor", out=a4[:], in0=r_, in1=k_, op=ALU.mult)
            P.V("tensor_tensor", out=a4[:], in0=a4[:], in1=rkb, op=ALU.mult)
            P.V("tensor_reduce", out=s8[:], in_=v3(a4[:], 8), axis=AX.X, op=ALU.add)
            P.V("tensor_tensor", out=v3(a4[:], 8), in0=v3(v_, 8), in1=s8[:].unsqueeze(2).to_broadcast([128, 8, 64]), op=ALU.mult)
            P.V("tensor_tensor", out=a2[:], in0=a2[:], in1=a4[:], op=ALU.add)
            P.S("activation", out=sgx[:], in_=xg_, func=AF.Sigmoid)
            P.T("transpose", out=pgT[:], in_=sgx[:], identity=G.identB[:])
            P.S("copy", out=sgT[:], in_=pgT[:])
            pm = pms[2]
            P.T("matmul", out=pm[:], lhsT=sgT[:], rhs=gup[:], start=True, stop=True)
            P.V("tensor_tensor", out=cat[:, 512:1024], in0=a2[:], in1=pm[:], op=ALU.mult, W=[(cat, 1)])
        else:
            nt = natt[it % 2]
            P.dma("sync", nt[:], nat_scr[r0:r0 + 128, :])
            P.S("copy", out=cat[:, 0:512], in_=nt[:], W=[(cat, 0)])
            gr_ = pt[:, 2560:3072]
            P.V("tensor_tensor", out=a1[:], in0=ya[:], in1=yb[:], op=ALU.add)
            P.G("tensor_tensor", out=a3[:], in0=a1[:], in1=a1[:], op=ALU.mult)
            P.V("tensor_reduce", out=v8[:, 0:4], in_=v3(a3[:], 4), axis=AX.X, op=ALU.add)
            P.S("activation", out=v8[:, 0:4], in_=v8[:, 0:4], func=AF.Sqrt, scale=1.0 / 128, bias=lneps[:, 0:1])
            P.V("reciprocal", out=v8[:, 0:4], in_=v8[:, 0:4])
            P.V("tensor_tensor", out=v3(a2[:], 4), in0=v3(a1[:], 4), in1=v8[:, 0:4].unsqueeze(2).to_broadcast([128, 4, 128]), op=ALU.mult)
            P.G("tensor_tensor", out=a2[:], in0=a2[:], in1=cst[:, 0, :], op=ALU.mult)
            P.S("activation", out=a4[:], in_=gr_, func=AF.Silu)
            P.V("tensor_tensor", out=cat[:, 512:1024], in0=a2[:], in1=a4[:], op=ALU.mult, W=[(cat, 1)])
        for k in range(8):
            P.T("transpose", out=pTb[:, k, :], in_=cat[:, k * 128:(k + 1) * 128], identity=G.identB[:], R=[cat])
        P.S("copy", out=catT[:], in_=pTb[:])
        for nb in range(2):
            pm = pms[nb]
            for k in range(8):
                P.T("matmul", out=pm[:], lhsT=catT[:, k, :], rhs=wout[:, k, nb * 512:(nb + 1) * 512], start=(k == 0), stop=(k == 7),
                    R=[catT, (wout, k)])
            P.V("tensor_tensor", out=xmid[:, nb * 512:(nb + 1) * 512], in0=pm[:], in1=mod[:, 0, nb * 512:(nb + 1) * 512], op=ALU.mult,
                W=[(xmid, nb)])
        P.G("tensor_tensor", out=xmid[:], in0=xmid[:], in1=xt[:], op=ALU.add)
        P.dma("sync", xmid_scr[r0:r0 + 128, :], xmid[:])
        emit_norm_mod(P, xmid, mod[:, 2, :], mod[:, 1, :], h2b, tmp, ss, rs, G.epsb, hf, hf_out=h2f)
        P.dma("sync", h2_scr[r0:r0 + 128, :], h2b[:])
        for k in range(8):
            P.T("transpose", out=pTf[k // 4][:, k % 4, :], in_=h2f[:, k * 128:(k + 1) * 128], identity=G.identF[:])
        P.S("copy", out=h2T[:, 0:4, :], in_=pTf[0][:], W=[(h2T, 0)])
        P.V("tensor_copy", out=h2T[:, 4:8, :], in_=pTf[1][:], W=[(h2T, 1)])
        for k in range(8):
            P.T("matmul", out=pl[:], lhsT=h2T[:, k, :], rhs=router[:, k, :], start=(k == 0), stop=(k == 7), R=[h2T, router])
        P.V("tensor_reduce", out=mx[:], in_=pl[:], axis=AX.X, op=ALU.max)
        P.V("tensor_scalar", out=mx[:], in0=mx[:], scalar1=-1.0, scalar2=None, op0=ALU.mult)
        P.S("activation", out=ex[:], in_=pl[:], func=AF.Exp, bias=mx[:, 0:1], accum_out=se[:])
        P.V("reciprocal", out=se[:], in_=se[:])
        P.V("tensor_scalar", out=aff[:], in0=ex[:], scalar1=se[:, 0:1], scalar2=None, op0=ALU.mult)
        P.dma("sync", aff_scr[r0:r0 + 128, :], aff[:])


def stage_moe(P, G, lyr, w1_d, w3_d, w2_d, mod_d, tric_d, xmid_scr, h2_scr, aff_scr, xl_scr, xc_scr, yl_scr, yc_scr,
              out_ap, out_row0, with_ctx, n_exp=16, n_iter=30):
    pre = f"mo{lyr}_"
    f32t = lambda n, shp: P.sb(pre + n, list(shp), F32)
    bft = lambda n, shp: P.sb(pre + n, list(shp), BF16)
    NE = 16
    NU = 32
    posIL = P.sb(pre + "posIL", [128, 16, 64], I32)
    posIC = P.sb(pre + "posIC", [128, 16, 2], I32)
    gmL = f32t("gmL", (128, 16, 64))
    gmC = f32t("gmC", (128, 16, 2))
    with P.scope():
        tric = f32t("tric", (128, 2, 128))
        P.dma("sync", tric[:], tric_d[:, :, :])
        onesF, triS = tric[:, 0, :], tric[:, 1, :]
        affL = f32t("affL", (128, 64, 16))
        affC = f32t("affC", (128, 2, 16))
        for q in range(4):
            P.dma("sync", affL[:, 16 * q:16 * q + 16, :],
                  aff_scr[256 + 2048 * q:256 + 2048 * (q + 1), :].rearrange("(n p) e -> p n e", p=128), W=[(affL, q)])
        P.dma("sync", affC[:], aff_scr[0:256, :].rearrange("(n p) e -> p n e", p=128))
        AL = f32t("AL", (128, 16, 64))
        AC = f32t("AC", (128, 16, 2))
        P.V("tensor_copy", out=AL[:], in_=affL[:].rearrange("p n e -> p e n"))
        P.V("tensor_copy", out=AC[:], in_=affC[:].rearrange("p n e -> p e n"))
        cmpL = f32t("cmpL", (128, 16, 64))
        cmpC = f32t("cmpC", (128, 16, 2))
        lo, hi, mid, cnt, ge, dl, kt = [f32t(n, (128, NU)) for n in ("lo", "hi", "mid", "cnt", "ge", "dl", "kt")]
        P.V("memset", ap=lo[:], constant=0.0, R=[], W=[lo])
        P.V("memset", ap=hi[:], constant=1.0, R=[], W=[hi])
        P.V("memset", ap=kt[:, 0:16], constant=1024.0, R=[], W=[(kt, 0)])
        P.V("memset", ap=kt[:, 16:32], constant=32.0, R=[], W=[(kt, 1)])
        ptot = P.ps(pre + "ptot", [128, NU])
        for it in range(n_iter):
            P.V("tensor_tensor", out=dl[:], in0=hi[:], in1=lo[:], op=ALU.subtract)
            P.V("scalar_tensor_tensor", out=mid[:], in0=dl[:], scalar=0.5, in1=lo[:], op0=ALU.mult, op1=ALU.add)
            P.V("tensor_tensor", out=cmpL[:], in0=AL[:], in1=mid[:, 0:16].unsqueeze(2).to_broadcast([128, 16, 64]), op=ALU.is_ge)
            P.V("tensor_reduce", out=cnt[:, 0:16], in_=cmpL[:], axis=AX.X, op=ALU.add, W=[(cnt, 0)])
            P.V("tensor_tensor", out=cmpC[:], in0=AC[:], in1=mid[:, 16:32].unsqueeze(2).to_broadcast([128, 16, 2]), op=ALU.is_ge)
            P.V("tensor_reduce", out=cnt[:, 16:32], in_=cmpC[:], axis=AX.X, op=ALU.add, W=[(cnt, 1)])
            P.T("matmul", out=ptot[:], lhsT=onesF, rhs=cnt[:], start=True, stop=True, R=[tric, cnt])
            P.V("tensor_tensor", out=ge[:], in0=ptot[:], in1=kt[:], op=ALU.is_ge)
            P.V("tensor_tensor", out=dl[:], in0=mid[:], in1=lo[:], op=ALU.subtract)
            P.V("tensor_tensor", out=dl[:], in0=dl[:], in1=ge[:], op=ALU.mult)
            P.V("tensor_tensor", out=lo[:], in0=lo[:], in1=dl[:], op=ALU.add)
            P.V("tensor_tensor", out=dl[:], in0=hi[:], in1=mid[:], op=ALU.subtract)
            P.V("tensor_tensor", out=dl[:], in0=dl[:], in1=ge[:], op=ALU.mult)
            P.V("tensor_tensor", out=hi[:], in0=mid[:], in1=dl[:], op=ALU.add)
        ones64 = f32t("ones64", (128, 64))
        P.V("memset", ap=ones64[:], constant=1.0, R=[], W=[ones64])
        pp = [P.ps(pre + f"pp{i}", [128, 512]) for i in range(2)]
        for (A, cm, th, ncol, posI, gm, sfx, KK) in ((AL, cmpL, lo[:, 0:16], 64, posIL, gmL, "L", 1024.0), (AC, cmpC, lo[:, 16:32], 2, posIC, gmC, "C", 32.0)):
            W = 16 * ncol
            ppT, ctT, csT = [f32t(n + sfx, (128, 16, ncol)) for n in ("ppT", "ctT", "csT")]
            flat = lambda tl: tl[:].rearrange("p e n -> p (e n)")
            P.V("tensor_tensor", out=cm[:], in0=A[:], in1=th.unsqueeze(2).to_broadcast([128, 16, ncol]), op=ALU.is_ge, R=[A, lo])
            P.V("tensor_tensor", out=gm[:], in0=A[:], in1=cm[:], op=ALU.mult)
            for (lhs, dst) in ((triS, ppT), (onesF, ctT)):
                for c0 in range(0, W, 512):
                    c1 = min(W, c0 + 512)
                    pq = pp[(c0 // 512) % 2]
                    P.T("matmul", out=pq[:, 0:c1 - c0], lhsT=lhs, rhs=flat(cm)[:, c0:c1], start=True, stop=True, R=[tric, cm])
                    P.S("copy", out=flat(dst)[:, c0:c1], in_=pq[:, 0:c1 - c0], W=[(dst, c0)])
            for e in range(16):
                P.V("tensor_tensor_scan", out=csT[:, e, :], data0=ones64[:, 0:ncol], data1=ctT[:, e, :], initial=0.0,
                    op0=ALU.mult, op1=ALU.add, R=[ctT, ones64], W=[(csT, e)])
            P.V("tensor_tensor", out=ppT[:], in0=ppT[:], in1=csT[:], op=ALU.add)
            P.V("tensor_tensor", out=ppT[:], in0=ppT[:], in1=ctT[:], op=ALU.subtract)
            P.V("scalar_tensor_tensor", out=ppT[:], in0=ppT[:], scalar=-KK, in1=cm[:], op0=ALU.add, op1=ALU.mult)
            P.V("tensor_scalar", out=ppT[:], in0=ppT[:], scalar1=KK, scalar2=KK, op0=ALU.add, op1=ALU.min)
            P.V("tensor_copy", out=posI[:], in_=ppT[:])
    lat_tiles = list(range(2, NTILE))
    ctx_tiles = [0, 1] if with_ctx else []
    with P.scope():
        h2ts = [bft(f"h2t{i}", (128, 1024)) for i in range(3)]
        for i, t in enumerate(ctx_tiles + lat_tiles):
            h2t = h2ts[i % 3]
            P.dma("sync", h2t[:], h2_scr[t * 128:(t + 1) * 128, :])
            for e in range(n_exp):
                if t < 2:
                    P.idma(R=[h2t, posIC], W=[xc_scr[e]], out=xc_scr[e][:, :], out_offset=bass.IndirectOffsetOnAxis(ap=posIC[:, e, t:t + 1], axis=0),
                           in_=h2t[:, :], in_offset=None)
                else:
                    P.idma(R=[h2t, posIL], W=[xl_scr[e]], out=xl_scr[e][:, :], out_offset=bass.IndirectOffsetOnAxis(ap=posIL[:, e, t - 2:t - 1], axis=0),
                           in_=h2t[:, :], in_offset=None)
    with P.scope():
        w1b, w3b = bft("w1b", (128, 8, 1536)), bft("w3b", (128, 8, 1536))
        w2b = bft("w2b", (128, 12, 1024))
        stg = [f32t(f"stg{i}", (128, 1536)) for i in range(4)]
        xins = [bft(f"xin{i}", (128, 1024)) for i in range(2)]
        NR = 1024 + (32 if with_ctx else 0)
        xT = bft("xT", (128, 8, NR))
        hidT = bft("hidT", (128, 12, NR))
        sl = [f32t(f"sl{i}", (128, 512)) for i in range(2)]
        yts = [f32t(f"yt{i}", (128, 1024)) for i in range(2)]
        pT = P.ps(pre + "pT", [128, 8, 128], BF16)
        ph = [P.ps(pre + f"ph{i}", [128, 512]) for i in range(4)]
        cast_i = [0]
        ev = 0

        def load_w(e, which):
            for (wd, wb, K8, N) in which:
                for k in range(K8):
                    s_ = stg[cast_i[0] % 4]
                    cast_i[0] += 1
                    P.dma("sync", s_[:, 0:N], wd[e, :, k, :])
                    P.G("tensor_copy", out=wb[:, k, :], in_=s_[:, 0:N], W=[(wb, k)])

        W13 = ((w1_d, w1b, 8, 1536), (w3_d, w3b, 8, 1536))
        W2 = ((w2_d, w2b, 12, 1024),)
        load_w(0, W13)
        blocks = [(xl_scr, yl_scr, b0 * 512, 512, b0 * 512) for b0 in range(2)] + ([(xc_scr, yc_scr, 0, 32, 1024)] if with_ctx else [])
        for e in range(n_exp):
            load_w(e, W2)
            ti = 0
            for (xs, ys, row0, nrow, col0) in blocks:
                for ci in range((nrow + 127) // 128):
                    rows = min(128, nrow - ci * 128)
                    xin = xins[ti % 2]
                    ti += 1
                    P.dma("sync", xin[0:rows, :], xs[e][row0 + ci * 128:row0 + ci * 128 + rows, :])
                    for k in range(8):
                        P.T("transpose", out=pT[:, k, 0:rows], in_=xin[0:rows, k * 128:(k + 1) * 128], identity=G.identB[0:rows, 0:rows])
                    c_ = col0 + ci * 128
                    P.S("copy", out=xT[:, :, c_:c_ + rows], in_=pT[:, :, 0:rows], W=[(xT, c_)])
            for (xs, ys, row0, nrow, col0) in blocks:
                for fc in range(12):
                    p1, p3 = ph[(2 * fc) % 4], ph[(2 * fc + 1) % 4]
                    for (pp_, wb) in ((p1, w1b), (p3, w3b)):
                        for k in range(8):
                            P.T("matmul", out=pp_[:, 0:nrow], lhsT=wb[:, k, fc * 128:(fc + 1) * 128], rhs=xT[:, k, col0:col0 + nrow],
                                start=(k == 0), stop=(k == 7), R=[xT, (wb, k)])
                    s_ = sl[fc % 2]
                    P.S("activation", out=s_[:, 0:nrow], in_=p1[:, 0:nrow], func=AF.Silu)
                    P.V("tensor_tensor", out=hidT[:, fc, col0:col0 + nrow], in0=s_[:, 0:nrow], in1=p3[:, 0:nrow], op=ALU.mult,
                        W=[(hidT, (fc, col0))])
            if e + 1 < n_exp:
                load_w(e + 1, W13)
            for (xs, ys, row0, nrow, col0) in blocks:
                for ci in range((nrow + 127) // 128):
                    rows = min(128, nrow - ci * 128)
                    yt = yts[ci % 2]
                    c_ = col0 + ci * 128
                    for db in range(2):
                        py = ph[ev % 4]
                        for fc in range(12):
                            P.T("matmul", out=py[0:rows, :], lhsT=hidT[:, fc, c_:c_ + rows], rhs=w2b[:, fc, db * 512:(db + 1) * 512],
                                start=(fc == 0), stop=(fc == 11), R=[hidT, (w2b, fc)])
                        if ev % 2 == 0:
                            P.S("copy", out=yt[0:rows, db * 512:(db + 1) * 512], in_=py[0:rows, :], W=[(yt, db)])
                        else:
                            P.V("tensor_copy", out=yt[0:rows, db * 512:(db + 1) * 512], in_=py[0:rows, :], W=[(yt, db)])
                        ev += 1
                    P.dma("sync", ys[e][row0 + ci * 128:row0 + ci * 128 + rows, :], yt[0:rows, :])
    with P.scope():
        gt2S = f32t("gt2S", (128, 1024))
        gt2C = f32t("gt2C", (128, 1024))
        P.dma("sync", gt2S[:], mod_d[lyr, 0, 5])
        P.dma("sync", gt2C[:], mod_d[lyr, 1, 5])
        ygs = [f32t(f"yg{i}", (128, 1024)) for i in range(4)]
        for y_ in ygs:
            P.V("memset", ap=y_[:], constant=0.0, R=[], W=[y_])
        for e in range(n_exp):
            P.dma("sync", yl_scr[e][1024:1025, :], ygs[0][0:1, :])
            if with_ctx:
                P.dma("sync", yc_scr[e][32:33, :], ygs[0][0:1, :])
        accs = [f32t(f"acc{i}", (128, 1024)) for i in range(2)]
        xms = [f32t(f"xm{i}", (128, 1024)) for i in range(2)]
        gi = 0
        for i, t in enumerate(ctx_tiles + lat_tiles):
            acc, xm = accs[i % 2], xms[i % 2]
            P.dma("sync", xm[:], xmid_scr[t * 128:(t + 1) * 128, :])
            for e in range(n_exp):
                yg = ygs[gi % 4]
                gi += 1
                if t < 2:
                    P.idma(R=[yc_scr[e], posIC], W=[yg], out=yg[:, :], out_offset=None, in_=yc_scr[e][:, :],
                           in_offset=bass.IndirectOffsetOnAxis(ap=posIC[:, e, t:t + 1], axis=0))
                    gcol = gmC[:, e, t:t + 1]
                else:
                    P.idma(R=[yl_scr[e], posIL], W=[yg], out=yg[:, :], out_offset=None, in_=yl_scr[e][:, :],
                           in_offset=bass.IndirectOffsetOnAxis(ap=posIL[:, e, t - 2:t - 1], axis=0))
                    gcol = gmL[:, e, t - 2:t - 1]
                if e == 0:
                    P.V("tensor_scalar", out=acc[:], in0=yg[:], scalar1=gcol, scalar2=None, op0=ALU.mult)
                else:
                    P.V("scalar_tensor_tensor", out=acc[:], in0=yg[:], scalar=gcol, in1=acc[:], op0=ALU.mult, op1=ALU.add)
            P.G("tensor_tensor", out=acc[:], in0=acc[:], in1=(gt2C if t < 2 else gt2S)[:], op=ALU.mult)
            P.G("tensor_tensor", out=acc[:], in0=acc[:], in1=xm[:], op=ALU.add)
            if t < 2 or out_row0 is None:
                P.dma("sync", out_ap[t * 128:(t + 1) * 128, :], acc[:])
            else:
                P.dma("sync", out_ap[t * 128 - out_row0:(t + 1) * 128 - out_row0, :], acc[:])


ODD_N = 3088


def odd_consts(inp):
    s = np.arange(64)
    earlyeq = (s[:, None] <= s[None, :]).astype(np.float32)
    MB = np.tile(np.concatenate([earlyeq, earlyeq.T], 0), (1, 4))
    ab = inp["gla_a_b"][0]
    abb = np.concatenate([np.broadcast_to(ab[0][None], (64, 256)), np.broadcast_to(ab[1][None], (64, 256))], 0)
    glc = np.stack([MB, abb], 1).astype(np.float32)
    aup = np.ascontiguousarray(inp["gla_a_up"][0].transpose(1, 0, 2)).astype(np.float32)
    nf = 16
    inv = (10000.0 ** (-np.arange(nf, dtype=np.float32) / nf)).astype(np.float32)
    t = np.arange(8192)
    ar = (t // 64).astype(np.float32)[:, None] * inv[None, :]
    ac = (t % 64).astype(np.float32)[:, None] * inv[None, :]
    cr, sr, cc, sc = np.cos(ar), np.sin(ar), np.cos(ac), np.sin(ac)
    C = np.concatenate([cr, cr, cc, cc], 1)
    S = np.concatenate([-sr, sr, -sc, sc], 1)
    Cf = np.concatenate([np.ones((256, 64), np.float32), C.astype(np.float32)], 0)
    Sf = np.concatenate([np.zeros((256, 64), np.float32), S.astype(np.float32)], 0)
    rope = np.ascontiguousarray(np.stack([Cf, Sf], 1))
    qkg = np.stack([bc128(np.tile(inp["nat_qn_g"][0], 8)), bc128(np.tile(inp["nat_kn_g"][0], 8))], 1).astype(np.float32)
    rpb = inp["nat_rpb"][0]
    kidx = np.arange(512)
    ro, cp = kidx // 64, kidx % 64
    q = np.arange(64)
    ws = np.clip(q - 8, 0, 48)
    valid = (cp[:, None] >= ws[None, :]) & (cp[:, None] < ws[None, :] + 16)
    cb = np.clip(cp[:, None] - q[None, :] + 15, 0, 30)
    bias = np.full((8, 8, 512, 64), -30000.0, np.float32)
    for pat in range(8):
        vals = rpb[:, (ro + pat)[:, None], cb]
        bias[:, pat] = np.where(valid[None], vals, -30000.0)
    natb = np.ascontiguousarray(bias.reshape(8, 8, 4, 128, 64).transpose(0, 3, 1, 2, 4))
    return dict(glc=np.ascontiguousarray(glc), aup=aup, rope=rope, qkg=np.ascontiguousarray(qkg), natb=natb,
                glng=bc128(np.tile(inp["gla_ln_g"][0], 4)).reshape(128, 1, 512).astype(np.float32),
                od_w_in=kmajor(inp["od_w_in"][0]), od_w_out=kmajor(inp["od_w_out"][0]))


def stage_gla(P, G, p_scr, of_scr, or_scr, glc_d, aup_d, rope_d, cumM_d, sel_d, nsteps=132):
    NH = 4
    f32t = lambda n, shp=(128, 512): P.sb("gl_" + n, list(shp), F32)
    bft = lambda n, shp=(128, 512): P.sb("gl_" + n, list(shp), BF16)
    glc = f32t("glc", (128, 2, 256))
    aup = f32t("aup", (16, 2, 256))
    cumM = f32t("cumM", (128, 128))
    sel = f32t("sel", (128, 2))
    P.dma("sync", glc[:], glc_d[:, :, :])
    P.dma("sync", aup[:], aup_d[:, :, :])
    P.dma("sync", cumM[:], cumM_d[:, :])
    P.dma("sync", sel[:], sel_d[:, :])
    MB, abb = glc[:, 0, :], glc[:, 1, :]
    qkvs = [f32t(f"qkv{i}", (128, 1024)) for i in range(2)]
    gas = [f32t(f"ga{i}", (128, 16)) for i in range(2)]
    ropes = [f32t(f"rope{i}", (128, 2, 64)) for i in range(2)]
    xs, xr, xr2 = f32t("xs"), f32t("xr"), f32t("xr2")
    gaT = f32t("gaT", (16, 128))
    xg, ex, sp = f32t("xg", (128, 256)), f32t("ex", (128, 256)), f32t("sp", (128, 256))
    E, Einv = f32t("E", (128, 256)), f32t("Einv", (128, 256))
    qt_t, kt_b = bft("qt_t", (128, 256)), bft("kt_b", (128, 256))
    vb = bft("vb")
    qT, kT = bft("qT", (64, 4, 128)), bft("kT", (64, 4, 128))
    Bqk = bft("Bqk", (128, 256))
    ya, y_sb = f32t("ya"), f32t("y_sb")
    Qf, tmpq = f32t("Qf", (64, 1024)), f32t("tmpq", (64, 1024))
    Qb = bft("Qb", (64, 1024))
    cC = f32t("cC", (64, 8))
    P.V("memset", ap=Qf[:], constant=0.0, R=[], W=[Qf])
    P.V("memset", ap=Qb[:], constant=0.0, R=[], W=[Qb])
    pf = [P.ps(f"glpf{i}", [128, 512]) for i in range(6)]
    pb = [P.ps(f"glpb{i}", [64, 4, 128], BF16) for i in range(2)]
    pfi = [0]

    def nxt():
        t = pf[pfi[0] % 6]
        pfi[0] += 1
        return t

    for n in range(nsteps):
        m0 = n
        m1 = (3 - n) if n < 4 else (135 - n)
        qkv, ga, rope = qkvs[n % 2], gas[n % 2], ropes[n % 2]
        for d, m in ((0, m0), (1, m1)):
            P.dma("sync", qkv[64 * d:64 * d + 64, :], p_scr[64 * m:64 * m + 64, 1536:2560], W=[(qkv, d)])
            P.dma("sync", ga[64 * d:64 * d + 64, :], p_scr[64 * m:64 * m + 64, 3072:3088], W=[(ga, d)])
            P.dma("sync", rope[64 * d:64 * d + 64, :, :], rope_d[64 * m:64 * m + 64, :, :], W=[(rope, d)])
        x4 = qkv[:, 0:512].rearrange("p (a two c) -> p a two c", two=2, c=16)
        s4 = xs[:].rearrange("p (a two c) -> p a two c", two=2, c=16)
        P.V("tensor_copy", out=s4[:, :, 0, :], in_=x4[:, :, 1, :], R=[qkv], W=[(xs, 0)])
        P.G("tensor_copy", out=s4[:, :, 1, :], in_=x4[:, :, 0, :], R=[qkv], W=[(xs, 1)])
        v8 = lambda ap: ap.rearrange("p (h j) -> p h j", h=8)
        Cb = rope[:, 0, :].unsqueeze(1).to_broadcast([128, 8, 64])
        Sb = rope[:, 1, :].unsqueeze(1).to_broadcast([128, 8, 64])
        P.V("tensor_tensor", out=v8(xr[:]), in0=v8(qkv[:, 0:512]), in1=Cb, op=ALU.mult, R=[qkv, rope], W=[xr])
        P.V("tensor_tensor", out=v8(xr2[:]), in0=v8(xs[:]), in1=Sb, op=ALU.mult, R=[xs, rope], W=[xr2])
        P.G("tensor_tensor", out=xr[:], in0=xr[:], in1=xr2[:], op=ALU.add)
        pt = nxt()
        P.T("transpose", out=pt[0:16, 0:128], in_=ga[:], identity=G.identF[:])
        P.V("tensor_copy", out=gaT[:], in_=pt[0:16, 0:128])
        pg = nxt()
        for d in range(2):
            P.T("matmul", out=pg[64 * d:64 * d + 64, 0:256], lhsT=gaT[:, 64 * d:64 * d + 64], rhs=aup[:, d, :], start=True, stop=True)
        P.V("tensor_tensor", out=xg[:], in0=pg[:, 0:256], in1=abb, op=ALU.add, R=[pg, glc])
        P.S("activation", out=ex[:], in_=xg[:], func=AF.Exp, scale=-1.0)
        P.S("activation", out=sp[:], in_=ex[:], func=AF.Ln, bias=G.oneb[:, 0:1])
        pL = nxt()
        P.T("matmul", out=pL[:, 0:256], lhsT=cumM[:], rhs=sp[:], start=True, stop=True)
        P.S("activation", out=E[:], in_=pL[:, 0:256], func=AF.Exp, scale=-1.0 / 16)
        P.S("activation", out=Einv[:], in_=pL[:, 0:256], func=AF.Exp, scale=1.0 / 16)
        P.V("scalar_tensor_tensor", out=qt_t[:], in0=xr[:, 0:256], scalar=0.125, in1=E[:], op0=ALU.mult, op1=ALU.mult)
        P.G("tensor_tensor", out=kt_b[:], in0=xr[:, 256:512], in1=Einv[:], op=ALU.mult)
        P.S("copy", out=vb[:], in_=qkv[:, 512:1024], R=[qkv])
        pc = nxt()
        for h in range(NH):
            P.T("matmul", out=pc[0:64, 2 * h:2 * h + 2], lhsT=E[:, 64 * h:64 * h + 64], rhs=sel[:], start=True, stop=True)
        P.V("tensor_copy", out=cC[:], in_=pc[0:64, 0:8])
        for qi, (src, dst) in enumerate(((qt_t, qT), (kt_b, kT))):
            pbt = pb[qi]
            for h in range(NH):
                P.T("transpose", out=pbt[:, h, :], in_=src[:, 64 * h:64 * h + 64], identity=G.identB[:])
            if qi == 0:
                P.S("copy", out=dst[:], in_=pbt[:])
            else:
                P.V("tensor_copy", out=dst[:], in_=pbt[:])
        p1 = nxt()
        for h in range(NH):
            for d in range(2):
                P.T("matmul", out=p1[64 * d:64 * d + 64, 64 * h:64 * h + 64], lhsT=kT[:, h, 64 * d:64 * d + 64],
                    rhs=qT[:, h, 64 * d:64 * d + 64], start=True, stop=True, R=[kT, qT], W=[p1])
        P.V("tensor_tensor", out=Bqk[:], in0=p1[:, 0:256], in1=MB, op=ALU.mult, R=[p1, glc])
        pya, pyb = nxt(), nxt()
        for h in range(NH):
            for d in range(2):
                u = 4 * d + h
                P.T("matmul", out=pya[64 * d:64 * d + 64, 128 * h:128 * h + 128], lhsT=qT[:, h, 64 * d:64 * d + 64],
                    rhs=Qb[:, 128 * u:128 * u + 128], start=True, stop=True, R=[qT, Qb], W=[pya])
                P.T("matmul", out=pyb[64 * d:64 * d + 64, 128 * h:128 * h + 128], lhsT=Bqk[64 * d:64 * d + 64, 64 * h:64 * h + 64],
                    rhs=vb[64 * d:64 * d + 64, 128 * h:128 * h + 128], start=True, stop=True, R=[Bqk, vb], W=[pyb])
        P.S("copy", out=ya[:], in_=pya[:])
        P.V("tensor_tensor", out=y_sb[:], in0=pyb[:], in1=ya[:], op=ALU.add)
        P.dma("sync", of_scr[64 * m0:64 * m0 + 64, :], y_sb[0:64, :])
        P.dma("sync", or_scr[64 * m1:64 * m1 + 64, :], y_sb[64:128, :])
        for d in range(2):
            pq = nxt()
            for h in range(NH):
                P.T("matmul", out=pq[0:64, 128 * h:128 * h + 128], lhsT=kt_b[64 * d:64 * d + 64, 64 * h:64 * h + 64],
                    rhs=vb[64 * d:64 * d + 64, 128 * h:128 * h + 128], start=True, stop=True, R=[kt_b, vb], W=[pq])
            P.V("tensor_tensor", out=tmpq[:, 512 * d:512 * d + 512], in0=pq[0:64, :], in1=Qf[:, 512 * d:512 * d + 512],
                op=ALU.add, W=[(tmpq, d)], R=[pq, Qf])
        P.V("tensor_tensor", out=Qf[:].rearrange("p (d h i) -> p d h i", d=2, h=NH), in0=tmpq[:].rearrange("p (d h i) -> p d h i", d=2, h=NH),
            in1=cC[:].rearrange("p (h d) -> p d h", d=2).unsqueeze(3).to_broadcast([64, 2, NH, 128]), op=ALU.mult)
        P.S("copy", out=Qb[:], in_=Qf[:])


def stage_nat(P, G, p_scr, nat_scr, qkg_d, natb_d, qT_scr, kT_scr, va_scr):
    f32t = lambda n, shp: P.sb("na_" + n, list(shp), F32)
    bft = lambda n, shp: P.sb("na_" + n, list(shp), BF16)
    with P.scope():
        if "nat" in NO_REORDER or "natprep" in NO_REORDER:
            P.mark_no_reorder()
        qkg = f32t("qkg", (128, 2, 512))
        P.dma("sync", qkg[:], qkg_d[:, :, :])
        pts = [f32t(f"pt{i}", (128, 1536)) for i in range(2)]
        sq = f32t("sq", (128, 1024))
        ms = f32t("ms", (128, 16))
        qkn = bft("qkn", (128, 1024))
        qkT = [bft(f"qkT{i}", (64, 16, 128)) for i in range(2)]
        vas = [bft(f"va{i}", (128, 8, 65)) for i in range(2)]
        for v_ in vas:
            P.V("memset", ap=v_[:], constant=1.0, R=[], W=[v_])
        pTs = [P.ps(f"na_pT{i}", [64, 8, 128], BF16) for i in range(2)]
        for t in range(NTILE):
            pt, va, qT_ = pts[t % 2], vas[t % 2], qkT[t % 2]
            P.dma("sync", pt[:], p_scr[t * 128:(t + 1) * 128, 0:1536])
            P.G("tensor_tensor", out=sq[:], in0=pt[:, 0:1024], in1=pt[:, 0:1024], op=ALU.mult)
            P.V("tensor_reduce", out=ms[:], in_=sq[:].rearrange("p (h j) -> p h j", h=16), axis=AX.X, op=ALU.add)
            P.S("activation", out=ms[:], in_=ms[:], func=AF.Sqrt, scale=1.0 / 64, bias=G.epsb[:, 0:1])
            P.V("reciprocal", out=ms[:], in_=ms[:])
            P.V("tensor_tensor", out=sq[:].rearrange("p (h j) -> p h j", h=16), in0=pt[:, 0:1024].rearrange("p (h j) -> p h j", h=16),
                in1=ms[:].unsqueeze(2).to_broadcast([128, 16, 64]), op=ALU.mult)
            P.V("scalar_tensor_tensor", out=qkn[:, 0:512], in0=sq[:, 0:512], scalar=0.125, in1=qkg[:, 0, :], op0=ALU.mult, op1=ALU.mult,
                W=[(qkn, 0)])
            P.G("tensor_tensor", out=qkn[:, 512:1024], in0=sq[:, 512:1024], in1=qkg[:, 1, :], op=ALU.mult, W=[(qkn, 1)])
            for half in range(2):
                for h in range(8):
                    c = half * 8 + h
                    P.T("transpose", out=pTs[half][:, h, :], in_=qkn[:, 64 * c:64 * c + 64], identity=G.identB[:], R=[qkn])
            P.S("copy", out=qT_[:, 0:8, :], in_=pTs[0][:], W=[(qT_, 0)])
            P.V("tensor_copy", out=qT_[:, 8:16, :], in_=pTs[1][:], W=[(qT_, 1)])
            P.dma("sync", qT_scr[:, :, t * 128:(t + 1) * 128].rearrange("h j t -> j h t"), qT_[:, 0:8, :])
            P.dma("sync", kT_scr[:, :, t * 128:(t + 1) * 128].rearrange("h j t -> j h t"), qT_[:, 8:16, :])
            P.S("copy", out=va[:, :, 0:64], in_=pt[:, 1024:1536].rearrange("p (h j) -> p h j", h=8))
            P.dma("sync", va_scr[t * 128:(t + 1) * 128, :, :], va[:])
    with P.scope():
        if "nat" in NO_REORDER or "natattn" in NO_REORDER:
            P.mark_no_reorder()
        qTs = [bft(f"qTh{i}", (64, T_TOK)) for i in range(2)]
        kTs = [bft(f"kTh{i}", (64, T_TOK)) for i in range(2)]
        Vas = [bft(f"Va{i}", (128, 66, 65)) for i in range(2)]
        Vbs = [bft(f"Vb{i}", (128, 65, 65)) for i in range(2)]
        nbs = [f32t(f"nb{i}", (128, 8, 4, 64)) for i in range(2)]
        sb = [f32t(f"sb{i}", (128, 4, 64)) for i in range(4)]
        pex = [bft(f"pex{i}", (128, 6, 64)) for i in range(4)]
        rec = f32t("rec", (128, 1))
        no = [f32t(f"no{i}", (128, 64)) for i in range(3)]
        recs = [f32t(f"rec{i}", (128, 1)) for i in range(3)]
        pss = [P.ps(f"na_ps{i}", [128, 6, 64]) for i in range(5)]
        pos_ = [P.ps(f"na_po{i}", [128, 65]) for i in range(3)]
        units = [(h, rp, sub) for h in range(8) for rp in range(64) for sub in range(2)]
        hbuf = {}

        def load_head(h):
            qT, kT, Va, Vb, nb = qTs[h % 2], kTs[h % 2], Vas[h % 2], Vbs[h % 2], nbs[h % 2]
            P.dma("sync", qT[:], qT_scr[h])
            P.dma("sync", kT[:], kT_scr[h])
            P.dma("sync", Va[:], va_scr[:, h, :].rearrange("(n p) c -> p n c", p=128))
            P.dma("sync", Vb[:], va_scr[64:64 + 65 * 128, h, :].rearrange("(n p) c -> p n c", p=128))
            P.dma("sync", nb[:], natb_d[h])
            hbuf[h] = (qT, kT, Va, Vb, nb)

        def geom(r):
            rs = min(max(r - 4, 0), 120)
            pat = 3 if 4 <= r <= 124 else (7 - r if r < 4 else 127 - r)
            return rs, pat, 256 + 64 * rs, 256 + 64 * r

        def emit_S(i):
            h, rp, sub = units[i]
            if h not in hbuf:
                load_head(h)
            qT, kT, Va, Vb, nb = hbuf[h]
            rs, pat, s0, q0 = geom(2 * rp + sub)
            ps = pss[i % 5]
            for blk in range(6):
                k0 = s0 + 128 * blk if blk < 4 else 128 * (blk - 4)
                P.T("matmul", out=ps[:, blk, :], lhsT=kT[:, k0:k0 + 128], rhs=qT[:, q0:q0 + 64], start=True, stop=True,
                    R=[kT, qT], W=[ps])
            s_, pe = sb[i % 4], pex[i % 4]
            P.V("tensor_tensor", out=s_[:], in0=ps[:, 0:4, :], in1=nb[:, pat, :, :], op=ALU.add, R=[ps, nb])
            P.S("activation", out=pe[:, 0:4, :], in_=s_[:], func=AF.Exp, W=[(pe, 0)])
            P.S("activation", out=pe[:, 4:6, :], in_=ps[:, 4:6, :], func=AF.Exp, W=[(pe, 1)])

        def emit_PV(i):
            h, rp, sub = units[i]
            qT, kT, Va, Vb, nb = hbuf[h]
            rs, pat, s0, q0 = geom(2 * rp + sub)
            pe = pex[i % 4]
            po = pos_[rp % 3]
            for blk in range(6):
                if blk >= 4:
                    vt = Va[:, blk - 4, :]
                elif rs % 2 == 0:
                    vt = Va[:, s0 // 128 + blk, :]
                else:
                    vt = Vb[:, (s0 - 64) // 128 + blk, :]
                P.T("matmul", out=po[64 * sub:64 * sub + 64, :], lhsT=pe[:, blk, :], rhs=vt, start=(blk == 0), stop=(blk == 5),
                    R=[pe, Va, Vb], W=[po])
            if sub == 1:
                n_ = no[rp % 3]
                rec = recs[rp % 3]
                P.V("reciprocal", out=rec[:], in_=po[:, 64:65])
                P.V("tensor_scalar", out=n_[:], in0=po[:, 0:64], scalar1=rec[:, 0:1], scalar2=None, op0=ALU.mult)
                P.dma("sync", nat_scr[256 + 128 * rp:256 + 128 * (rp + 1), 64 * h:64 * h + 64], n_[:])

        emit_S(0)
        for i in range(len(units)):
            if i + 1 < len(units):
                emit_S(i + 1)
            emit_PV(i)


def build_main_nc():
    nc, _ = build_main(upto="all", fuse_adaln=True)
    return nc


def kernel(**inp):
    inp = {k: np.asarray(v) for k, v in inp.items()}
    maps = [main_inputs(inp, None, b) for b in range(4)]
    if "main" not in _CACHE:
        _CACHE["main"] = build_main_nc()
    res = run_bass_kernel_spmd(_CACHE["main"], maps, core_ids=[0, 1, 2, 3]).results
    return np.stack([res[b]["out"] for b in range(4)], 0).astype(np.float32)


def stage_adaln(P, G, cT_d, adaw_d, adab_d, ng_d, mods_scr):
    f32t = lambda n, shp: P.sb("ad_" + n, list(shp), F32)
    cT = f32t("cT", (128, 8, 2))
    sc = f32t("sc", (128, 8, 2))
    P.dma("sync", cT[:], cT_d[:, :, :])
    P.S("activation", out=sc[:], in_=cT[:], func=AF.Silu)
    selr = f32t("selr", (2, 2, 128))
    P.dma("sync", selr[:], G.selr_d[:, :, :])
    aws = [f32t(f"aw{i}", (128, 8, 512)) for i in range(2)]
    ab = f32t("ab", (2, 6144))
    ng = f32t("ng", (2, 2, 1024))
    m = f32t("m", (2, 6144))
    bc = [f32t(f"bc{i}", (128, 1024)) for i in range(2)]
    pms = [P.ps(f"ad_pm{i}", [2, 512]) for i in range(2)]
    pbs = [P.ps(f"ad_pb{i}", [128, 512]) for i in range(4)]
    ev = 0
    for l in range(2):
        P.dma("sync", ab[:], adab_d[l, 0:2, :])
        P.dma("sync", ng[:], ng_d[l, 0:2, :, :])
        for cb in range(12):
            aw = aws[cb % 2]
            P.dma("sync", aw[:], adaw_d[l, :, :, cb * 512:(cb + 1) * 512])
            pm = pms[cb % 2]
            for k in range(8):
                P.T("matmul", out=pm[:], lhsT=sc[:, k, :], rhs=aw[:, k, :], start=(k == 0), stop=(k == 7))
            P.V("tensor_tensor", out=m[:, cb * 512:(cb + 1) * 512], in0=pm[:], in1=ab[:, cb * 512:(cb + 1) * 512], op=ALU.add,
                W=[(m, cb)])
        P.V("scalar_tensor_tensor", out=m[:, 1024:2048], in0=m[:, 1024:2048], scalar=1.0, in1=ng[:, 0, :], op0=ALU.add, op1=ALU.mult)
        P.V("scalar_tensor_tensor", out=m[:, 4096:5120], in0=m[:, 4096:5120], scalar=1.0, in1=ng[:, 1, :], op0=ALU.add, op1=ALU.mult)
        for s in range(2):
            for v in range(6):
                b_ = bc[ev % 2]
                for hb in range(2):
                    pb = pbs[(2 * ev + hb) % 4]
                    P.T("matmul", out=pb[:], lhsT=selr[:, s, :], rhs=m[:, v * 1024 + hb * 512:v * 1024 + hb * 512 + 512], start=True, stop=True)
                    if hb == 0:
                        P.S("copy", out=b_[:, 0:512], in_=pb[:], W=[(b_, 0)])
                    else:
                        P.V("tensor_copy", out=b_[:, 512:1024], in_=pb[:], W=[(b_, 1)])
                ev += 1
                P.dma("sync", mods_scr[l, s, v], b_[:])


def build_main_nc():
    nc, _ = build_main(upto="all", fuse_adaln=True)
    return nc


def kernel(**inp):
    inp = {k: np.asarray(v) for k, v in inp.items()}
    maps = [main_inputs(inp, None, b) for b in range(4)]
    if "main" not in _CACHE:
        _CACHE["main"] = build_main_nc()
    res = run_bass_kernel_spmd(_CACHE["main"], maps, core_ids=[0, 1, 2, 3]).results
    return np.stack([res[b]["out"] for b in range(4)], 0).astype(np.float32)


def stage_adaln(P, G, cT_d, adaw_d, adab_d, ng_d, mods_scr):
    f32t = lambda n, shp: P.sb("ad_" + n, list(shp), F32)
    cT = f32t("cT", (128, 8, 2))
    sc = f32t("sc", (128, 8, 2))
    rep = [f32t(f"rep{s}", (128, 8, 128)) for s in range(2)]
    P.dma("sync", cT[:], cT_d[:, :, :])
    P.S("activation", out=sc[:], in_=cT[:], func=AF.Silu)
    for s in range(2):
        P.V("tensor_copy", out=rep[s][:], in_=sc[:, :, s].unsqueeze(2).to_broadcast([128, 8, 128]))
    aws = [f32t(f"aw{i}", (128, 8, 512)) for i in range(2)]
    ab = f32t("ab", (128, 6144))
    ng = f32t("ng", (128, 2, 1024))
    ms = [f32t(f"m{s}", (128, 6144)) for s in range(2)]
    pms = [P.ps(f"ad_pm{i}", [128, 512]) for i in range(4)]
    ev = 0
    for l in range(2):
        P.dma("sync", ab[:], adab_d[l])
        P.dma("sync", ng[:], ng_d[l])
        for cb in range(12):
            aw = aws[cb % 2]
            P.dma("sync", aw[:], adaw_d[l, :, :, cb * 512:(cb + 1) * 512])
            for s in range(2):
                pm = pms[ev % 4]
                ev += 1
                for k in range(8):
                    P.T("matmul", out=pm[:], lhsT=rep[s][:, k, :], rhs=aw[:, k, :], start=(k == 0), stop=(k == 7))
                P.V("tensor_tensor", out=ms[s][:, cb * 512:(cb + 1) * 512], in0=pm[:], in1=ab[:, cb * 512:(cb + 1) * 512], op=ALU.add,
                    W=[(ms[s], cb)])
        for s in range(2):
            m = ms[s]
            P.V("scalar_tensor_tensor", out=m[:, 1024:2048], in0=m[:, 1024:2048], scalar=1.0, in1=ng[:, 0, :], op0=ALU.add, op1=ALU.mult)
            P.V("scalar_tensor_tensor", out=m[:, 4096:5120], in0=m[:, 4096:5120], scalar=1.0, in1=ng[:, 1, :], op0=ALU.add, op1=ALU.mult)
            for v in range(6):
                P.dma("sync", mods_scr[l, s, v], m[:, v * 1024:(v + 1) * 1024])
```

```python
import ml_dtypes
import contextlib
import numpy as np
import concourse.bass as bass
import concourse.mybir as mybir
from concourse.bass_utils import run_bass_kernel_spmd

F32 = mybir.dt.float32
BF16 = mybir.dt.bfloat16
I32 = mybir.dt.int32
AF = mybir.ActivationFunctionType
ALU = mybir.AluOpType
AX = mybir.AxisListType

ENGS = ("tensor", "vector", "scalar", "gpsimd", "sync")
N_DMA_SEM = 8
import os as _os
REORDER = _os.environ.get('REORDER', '1') == '1'
PE_WIN = int(_os.environ.get('PE_WIN', '1'))


class Prog:
    def __init__(self, same_engine_sync=True, reorder=None):
        self.reorder = REORDER if reorder is None else reorder
        self.seg_noreorder = set()
        self.nc = bass.Bass("TRN2", target_bir_lowering=False)
        self.stack = contextlib.ExitStack()
        self.ops = []
        self.same_engine_sync = same_engine_sync
        self.n_names = 0

    def dram(self, name, shape, dt, kind):
        k = {"in": "ExternalInput", "out": "ExternalOutput", "tmp": "Internal"}[kind]
        return self.nc.dram_tensor(name, list(shape), dt, kind=k).ap()

    def sb(self, name, shape, dt):
        return self.stack.enter_context(self.nc.sbuf_tensor(name, list(shape), dt))

    def ps(self, name, shape, dt=F32):
        return self.stack.enter_context(self.nc.psum_tensor(name, list(shape), dt))

    @staticmethod
    def _keys(lst):
        out = []
        for x in lst:
            if x is None:
                continue
            if isinstance(x, tuple):
                t, sub = x
                out.append((t if isinstance(t, str) else t.name, sub))
            elif isinstance(x, str):
                out.append((x, None))
            else:
                out.append((x.name, None))
        return out

    def op(self, eng, meth, R=None, W=None, **kw):
        if W is None:
            W = [v for k, v in kw.items() if k in ("out", "accum_out") and hasattr(v, "name")]
        if R is None:
            R = [v for k, v in kw.items() if k not in ("out", "accum_out") and hasattr(v, "name") and hasattr(v, "partition_size")]
        self.ops.append((eng, "c", (meth, kw), self._keys(R), self._keys(W)))

    def dma(self, eng, out, in_, R=None, W=None, **kw):
        if W is None:
            W = [out]
        if R is None:
            R = [in_]
        self.ops.append((eng, "d", ("dma_start", dict(out=out, in_=in_, **kw)), self._keys(R), self._keys(W)))

    def idma(self, R, W, **kw):
        self.ops.append(("gpsimd", "d", ("indirect_dma_start", kw), self._keys(R), self._keys(W)))

    def barrier(self):
        self.ops.append(("*", "b", None, [], []))

    def mark_no_reorder(self):
        self.seg_noreorder.add(sum(1 for o in self.ops if o[1] == "b"))

    @contextlib.contextmanager
    def scope(self):
        outer, self.stack = self.stack, contextlib.ExitStack()
        try:
            yield
        finally:
            self.barrier()
            self.stack.close()
            self.stack = outer

    def V(self, meth, **kw):
        self.op("vector", meth, **kw)

    def S(self, meth, **kw):
        self.op("scalar", meth, **kw)

    def G(self, meth, **kw):
        self.op("gpsimd", meth, **kw)

    def T(self, meth, **kw):
        self.op("tensor", meth, **kw)

    @staticmethod
    def _cost(eng, kind, fn):
        meth, kw = fn
        o = kw.get("out", None)
        if o is None:
            o = kw.get("ap", None)
        n = 1
        if o is not None and hasattr(o, "shape"):
            for d in o.shape[1:]:
                n *= d
        if kind == "d":
            if meth == "indirect_dma_start":
                return 1.6, 3.0
            return (0.6 if eng == "gpsimd" else 0.15), 2.0 + n * 128 * 4 / 150e3
        if eng == "tensor":
            return 0.18 + 0.0005 * n, 0.0
        if eng == "vector":
            return 0.08 + 0.00105 * n, 0.0
        if eng == "scalar":
            return 0.22 + 0.00105 * n, 0.0
        return 0.12 + 0.0022 * n, 0.0

    def build(self):
        nc = self.nc
        st = self.stack
        ops = self.ops
        NOPS = len(ops)
        preds = [None] * NOPS
        seg_of = [0] * NOPS
        state = {}
        seg = 0

        def subs_of(name, sub):
            d = state.get(name)
            if d is None:
                return []
            if sub is None:
                return list(d.values())
            res = []
            if None in d:
                res.append(d[None])
            if sub in d:
                res.append(d[sub])
            return res

        for i, (eng, kind, fn, R, W) in enumerate(ops):
            if kind == "b":
                seg += 1
                state.clear()
                seg_of[i] = seg
                continue
            seg_of[i] = seg
            ps = set()
            for (n, sb_) in R:
                for en in subs_of(n, sb_):
                    if en["w"] is not None:
                        ps.add(en["w"])
            for (n, sb_) in W:
                for en in subs_of(n, sb_):
                    if en["w"] is not None:
                        ps.add(en["w"])
                    ps.update(en["r"])
            ps.discard(i)
            preds[i] = sorted(ps)
            for (n, sb_) in R:
                d = state.setdefault(n, {})
                if sb_ not in d:
                    d[sb_] = dict(w=None, r=[])
                d[sb_]["r"].append(i)
            for (n, sb_) in W:
                if sb_ is None:
                    state[n] = {None: dict(w=i, r=[])}
                else:
                    d = state.setdefault(n, {})
                    d[sb_] = dict(w=i, r=[])
        WIN = 24
        order = {e: [] for e in ENGS}
        fin = [0.0] * NOPS
        done = [False] * NOPS
        i0 = 0
        tnow = {e: 0.0 for e in ENGS}
        while i0 < NOPS:
            if ops[i0][1] == "b":
                for e in ENGS:
                    order[e].append(-1)
                tb = max(tnow.values())
                for e in ENGS:
                    tnow[e] = tb
                i0 += 1
                continue
            i1 = i0
            while i1 < NOPS and ops[i1][1] != "b":
                i1 += 1
            q = {e: [] for e in ENGS}
            for i in range(i0, i1):
                q[ops[i][0]].append(i)
            head = {e: 0 for e in ENGS}
            remaining = i1 - i0
            if (not self.reorder) or (seg_of[i0] in self.seg_noreorder):
                for i in range(i0, i1):
                    order[ops[i][0]].append(i)
                remaining = 0
            pe_forced = None
            pos_in_q = {i: j for j, i in enumerate(q["tensor"])}
            while remaining:
                best = None
                for e in ENGS:
                    lst = q[e]
                    h = head[e]
                    if e == "tensor" and pe_forced is not None:
                        i = lst[pe_forced]
                        tr = tnow[e]
                        ok = True
                        for p in preds[i]:
                            if not done[p]:
                                ok = False
                                break
                            lat = 0.0 if ops[p][0] == e else 0.9
                            tr = max(tr, fin[p] + lat)
                        if ok:
                            key = (tr, i)
                            if best is None or key < best[0]:
                                best = (key, e, i)
                        continue
                    while h < len(lst) and done[lst[h]]:
                        h += 1
                    head[e] = h
                    cnt = 0
                    j = h
                    while j < len(lst) and cnt < (PE_WIN if e == "tensor" else WIN):
                        i = lst[j]
                        j += 1
                        if done[i]:
                            continue
                        cnt += 1
                        ok = True
                        tr = tnow[e]
                        for p in preds[i]:
                            if not done[p]:
                                ok = False
                                break
                            lat = 0.0 if (ops[p][0] == e and e == "tensor") else (0.35 if ops[p][0] == e else 0.9)
                            if fin[p] + lat > tr:
                                tr = fin[p] + lat
                        if not ok:
                            continue
                        key = (tr, i)
                        if best is None or key < best[0]:
                            best = (key, e, i)
                        if tr <= tnow[e]:
                            break
                assert best is not None, "scheduler deadlock"
                (tr, _), e, i = best
                iss, lat = self._cost(e, ops[i][1], ops[i][2])
                tnow[e] = tr + iss
                fin[i] = tr + iss + lat
                done[i] = True
                order[e].append(i)
                remaining -= 1
                if e == "tensor":
                    kw_ = ops[i][2][1]
                    if ops[i][2][0] == "matmul" and kw_.get("stop", True) is False:
                        pe_forced = pos_in_q[i] + 1
                    else:
                        pe_forced = None
            i0 = i1
        self.sim_time_us = max(tnow.values())
        EPOCH = 20000
        ntot = {e: sum(1 for i in order[e] if i >= 0 and ops[i][1] == "c") for e in ENGS if e != "sync"}
        esem = {e: [st.enter_context(nc.semaphore(f"sem_{e}_{i}")) for i in range(ntot[e] // EPOCH + 1)] for e in ntot}
        dsem = {e: [st.enter_context(nc.semaphore(f"dsem_{e}_{i}")) for i in range(N_DMA_SEM)]
                for e in ("sync", "gpsimd", "scalar")}
        handle = [None] * NOPS
        ecount = {e: 0 for e in esem}
        dcount = {e: 0 for e in dsem}
        for e in ENGS:
            for i in order[e]:
                if i < 0:
                    continue
                if ops[i][1] == "c":
                    ecount[e] += 1
                    handle[i] = ("e", e, ecount[e])
                else:
                    k = dcount[e]
                    dcount[e] += 1
                    handle[i] = ("d", e, k % N_DMA_SEM, 16 * (k // N_DMA_SEM + 1))
        streams = {e: [] for e in ENGS}
        waited = {e: {} for e in ENGS}

        def need_wait(eng, h):
            if h is None:
                return
            if h[0] == "e":
                _, e2, cnt = h
                if e2 == eng and (eng == "tensor" or not self.same_engine_sync):
                    return
                key = ("e", e2)
            else:
                _, q_, idx, cnt = h
                key = ("d", q_, idx)
            if waited[eng].get(key, 0) >= cnt:
                return
            waited[eng][key] = cnt
            streams[eng].append(("wait", h))

        for e in ENGS:
            ec = {x: 0 for x in esem}
            dc = {x: 0 for x in dsem}
            bar_targets = []
        cum = {e: [] for e in ENGS}
        for e in ENGS:
            ce, cd = 0, 0
            for i in order[e]:
                if i < 0:
                    cum[e].append((ce, cd))
                elif ops[i][1] == "c":
                    ce += 1
                else:
                    cd += 1
            cum[e].append((ce, cd))
        def dma_latest(q_, n, idx):
            if n <= idx:
                return None
            last = n - 1 - ((n - 1 - idx) % N_DMA_SEM)
            return ("d", q_, idx, 16 * (last // N_DMA_SEM + 1))
        for e in ENGS:
            bi = 0
            for i in order[e]:
                if i < 0:
                    for e2 in ENGS:
                        ce, cd = cum[e2][bi]
                        if e2 in esem and ce and e2 != e:
                            need_wait(e, ("e", e2, ce))
                        if e2 in dsem:
                            for idx in range(N_DMA_SEM):
                                need_wait(e, dma_latest(e2, cd, idx))
                    bi += 1
                    continue
                for p in preds[i]:
                    need_wait(e, handle[p])
                h = handle[i]
                if h[0] == "d" and h[3] > 16:
                    need_wait(e, ("d", h[1], h[2], h[3] - 16))
                streams[e].append(("op", ops[i][1], ops[i][2], h))
        for q_ in dsem:
            for idx in range(N_DMA_SEM):
                need_wait(q_, dma_latest(q_, dcount[q_], idx))
        for e in esem:
            if ecount[e]:
                need_wait("sync", ("e", e, ecount[e]))

        def emit(engname, engobj):
            for item in streams[engname]:
                if item[0] == "wait":
                    h = item[1]
                    if h[0] == "e":
                        engobj.wait_ge(esem[h[1]][(h[2] - 1) // EPOCH], (h[2] - 1) % EPOCH + 1)
                    else:
                        engobj.wait_ge(dsem[h[1]][h[2]], h[3])
                else:
                    _, kind, (meth, kw), h = item
                    try:
                        ins = getattr(engobj, meth)(**kw)
                    except Exception:
                        print("EMIT FAIL", engname, meth, {k: (getattr(v, "shape", v), getattr(v, "name", "")) for k, v in kw.items()})
                        raise
                    if h[0] == "e":
                        ins.then_inc(esem[h[1]][(h[2] - 1) // EPOCH], 1)
                    else:
                        ins.then_inc(dsem[h[1]][h[2]], 16)

        with nc.Block() as block:
            @block.sync
            def _(e):
                emit("sync", e)

            @block.tensor
            def _(e):
                emit("tensor", e)

            @block.vector
            def _(e):
                emit("vector", e)

            @block.scalar
            def _(e):
                emit("scalar", e)

            @block.gpsimd
            def _(e):
                emit("gpsimd", e)
        self.n_instr = {e: len(streams[e]) for e in ENGS}
        st.close()
        return nc


BF = ml_dtypes.bfloat16
NCORE = 8
_CACHE = {}


def run_prog(key, builder, in_maps):
    if key not in _CACHE:
        _CACHE[key] = builder()
    res = run_bass_kernel_spmd(_CACHE[key], in_maps, core_ids=list(range(NCORE)))
    return res.results


def kmajor(w):
    K, N = w.shape
    return np.ascontiguousarray(w.reshape(K // 128, 128, N).transpose(1, 0, 2))


def build_p0():
    P = Prog()
    cT_d = P.dram("cT", [128, 8, 5], F32, "in")
    aw_d = P.dram("aw", [128, 8, 1536], F32, "in")
    ab_d = P.dram("ab", [5, 1536], F32, "in")
    ng_d = P.dram("ng", [5, 2, 256], F32, "in")
    o_d = P.dram("o", [5, 6, 256], F32, "out")
    cT = P.sb("cTs", [128, 8, 5], F32)
    sc = P.sb("sc", [128, 8, 5], F32)
    aw = P.sb("aws", [128, 8, 1536], F32)
    ab = P.sb("abs", [5, 1536], F32)
    ng = P.sb("ngs", [5, 2, 256], F32)
    m = P.sb("m", [5, 1536], F32)
    o = P.sb("os", [5, 6, 256], F32)
    pms = [P.ps(f"pm{i}", [5, 512]) for i in range(3)]
    P.dma("sync", cT[:], cT_d[:, :, :])
    for k in range(8):
        P.dma("sync", aw[:, k, :], aw_d[:, k, :], W=[(aw, k)])
    P.dma("sync", ab[:], ab_d[:, :])
    P.dma("sync", ng[:], ng_d[:, :, :])
    P.S("activation", out=sc[:], in_=cT[:], func=AF.Silu)
    for cb in range(3):
        for k in range(8):
            P.T("matmul", out=pms[cb][:], lhsT=sc[:, k, :], rhs=aw[:, k, cb * 512:(cb + 1) * 512],
                start=(k == 0), stop=(k == 7), R=[sc, (aw, k)])
        P.V("tensor_tensor", out=m[:, cb * 512:(cb + 1) * 512], in0=pms[cb][:], in1=ab[:, cb * 512:(cb + 1) * 512], op=ALU.add,
            W=[(m, cb)])
    P.V("tensor_copy", out=o[:].rearrange("p a b -> p (a b)"), in_=m[:])
    P.V("scalar_tensor_tensor", out=o[:, 1, :], in0=m[:, 256:512], scalar=1.0, in1=ng[:, 0, :], op0=ALU.add, op1=ALU.mult)
    P.V("scalar_tensor_tensor", out=o[:, 4, :], in0=m[:, 1024:1280], scalar=1.0, in1=ng[:, 1, :], op0=ALU.add, op1=ALU.mult)
    P.dma("sync", o_d[:, :, :], o[:])
    return P.build()


def run_p0(inp):
    c5 = np.concatenate([inp["c"], inp["c_ctx"][None, :]], 0)
    cT = kmajor(np.ascontiguousarray(c5.T))
    maps = []
    for c in range(NCORE):
        l, q = c // 4, c % 4
        cols = np.concatenate([np.arange(k * 1024 + q * 256, k * 1024 + q * 256 + 256) for k in range(6)])
        aw = kmajor(np.ascontiguousarray(inp["ada_w"][l][:, cols]))
        ab = np.ascontiguousarray(np.broadcast_to(inp["ada_b"][l][cols][None, :], (5, 1536)))
        ng = np.stack([inp["norm1_g"][l][q * 256:(q + 1) * 256], inp["norm2_g"][l][q * 256:(q + 1) * 256]], 0)
        ng = np.ascontiguousarray(np.broadcast_to(ng[None], (5, 2, 256)))
        maps.append(dict(cT=cT, aw=aw, ab=ab, ng=ng))
    res = run_prog("p0", build_p0, maps)
    mods = np.zeros((2, 5, 6, 1024), np.float32)
    for c in range(NCORE):
        l, q = c // 4, c % 4
        mods[l, :, :, q * 256:(q + 1) * 256] = res[c]["o"]
    return mods


def bc128(v):
    return np.ascontiguousarray(np.broadcast_to(v[None], (128,) + v.shape))


import os
RW_CUT = int(os.environ.get('RW_CUT', '0'))
SAME_ENGINE_SYNC = os.environ.get('SES', '1') == '1'
NO_REORDER = set(os.environ.get('NO_REORDER', 'nat').split(','))
T_TOK = 8448
NTILE = 66
C0 = 0.6065306597126334


def emit_weight_bf16(P, w_d, w_sb, K8, N, tag, stg=None):
    if stg is None:
        stg = [P.sb(f"wstg{tag}{i}", [128, N], F32) for i in range(2)]
    for k in range(K8):
        s = stg[k % 2]
        P.dma("sync", s[:, 0:N], w_d[:, k, :])
        if k % 2 == 0:
            P.V("tensor_copy", out=w_sb[:, k, :], in_=s[:, 0:N], W=[(w_sb, k)])
        else:
            P.G("tensor_copy", out=w_sb[:, k, :], in_=s[:, 0:N], W=[(w_sb, k)])
    return stg


def emit_norm_mod(P, xt, gs, sh, hb, tmp, ss, rs, epsb, hf, hf_out=None):
    P.S("activation", out=tmp[:], in_=xt[:], func=AF.Square, accum_out=ss[:])
    P.S("activation", out=rs[:], in_=ss[:], func=AF.Sqrt, scale=1.0 / 1024, bias=epsb[:, 0:1])
    P.V("reciprocal", out=rs[:], in_=rs[:])
    P.V("scalar_tensor_tensor", out=hf[:], in0=xt[:], scalar=rs[:, 0:1], in1=gs, op0=ALU.mult, op1=ALU.mult)
    P.G("tensor_tensor", out=hb[:], in0=hf[:], in1=sh, op=ALU.add)
    if hf_out is not None:
        P.G("tensor_tensor", out=hf_out[:], in0=hf[:], in1=sh, op=ALU.add)


class Ctx:
    pass


def stage_pre(P, G, N, w_d, mod_d, lyr, x_src, p_scr, post=None):
    modS = P.sb(f"s1{lyr}_modS", [128, 2, 1024], F32)
    modC = P.sb(f"s1{lyr}_modC", [128, 2, 1024], F32)
    w_sb = P.sb(f"s1{lyr}_w", [128, 8, N], BF16)
    P.dma("sync", modS[:], mod_d[lyr, 0, 0:2].rearrange("v p f -> p v f"))
    P.dma("sync", modC[:], mod_d[lyr, 1, 0:2].rearrange("v p f -> p v f"))
    emit_weight_bf16(P, w_d, w_sb, 8, N, f"s1{lyr}")
    xts = [P.sb(f"s1{lyr}_xt{i}", [128, 1024], F32) for i in range(2)]
    tmp = P.sb(f"s1{lyr}_tmp", [128, 1024], F32)
    hf = P.sb(f"s1{lyr}_hf", [128, 1024], F32)
    hb = P.sb(f"s1{lyr}_hb", [128, 1024], BF16)
    hT = P.sb(f"s1{lyr}_hT", [128, 8, 128], BF16)
    ss = P.sb(f"s1{lyr}_ss", [128, 1], F32)
    rs = P.sb(f"s1{lyr}_rs", [128, 1], F32)
    pos = [P.sb(f"s1{lyr}_po{i}", [128, N], F32) for i in range(2)]
    pT = P.ps(f"s1{lyr}_pT", [128, 8, 128], BF16)
    pms = [P.ps(f"s1{lyr}_pm{i}", [128, 512]) for i in range(4)]
    nblk = (N + 511) // 512
    ev = 0
    for t in range(NTILE):
        xt = xts[t % 2]
        po = pos[t % 2]
        mod = modC if t < 2 else modS
        P.dma("sync", xt[:], x_src[t * 128:(t + 1) * 128, :])
        emit_norm_mod(P, xt, mod[:, 1, :], mod[:, 0, :], hb, tmp, ss, rs, G.epsb, hf)
        for k in range(8):
            P.T("transpose", out=pT[:, k, :], in_=hb[:, k * 128:(k + 1) * 128], identity=G.identB[:])
        P.S("copy", out=hT[:], in_=pT[:])
        for nb in range(nblk):
            c0, c1 = nb * 512, min(N, nb * 512 + 512)
            pm = pms[ev % 4]
            for k in range(8):
                P.T("matmul", out=pm[:, 0:c1 - c0], lhsT=hT[:, k, :], rhs=w_sb[:, k, c0:c1], start=(k == 0), stop=(k == 7),
                    R=[hT, (w_sb, k)])
            if ev % 2 == 0:
                P.V("tensor_copy", out=po[:, c0:c1], in_=pm[:, 0:c1 - c0], W=[(po, nb)])
            else:
                P.S("copy", out=po[:, c0:c1], in_=pm[:, 0:c1 - c0], W=[(po, nb)])
            ev += 1
        P.dma("sync", p_scr[t * 128:(t + 1) * 128, :], po[:])
        if post is not None:
            post(t, po)


def rwkv_consts(inp):
    def two(v):
        return np.concatenate([np.broadcast_to(v[0][None], (64, 512)), np.broadcast_to(v[1][None], (64, 512))], 0)
    s = np.arange(64)
    early = (s[:, None] < s[None, :]).astype(np.float32)
    earlyeq = (s[:, None] <= s[None, :]).astype(np.float32)
    MA = np.concatenate([early, early.T], 0)
    MB = np.concatenate([earlyeq, earlyeq.T], 0)
    MC = np.concatenate([early.T, early], 0)
    eye = np.concatenate([np.eye(64, dtype=np.float32)] * 2, 0)
    t8 = lambda m: np.tile(m, (1, 8))
    rwc = np.stack([two(inp["rw_w0"][0]), two(inp["rw_a0"][0]), bc128(inp["rw_k_k"][0]), bc128(inp["rw_k_a"][0]),
                    t8(MA), t8(MB), t8(MC), t8(eye)], 1).astype(np.float32)
    wup = np.stack([inp["rw_w_up"][0], inp["rw_a_up"][0]], 0).transpose(2, 0, 1, 3)
    cumM = np.zeros((128, 128), np.float32)
    cumM[:64, :64] = earlyeq
    cumM[64:, 64:] = earlyeq.T
    sel = np.zeros((128, 2), np.float32)
    sel[63, 0] = 1.0
    sel[64, 1] = 1.0
    return dict(rwc=np.ascontiguousarray(rwc), wup=np.ascontiguousarray(wup.astype(np.float32)), cumM=cumM, sel=sel)


def stage_rwkv(P, G, p_scr, yf_scr, yr_scr, rwc_d, wup_d, cumM_d, sel_d, nsteps=132):
    NH = 8
    f32t = lambda n, shp=(128, 512): P.sb("rw_" + n, list(shp), F32)
    rwc = f32t("rwc", (128, 8, 512))
    wup = f32t("wup", (64, 2, 2, 512))
    cumM = f32t("cumM", (128, 128))
    sel = f32t("sel", (128, 2))
    P.dma("sync", rwc[:], rwc_d[:, :, :])
    P.dma("sync", wup[:], wup_d[:, :, :, :])
    P.dma("sync", cumM[:], cumM_d[:, :])
    P.dma("sync", sel[:], sel_d[:, :])
    w0b, a0b, kkb, kab, MA, MB, MC, EYE = [rwc[:, i, :] for i in range(8)]
    rkvs = [f32t(f"rkv{i}", (128, 1536)) for i in range(2)]
    xwas = [f32t(f"xwa{i}", (128, 128)) for i in range(2)]
    txw = f32t("txw", (128, 64))
    xT = f32t("xT", (64, 2, 128))
    wr, sg, ar, av = f32t("wr"), f32t("sg"), f32t("ar"), f32t("av")
    E, Einv, Eprev, Lp = f32t("E"), f32t("Einv"), f32t("Eprev"), f32t("Lp")
    kk0, sq, kk, t1, keff, bb = f32t("kk0"), f32t("sq"), f32t("kk"), f32t("t1"), f32t("keff"), f32t("bb")
    n2, rn = f32t("n2", (128, 8)), f32t("rn", (128, 8))
    tiny = f32t("tiny", (128, 1))
    P.G("memset", ap=tiny[:], constant=1e-12, R=[], W=[tiny])
    bft = lambda n, shp=(128, 512): P.sb("rw_" + n, list(shp), BF16)
    kap_t, kt_b, bt_b, rt_t, vb = bft("kap_t"), bft("kt_b"), bft("bt_b"), bft("rt_t"), bft("vb")
    kapT, ktT, btT, rtT = [bft(n, (64, 8, 128)) for n in ("kapT", "ktT", "btT", "rtT")]
    AkkT_s, BrkT_s, BrbT_s, negU = bft("AkkT_s"), bft("BrkT_s"), bft("BrbT_s"), bft("negU")
    Xs = [bft(f"X{i}") for i in range(2)]
    Xts = [bft(f"Xt{i}") for i in range(2)]
    Tts = [bft(f"Tt{i}") for i in range(2)]
    W1a, y_sb = f32t("W1a"), f32t("y_sb")
    W1 = bft("W1")
    Qf = f32t("Qf", (64, 1024))
    Qb = bft("Qb", (64, 1024))
    tmpq = f32t("tmpq", (64, 1024))
    cC = f32t("cC", (64, 16))
    P.V("memset", ap=Qf[:], constant=0.0, R=[], W=[Qf])
    P.V("memset", ap=Qb[:], constant=0.0, R=[], W=[Qb])
    pf = [P.ps(f"rpf{i}", [128, 512]) for i in range(6)]
    pb = [P.ps(f"rpb{i}", [64, 8, 128], BF16) for i in range(2)]
    pfi = [0]

    def nxt():
        t = pf[pfi[0] % 6]
        pfi[0] += 1
        return t

    def unit_mm(out_t, lhs_t, rhs_t, **kw):
        for h in range(NH):
            for d in range(2):
                r0, c0 = 64 * d, 64 * h
                P.T("matmul", out=out_t[r0:r0 + 64, c0:c0 + 64], lhsT=lhs_t[r0:r0 + 64, c0:c0 + 64],
                    rhs=rhs_t[r0:r0 + 64, c0:c0 + 64], start=True, stop=True, R=[lhs_t, rhs_t], W=[out_t])

    def feat_mm(out_t, lT, rT_):
        for h in range(NH):
            for d in range(2):
                P.T("matmul", out=out_t[64 * d:64 * d + 64, 64 * h:64 * h + 64], lhsT=lT[:, h, 64 * d:64 * d + 64],
                    rhs=rT_[:, h, 64 * d:64 * d + 64], start=True, stop=True, R=[lT, rT_], W=[out_t])

    for n in range(nsteps):
        m0 = n
        m1 = (3 - n) if n < 4 else (135 - n)
        rkv, xwa = rkvs[n % 2], xwas[n % 2]
        for d, m in ((0, m0), (1, m1)):
            P.dma("sync", rkv[64 * d:64 * d + 64, :], p_scr[64 * m:64 * m + 64, 1536:3072], W=[(rkv, d)])
            P.dma("sync", xwa[64 * d:64 * d + 64, :], p_scr[64 * m:64 * m + 64, 3072:3200], W=[(xwa, d)])
        r_, k_, v_ = rkv[:, 0:512], rkv[:, 512:1024], rkv[:, 1024:1536]
        P.S("activation", out=txw[:], in_=xwa[:, 0:64], func=AF.Tanh)
        pt = nxt()
        P.T("transpose", out=pt[0:64, 0:128], in_=txw[:], identity=G.identF[:])
        P.T("transpose", out=pt[0:64, 128:256], in_=xwa[:, 64:128], identity=G.identF[:])
        P.V("tensor_copy", out=xT[:].rearrange("p a b -> p (a b)"), in_=pt[0:64, 0:256])
        pw, pa = nxt(), nxt()
        for d in range(2):
            P.T("matmul", out=pw[64 * d:64 * d + 64, :], lhsT=xT[:, 0, 64 * d:64 * d + 64], rhs=wup[:, 0, d, :], start=True, stop=True)
            P.T("matmul", out=pa[64 * d:64 * d + 64, :], lhsT=xT[:, 1, 64 * d:64 * d + 64], rhs=wup[:, 1, d, :], start=True, stop=True)
        P.V("tensor_tensor", out=wr[:], in0=pw[:], in1=w0b, op=ALU.add)
        P.S("activation", out=sg[:], in_=wr[:], func=AF.Sigmoid)
        P.V("tensor_tensor", out=ar[:], in0=pa[:], in1=a0b, op=ALU.add)
        P.S("activation", out=av[:], in_=ar[:], func=AF.Sigmoid)
        if RW_CUT == 1:
            continue
        pL = nxt()
        P.T("matmul", out=pL[:], lhsT=cumM[:], rhs=sg[:], start=True, stop=True)
        P.S("activation", out=E[:], in_=pL[:], func=AF.Exp, scale=-C0)
        P.S("activation", out=Einv[:], in_=pL[:], func=AF.Exp, scale=C0)
        P.V("tensor_tensor", out=Lp[:], in0=pL[:], in1=sg[:], op=ALU.subtract)
        P.S("activation", out=Eprev[:], in_=Lp[:], func=AF.Exp, scale=-C0)
        if RW_CUT == 2:
            continue
        P.G("tensor_tensor", out=kk0[:], in0=k_, in1=kkb, op=ALU.mult, R=[rkv, rwc])
        P.G("tensor_tensor", out=sq[:], in0=kk0[:], in1=kk0[:], op=ALU.mult)
        P.V("tensor_reduce", out=n2[:], in_=sq[:].rearrange("p (h j) -> p h j", h=NH), axis=AX.X, op=ALU.add)
        P.S("activation", out=rn[:], in_=n2[:], func=AF.Sqrt, bias=tiny[:, 0:1])
        P.V("reciprocal", out=rn[:], in_=rn[:])
        P.V("tensor_tensor", out=kk[:].rearrange("p (h j) -> p h j", h=NH), in0=kk0[:].rearrange("p (h j) -> p h j", h=NH),
            in1=rn[:].unsqueeze(2).to_broadcast([128, NH, 64]), op=ALU.mult)
        P.V("scalar_tensor_tensor", out=t1[:], in0=av[:], scalar=-1.0, in1=kab, op0=ALU.add, op1=ALU.mult, R=[av, rwc])
        P.V("scalar_tensor_tensor", out=keff[:], in0=t1[:], scalar=1.0, in1=k_, op0=ALU.add, op1=ALU.mult, R=[t1, rkv])
        P.G("tensor_tensor", out=bb[:], in0=kk[:], in1=av[:], op=ALU.mult)
        if RW_CUT == 3:
            continue
        P.V("tensor_tensor", out=kap_t[:], in0=kk[:], in1=Eprev[:], op=ALU.mult)
        P.G("tensor_tensor", out=kt_b[:], in0=keff[:], in1=Einv[:], op=ALU.mult)
        P.V("tensor_tensor", out=bt_b[:], in0=bb[:], in1=Einv[:], op=ALU.mult)
        P.G("tensor_tensor", out=rt_t[:], in0=r_, in1=E[:], op=ALU.mult, R=[rkv, E])
        P.S("copy", out=vb[:], in_=v_, R=[rkv])
        if RW_CUT == 4:
            continue
        pc = nxt()
        for h in range(NH):
            P.T("matmul", out=pc[0:64, 2 * h:2 * h + 2], lhsT=E[:, 64 * h:64 * h + 64], rhs=sel[:], start=True, stop=True)
        P.V("tensor_copy", out=cC[:], in_=pc[0:64, 0:16])
        if RW_CUT == 5:
            continue
        for qi, (src, dst) in enumerate(((kap_t, kapT), (kt_b, ktT), (bt_b, btT), (rt_t, rtT))):
            pbt = pb[qi % 2]
            for h in range(NH):
                P.T("transpose", out=pbt[:, h, :], in_=src[:, 64 * h:64 * h + 64], identity=G.identB[:])
            if qi % 2 == 0:
                P.S("copy", out=dst[:], in_=pbt[:])
            else:
                P.V("tensor_copy", out=dst[:], in_=pbt[:])
        if RW_CUT == 6:
            continue
        X, Xt, Tt = Xs[0], Xts[0], Tts[0]
        p1 = nxt(); feat_mm(p1, ktT, kapT)
        P.V("tensor_tensor", out=AkkT_s[:], in0=p1[:], in1=MA, op=ALU.mult, R=[p1, rwc])
        p2 = nxt(); feat_mm(p2, btT, kapT)
        P.V("tensor_tensor", out=X[:], in0=p2[:], in1=MA, op=ALU.mult, R=[p2, rwc])
        p3 = nxt(); feat_mm(p3, kapT, btT)
        P.V("tensor_tensor", out=Xt[:], in0=p3[:], in1=MC, op=ALU.mult, R=[p3, rwc])
        p4 = nxt(); feat_mm(p4, ktT, rtT)
        P.V("tensor_tensor", out=BrkT_s[:], in0=p4[:], in1=MB, op=ALU.mult, R=[p4, rwc])
        p5 = nxt(); feat_mm(p5, btT, rtT)
        P.V("tensor_tensor", out=BrbT_s[:], in0=p5[:], in1=MB, op=ALU.mult, R=[p5, rwc])
        if RW_CUT == 7:
            continue
        P.G("tensor_tensor", out=Tt[:], in0=EYE, in1=X[:], op=ALU.subtract, R=[rwc, X])
        cur = 0
        for lev in range(1, 6):
            X, Xt, Tt = Xs[cur], Xts[cur], Tts[cur]
            Xn, Xtn, Ttn = Xs[1 - cur], Xts[1 - cur], Tts[1 - cur]
            pxt = nxt(); unit_mm(pxt, X, Xt)
            P.S("copy", out=Xtn[:], in_=pxt[:])
            if lev < 5:
                px = nxt(); unit_mm(px, Xt, X)
                P.V("tensor_copy", out=Xn[:], in_=px[:])
            ptt = nxt(); unit_mm(ptt, Xtn, Tt)
            P.V("tensor_tensor", out=Ttn[:], in0=ptt[:], in1=Tt[:], op=ALU.add)
            cur = 1 - cur
        Tt = Tts[cur]
        if RW_CUT == 8:
            continue
        pwa = nxt()
        for h in range(NH):
            for d in range(2):
                u = 8 * d + h
                P.T("matmul", out=pwa[64 * d:64 * d + 64, 64 * h:64 * h + 64], lhsT=kapT[:, h, 64 * d:64 * d + 64],
                    rhs=Qb[:, 64 * u:64 * u + 64], start=True, stop=True, R=[kapT, Qb], W=[pwa])
        P.S("copy", out=W1a[:], in_=pwa[:])
        pwb = nxt(); unit_mm(pwb, AkkT_s, vb)
        P.V("tensor_tensor", out=W1[:], in0=pwb[:], in1=W1a[:], op=ALU.add)
        pu = nxt(); unit_mm(pu, Tt, W1)
        P.S("mul", out=negU[:], in_=pu[:], mul=-1.0)
        if RW_CUT == 9:
            continue
        pya = nxt()
        for h in range(NH):
            for d in range(2):
                u = 8 * d + h
                P.T("matmul", out=pya[64 * d:64 * d + 64, 64 * h:64 * h + 64], lhsT=rtT[:, h, 64 * d:64 * d + 64],
                    rhs=Qb[:, 64 * u:64 * u + 64], start=True, stop=True, R=[rtT, Qb], W=[pya])
        P.S("copy", out=W1a[:], in_=pya[:])
        pyb = nxt()
        for h in range(NH):
            for d in range(2):
                r0, c0 = 64 * d, 64 * h
                o = pyb[r0:r0 + 64, c0:c0 + 64]
                P.T("matmul", out=o, lhsT=BrkT_s[r0:r0 + 64, c0:c0 + 64], rhs=vb[r0:r0 + 64, c0:c0 + 64], start=True, stop=False,
                    R=[BrkT_s, vb], W=[pyb])
                P.T("matmul", out=o, lhsT=BrbT_s[r0:r0 + 64, c0:c0 + 64], rhs=negU[r0:r0 + 64, c0:c0 + 64], start=False, stop=True,
                    R=[BrbT_s, negU], W=[pyb])
        P.V("tensor_tensor", out=y_sb[:], in0=pyb[:], in1=W1a[:], op=ALU.add)
        P.dma("sync", yf_scr[64 * m0:64 * m0 + 64, :], y_sb[0:64, :])
        P.dma("sync", yr_scr[64 * m1:64 * m1 + 64, :], y_sb[64:128, :])
        if RW_CUT == 10:
            continue
        for d in range(2):
            pq = nxt()
            for h in range(NH):
                r0, c0 = 64 * d, 64 * h
                o = pq[0:64, c0:c0 + 64]
                P.T("matmul", out=o, lhsT=kt_b[r0:r0 + 64, c0:c0 + 64], rhs=vb[r0:r0 + 64, c0:c0 + 64], start=True, stop=False,
                    R=[kt_b, vb], W=[pq])
                P.T("matmul", out=o, lhsT=bt_b[r0:r0 + 64, c0:c0 + 64], rhs=negU[r0:r0 + 64, c0:c0 + 64], start=False, stop=True,
                    R=[bt_b, negU], W=[pq])
            P.V("tensor_tensor", out=tmpq[:, 512 * d:512 * d + 512], in0=pq[0:64, :], in1=Qf[:, 512 * d:512 * d + 512],
                op=ALU.add, W=[(tmpq, d)], R=[pq, Qf])
        P.V("tensor_tensor", out=Qf[:].rearrange("p (d h i) -> p d h i", d=2, h=NH), in0=tmpq[:].rearrange("p (d h i) -> p d h i", d=2, h=NH),
            in1=cC[:].rearrange("p (h d) -> p d h", d=2).unsqueeze(3).to_broadcast([64, 2, NH, 64]), op=ALU.mult)
        P.S("copy", out=Qb[:], in_=Qf[:])


def emit_globals(P, G, ident_d, identf_d):
    G.identB = P.sb("g_identB", [128, 128], BF16)
    G.identF = P.sb("g_identF", [128, 128], F32)
    G.epsb = P.sb("g_epsb", [128, 1], F32)
    P.dma("sync", G.identB[:], ident_d[:, :])
    P.dma("sync", G.identF[:], identf_d[:, :])
    P.G("memset", ap=G.epsb[:], constant=1e-6, R=[], W=[G.epsb])
    G.oneb = P.sb("g_oneb", [128, 1], F32)
    P.G("memset", ap=G.oneb[:], constant=1.0, R=[], W=[G.oneb])


def build_main(upto="all", dbg=(), rw_steps=132, n_exp=16, n_moe_layers=2, fuse_adaln=False):
    P = Prog(same_engine_sync=SAME_ENGINE_SYNC)
    G = Ctx()
    scr = lambda name, shape, dt=F32: P.dram(name, shape, dt, "out" if name in dbg else "tmp")
    xin_d = P.dram("xin", [T_TOK, 1024], F32, "in")
    if fuse_adaln:
        mod_d = scr("mods_scr", [2, 2, 6, 128, 1024])
        cT_d = P.dram("cT2", [128, 8, 2], F32, "in")
        G.selr_d = P.dram("selr", [2, 2, 128], F32, "in")
        adaw_d = P.dram("adaw", [2, 128, 8, 6144], F32, "in")
        adab_d = P.dram("adab", [2, 128, 6144], F32, "in")
        ng_d = P.dram("ng", [2, 128, 2, 1024], F32, "in")
    else:
        mod_d = P.dram("mods", [2, 2, 6, 128, 1024], F32, "in")
    ident_d = P.dram("identb", [128, 128], BF16, "in")
    identf_d = P.dram("identf", [128, 128], F32, "in")
    ew_in_d = P.dram("ev_w_in", [128, 8, 3328], F32, "in")
    rwc_d = P.dram("rwc", [128, 8, 512], F32, "in")
    wup_d = P.dram("wup", [64, 2, 2, 512], F32, "in")
    cumM_d = P.dram("cumM", [128, 128], F32, "in")
    sel_d = P.dram("sel", [128, 2], F32, "in")
    p_scr = scr("p_scr", [T_TOK, 3328])
    yf_scr = scr("yf_scr", [T_TOK, 512])
    yr_scr = scr("yr_scr", [T_TOK, 512])
    emit_globals(P, G, ident_d, identf_d)
    if fuse_adaln:
        with P.scope():
            stage_adaln(P, G, cT_d, adaw_d, adab_d, ng_d, mod_d)
    with P.scope():
        stage_pre(P, G, 3328, ew_in_d, mod_d, 0, xin_d, p_scr)
    if upto == "pre":
        return P.build(), P
    with P.scope():
        stage_rwkv(P, G, p_scr, yf_scr, yr_scr, rwc_d, wup_d, cumM_d, sel_d, nsteps=rw_steps)
    if upto == "rwkv":
        return P.build(), P
    evc_d = P.dram("evc", [128, 6, 512], F32, "in")
    gup_d = P.dram("g_up", [128, 512], F32, "in")
    ewout_d = P.dram("ev_w_out", [128, 8, 1024], F32, "in")
    router_d = P.dram("router", [2, 128, 8, 16], F32, "in")
    xmid_scr = scr("xmid_scr", [T_TOK, 1024])
    h2_scr = scr("h2_scr", [T_TOK, 1024], BF16)
    aff_scr = scr("aff_scr", [T_TOK, 16])
    with P.scope():
        stage_post(P, G, 0, "even", xin_d, p_scr, 3328, yf_scr, yr_scr, None, evc_d, gup_d, ewout_d, router_d[0], mod_d,
                   xmid_scr, h2_scr, aff_scr, list(range(NTILE)))
    if upto == "post0":
        return P.build(), P
    tric_d = P.dram("tric", [128, 2, 128], F32, "in")
    moe_w = [[P.dram(f"moe_{n}_{l}", [16, 128, k8, nn], F32, "in") for (n, k8, nn) in (("w1", 8, 1536), ("w3", 8, 1536), ("w2", 12, 1024))]
             for l in range(n_moe_layers)]
    xl_scr = [scr(f"xl_scr{e}", [1025, 1024], BF16) for e in range(16)]
    xc_scr = [scr(f"xc_scr{e}", [33, 1024], BF16) for e in range(16)]
    yl_scr = [scr(f"yl_scr{e}", [1025, 1024]) for e in range(16)]
    yc_scr = [scr(f"yc_scr{e}", [33, 1024]) for e in range(16)]
    x1_scr = scr("x1_scr", [T_TOK, 1024])
    with P.scope():
        stage_moe(P, G, 0, moe_w[0][0], moe_w[0][1], moe_w[0][2], mod_d, tric_d, xmid_scr, h2_scr, aff_scr, xl_scr, xc_scr, yl_scr, yc_scr,
                  x1_scr, None, True, n_exp=n_exp)
    if upto == "moe0":
        return P.build(), P
    ow_in_d = P.dram("od_w_in", [128, 8, ODD_N], F32, "in")
    owout_d = P.dram("od_w_out", [128, 8, 1024], F32, "in")
    glc_d = P.dram("glc", [128, 2, 256], F32, "in")
    aup_d = P.dram("aup", [16, 2, 256], F32, "in")
    rope_d = P.dram("rope", [T_TOK, 2, 64], F32, "in")
    qkg_d = P.dram("qkg", [128, 2, 512], F32, "in")
    natb_d = P.dram("natb", [8, 128, 8, 4, 64], F32, "in")
    glng_d = P.dram("glng", [128, 1, 512], F32, "in")
    out_d = P.dram("out", [8192, 1024], F32, "out")
    p1_scr = scr("p1_scr", [T_TOK, ODD_N])
    of_scr = scr("of_scr", [T_TOK, 512])
    or_scr = scr("or_scr", [T_TOK, 512])
    nat_scr = scr("nat_scr", [T_TOK, 512])
    qT_scr = scr("qT_scr", [8, 64, T_TOK], BF16)
    kT_scr = scr("kT_scr", [8, 64, T_TOK], BF16)
    va_scr = scr("va_scr", [T_TOK, 8, 65], BF16)
    xmid1_scr = scr("xmid1_scr", [T_TOK, 1024])
    with P.scope():
        stage_pre(P, G, ODD_N, ow_in_d, mod_d, 1, x1_scr, p1_scr)
    with P.scope():
        if "gla" in NO_REORDER:
            P.mark_no_reorder()
        stage_gla(P, G, p1_scr, of_scr, or_scr, glc_d, aup_d, rope_d, cumM_d, sel_d, nsteps=rw_steps)
    if upto == "gla":
        return P.build(), P
    with P.scope():
        stage_nat(P, G, p1_scr, nat_scr, qkg_d, natb_d, qT_scr, kT_scr, va_scr)
    if upto == "nat":
        return P.build(), P
    lat = list(range(2, NTILE))
    with P.scope():
        stage_post(P, G, 1, "odd", x1_scr, p1_scr, ODD_N, of_scr, or_scr, nat_scr, glng_d, None, owout_d, router_d[1], mod_d,
                   xmid1_scr, h2_scr, aff_scr, lat)
    if upto == "post1":
        return P.build(), P
    with P.scope():
        stage_moe(P, G, 1, moe_w[1][0], moe_w[1][1], moe_w[1][2], mod_d, tric_d, xmid1_scr, h2_scr, aff_scr, xl_scr, xc_scr, yl_scr, yc_scr,
                  out_d, 256, False, n_exp=n_exp)
    return P.build(), P


def mods_bcast(mods, b):
    m = np.stack([mods[:, b], mods[:, 4]], 1)
    return np.ascontiguousarray(np.broadcast_to(m[:, :, :, None, :], (2, 2, 6, 128, 1024)))


def main_inputs(inp, mods, b, n_moe_layers=2):
    d = dict(xin=np.ascontiguousarray(np.concatenate([inp["ctx"][b], inp["x"][b]], 0)),
             identb=IDENT_BF, identf=IDENT_F, ev_w_in=kmajor(inp["ev_w_in"][0]))
    if mods is not None:
        d["mods"] = mods_bcast(mods, b)
    else:
        c2 = np.stack([inp["c"][b], inp["c_ctx"]], 1)
        d["cT2"] = kmajor(np.ascontiguousarray(c2))
        d["selr"] = np.ascontiguousarray(np.broadcast_to(np.eye(2, dtype=np.float32)[:, :, None], (2, 2, 128)))
        d["adaw"] = np.stack([kmajor(inp["ada_w"][l]) for l in range(2)], 0)
        d["adab"] = np.stack([bc128(inp["ada_b"][l]) for l in range(2)], 0)
        d["ng"] = np.stack([bc128(np.stack([inp["norm1_g"][l], inp["norm2_g"][l]], 0)) for l in range(2)], 0)
    d.update(rwkv_consts(inp))
    d.update(post_consts_even(inp))
    d.update(odd_consts(inp))
    d["router"] = np.stack([kmajor(inp["moe_router"][l]) for l in range(2)], 0)
    tri = np.zeros((128, 2, 128), np.float32)
    tri[:, 0, :] = 1.0
    tri[:, 1, :] = (np.arange(128)[:, None] < np.arange(128)[None, :])
    d["tric"] = tri
    for l in range(n_moe_layers):
        d[f"moe_w1_{l}"] = np.ascontiguousarray(inp["moe_w1"][l].reshape(16, 8, 128, 1536).transpose(0, 2, 1, 3))
        d[f"moe_w3_{l}"] = np.ascontiguousarray(inp["moe_w3"][l].reshape(16, 8, 128, 1536).transpose(0, 2, 1, 3))
        d[f"moe_w2_{l}"] = np.ascontiguousarray(inp["moe_w2"][l].reshape(16, 12, 128, 1024).transpose(0, 2, 1, 3))
    return d


IDENT_BF = np.eye(128, dtype=np.float32).astype(BF)
IDENT_F = np.eye(128, dtype=np.float32)


def post_consts_even(inp):
    ev = np.stack([bc128(inp["conv_w"][0][0]), bc128(inp["conv_w"][0][1]), bc128(inp["conv_w"][0][2]),
                   bc128(inp["rw_r_k"][0].reshape(512)), bc128(inp["rw_ln_g"][0]), bc128(inp["rw_ln_b"][0])], 1)
    return dict(evc=np.ascontiguousarray(ev.astype(np.float32)), g_up=np.ascontiguousarray(inp["rw_g_up"][0]),
                ev_w_out=kmajor(inp["ev_w_out"][0]))


def stage_post(P, G, lyr, kind, x_src, p_scr, N, ya_scr, yb_scr, nat_scr, cst_d, gup_d, wout_d, router_d, mod_d,
               xmid_scr, h2_scr, aff_scr, tiles):
    pre = f"po{lyr}_"
    f32t = lambda n, shp=(128, 512): P.sb(pre + n, list(shp), F32)
    bft = lambda n, shp=(128, 512): P.sb(pre + n, list(shp), BF16)
    modS = f32t("modS", (128, 3, 1024))
    modC = f32t("modC", (128, 3, 1024))
    for dst, si in ((modS, 0), (modC, 1)):
        P.dma("sync", dst[:], mod_d[lyr, si, 2:5].rearrange("v p f -> p v f"))
    wout = bft("wout", (128, 8, 1024))
    stg = emit_weight_bf16(P, wout_d, wout, 8, 1024, pre + "w")
    router = f32t("router", (128, 8, 16))
    P.dma("sync", router[:], router_d[:, :, :])
    ncst = 6 if kind == "even" else 1
    cst = f32t("cst", (128, ncst, 512))
    P.dma("sync", cst[:], cst_d[:, :, :])
    if kind == "even":
        gup_f = f32t("gup_f", (128, 512))
        gup = bft("gup", (128, 512))
        P.dma("sync", gup_f[:], gup_d[:, :])
        P.V("tensor_copy", out=gup[:], in_=gup_f[:])
        cw0, cw1, cw2, rkb, lng, lnb = [cst[:, i, :] for i in range(6)]
    pt_ = [f32t(f"pt{i}", (128, N)) for i in range(2)]
    xts = [f32t(f"xt{i}", (128, 1024)) for i in range(2)]
    yas = [f32t(f"ya{i}") for i in range(2)]
    ybs = [f32t(f"yb{i}") for i in range(2)]
    if kind == "even":
        pu_, pg_, nu_, ng_ = f32t("pu"), f32t("pg"), f32t("nu"), f32t("ng")
    else:
        natt = [f32t(f"nat{i}") for i in range(2)]
    TS = [dict(a1=f32t(f"a1{i}"), a2=f32t(f"a2{i}"), a3=f32t(f"a3{i}"), a4=f32t(f"a4{i}"), s8=f32t(f"s8{i}", (128, 8)),
               m8=f32t(f"m8{i}", (128, 8)), v8=f32t(f"v8{i}", (128, 8)), sgx=bft(f"sgx{i}", (128, 128)), sgT=bft(f"sgT{i}", (128, 128)),
               cat=bft(f"cat{i}", (128, 1024)), catT=bft(f"catT{i}", (128, 8, 128)), xmid=f32t(f"xmid{i}", (128, 1024)),
               h2f=f32t(f"h2f{i}", (128, 1024)), h2b=bft(f"h2b{i}", (128, 1024)), h2T=f32t(f"h2T{i}", (128, 8, 128)),
               ss=f32t(f"ss{i}", (128, 1)), rs=f32t(f"rs{i}", (128, 1)), mx=f32t(f"mx{i}", (128, 1)), se=f32t(f"se{i}", (128, 1)),
               ex=f32t(f"ex{i}", (128, 16)), aff=f32t(f"aff{i}", (128, 16))) for i in range(2)]
    lneps = f32t("lneps", (128, 1))
    P.G("memset", ap=lneps[:], constant=(64e-5 if kind == "even" else 1e-6), R=[], W=[lneps])
    tmp, hf = f32t("tmp", (128, 1024)), f32t("hf", (128, 1024))
    pTb = P.ps(pre + "pTb", [128, 8, 128], BF16)
    pTf = [P.ps(pre + f"pTf{i}", [128, 4, 128]) for i in range(2)]
    pms = [P.ps(pre + f"pm{i}", [128, 512]) for i in range(3)]
    pl = P.ps(pre + "pl", [128, 16])
    pgT = P.ps(pre + "pgT", [128, 128], BF16)
    v3 = lambda ap, h: ap.rearrange("p (h j) -> p h j", h=h)
    for it, t in enumerate(tiles):
        r0 = t * 128
        first = t in (0, 2)
        last = t in (1, NTILE - 1)
        pt, xt, ya, yb = pt_[it % 2], xts[it % 2], yas[it % 2], ybs[it % 2]
        mod = modC if t < 2 else modS
        ts_ = TS[it % 2]
        a1, a2, a3, a4, s8, m8, v8, sgx, sgT, cat, catT, xmid = [ts_[k] for k in ("a1", "a2", "a3", "a4", "s8", "m8", "v8", "sgx", "sgT", "cat", "catT", "xmid")]
        h2f, h2b, h2T, ss, rs, mx, se, ex, aff = [ts_[k] for k in ("h2f", "h2b", "h2T", "ss", "rs", "mx", "se", "ex", "aff")]
        P.dma("sync", pt[:], p_scr[r0:r0 + 128, :])
        P.dma("sync", xt[:], x_src[r0:r0 + 128, :])
        P.dma("sync", ya[:], ya_scr[r0:r0 + 128, :])
        P.dma("sync", yb[:], yb_scr[r0:r0 + 128, :])
        if kind == "even":
            if first:
                P.V("memset", ap=pu_[:], constant=0.0, R=[], W=[pu_])
                P.V("memset", ap=pg_[:], constant=0.0, R=[], W=[pg_])
                P.dma("sync", pu_[1:128, :], p_scr[r0:r0 + 127, 0:512])
                P.dma("sync", pg_[1:128, :], p_scr[r0:r0 + 127, 1024:1536])
            else:
                P.dma("sync", pu_[:], p_scr[r0 - 1:r0 + 127, 0:512])
                P.dma("sync", pg_[:], p_scr[r0 - 1:r0 + 127, 1024:1536])
            if last:
                P.V("memset", ap=nu_[:], constant=0.0, R=[], W=[nu_])
                P.V("memset", ap=ng_[:], constant=0.0, R=[], W=[ng_])
                P.dma("sync", nu_[0:127, :], p_scr[r0 + 1:r0 + 128, 0:512])
                P.dma("sync", ng_[0:127, :], p_scr[r0 + 1:r0 + 128, 1024:1536])
            else:
                P.dma("sync", nu_[:], p_scr[r0 + 1:r0 + 129, 0:512])
                P.dma("sync", ng_[:], p_scr[r0 + 1:r0 + 129, 1024:1536])
            P.G("tensor_tensor", out=a1[:], in0=pu_[:], in1=pg_[:], op=ALU.mult)
            P.G("tensor_tensor", out=a1[:], in0=a1[:], in1=cw0, op=ALU.mult)
            P.V("tensor_tensor", out=a2[:], in0=pt[:, 0:512], in1=pt[:, 1024:1536], op=ALU.mult)
            P.V("tensor_tensor", out=a2[:], in0=a2[:], in1=cw1, op=ALU.mult)
            P.G("tensor_tensor", out=a3[:], in0=nu_[:], in1=ng_[:], op=ALU.mult)
            P.G("tensor_tensor", out=a3[:], in0=a3[:], in1=cw2, op=ALU.mult)
            P.V("tensor_tensor", out=a2[:], in0=a2[:], in1=a1[:], op=ALU.add)
            P.V("tensor_tensor", out=a2[:], in0=a2[:], in1=a3[:], op=ALU.add)
            P.V("tensor_tensor", out=cat[:, 0:512], in0=a2[:], in1=pt[:, 512:1024], op=ALU.mult, W=[(cat, 0)])
            r_, k_, v_, xg_ = pt[:, 1536:2048], pt[:, 2048:2560], pt[:, 2560:3072], pt[:, 3200:3328]
            P.V("tensor_tensor", out=a1[:], in0=ya[:], in1=yb[:], op=ALU.add)
            P.V("tensor_reduce", out=s8[:], in_=v3(a1[:], 8), axis=AX.X, op=ALU.add)
            P.V("tensor_scalar", out=m8[:], in0=s8[:], scalar1=-1.0 / 64, scalar2=None, op0=ALU.mult)
            P.V("tensor_tensor", out=v3(a2[:], 8), in0=v3(a1[:], 8), in1=m8[:].unsqueeze(2).to_broadcast([128, 8, 64]), op=ALU.add)
            P.G("tensor_tensor", out=a3[:], in0=a2[:], in1=a2[:], op=ALU.mult)
            P.V("tensor_reduce", out=v8[:], in_=v3(a3[:], 8), axis=AX.X, op=ALU.add)
            P.S("activation", out=v8[:], in_=v8[:], func=AF.Sqrt, scale=1.0 / 64, bias=lneps[:, 0:1])
            P.V("reciprocal", out=v8[:], in_=v8[:])
            P.V("tensor_tensor", out=v3(a2[:], 8), in0=v3(a2[:], 8), in1=v8[:].unsqueeze(2).to_broadcast([128, 8, 64]), op=ALU.mult)
            P.G("tensor_tensor", out=a2[:], in0=a2[:], in1=lng, op=ALU.mult)
            P.G("tensor_tensor", out=a2[:], in0=a2[:], in1=lnb, op=ALU.add)
            P.V("tensor_tensor", out=a4[:], in0=r_, in1=k_, op=ALU.mult)
            P.V("tensor_tensor", out=a4[:], in0=a4[:], in1=rkb, op=ALU.mult)
            P.V("tensor_reduce", out=s8[:], in_=v3(a4[:], 8), axis=AX.X, op=ALU.add)
            P.V("tensor_tensor", out=v3(a4[:], 8), in0=v3(v_, 8), in1=s8[:].unsqueeze(2).to_broadcast([128, 8, 64]), op=ALU.mult)
            P.V("tensor_tensor", out=a2[:], in0=a2[:], in1=a4[:], op=ALU.add)
            P.S("activation", out=sgx[:], in_=xg_, func=AF.Sigmoid)
            P.T("transpose", out=pgT[:], in_=sgx[:], identity=G.identB[:])
            P.S("copy", out=sgT[:], in_=pgT[:])
            pm = pms[2]
            P.T("matmul", out=pm[:], lhsT=sgT[:], rhs=gup[:], start=True, stop=True)
            P.V("tensor_tensor", out=cat[:, 512:1024], in0=a2[:], in1=pm[:], op=ALU.mult, W=[(cat, 1)])
        else:
            nt = natt[it % 2]
            P.dma("sync", nt[:], nat_scr[r0:r0 + 128, :])
            P.S("copy", out=cat[:, 0:512], in_=nt[:], W=[(cat, 0)])
            gr_ = pt[:, 2560:3072]
            P.V("tensor_tensor", out=a1[:], in0=ya[:], in1=yb[:], op=ALU.add)
            P.G("tensor_tensor", out=a3[:], in0=a1[:], in1=a1[:], op=ALU.mult)
            P.V("tensor_reduce", out=v8[:, 0:4], in_=v3(a3[:], 4), axis=AX.X, op=ALU.add)
            P.S("activation", out=v8[:, 0:4], in_=v8[:, 0:4], func=AF.Sqrt, scale=1.0 / 128, bias=lneps[:, 0:1])
            P.V("reciprocal", out=v8[:, 0:4], in_=v8[:, 0:4])
            P.V("tensor_tensor", out=v3(a2[:], 4), in0=v3(a1[:], 4), in1=v8[:, 0:4].unsqueeze(2).to_broadcast([128, 4, 128]), op=ALU.mult)
            P.G("tensor_tensor", out=a2[:], in0=a2[:], in1=cst[:, 0, :], op=ALU.mult)
            P.S("activation", out=a4[:], in_=gr_, func=AF.Silu)
            P.V("tensor_tensor", out=cat[:, 512:1024], in0=a2[:], in1=a4[:], op=ALU.mult, W=[(cat, 1)])
        for k in range(8):
            P.T("transpose", out=pTb[:, k, :], in_=cat[:, k * 128:(k + 1) * 128], identity=G.identB[:], R=[cat])
        P.S("copy", out=catT[:], in_=pTb[:])
        for nb in range(2):
            pm = pms[nb]
            for k in range(8):
                P.T("matmul", out=pm[:], lhsT=catT[:, k, :], rhs=wout[:, k, nb * 512:(nb + 1) * 512], start=(k == 0), stop=(k == 7),
                    R=[catT, (wout, k)])
            P.V("tensor_tensor", out=xmid[:, nb * 512:(nb + 1) * 512], in0=pm[:], in1=mod[:, 0, nb * 512:(nb + 1) * 512], op=ALU.mult,
                W=[(xmid, nb)])
        P.G("tensor_tensor", out=xmid[:], in0=xmid[:], in1=xt[:], op=ALU.add)
        P.dma("sync", xmid_scr[r0:r0 + 128, :], xmid[:])
        emit_norm_mod(P, xmid, mod[:, 2, :], mod[:, 1, :], h2b, tmp, ss, rs, G.epsb, hf, hf_out=h2f)
        P.dma("sync", h2_scr[r0:r0 + 128, :], h2b[:])
        for k in range(8):
            P.T("transpose", out=pTf[k // 4][:, k % 4, :], in_=h2f[:, k * 128:(k + 1) * 128], identity=G.identF[:])
        P.S("copy", out=h2T[:, 0:4, :], in_=pTf[0][:], W=[(h2T, 0)])
        P.V("tensor_copy", out=h2T[:, 4:8, :], in_=pTf[1][:], W=[(h2T, 1)])
        for k in range(8):
            P.T("matmul", out=pl[:], lhsT=h2T[:, k, :], rhs=router[:, k, :], start=(k == 0), stop=(k == 7), R=[h2T, router])
        P.V("tensor_reduce", out=mx[:], in_=pl[:], axis=AX.X, op=ALU.max)
        P.V("tensor_scalar", out=mx[:], in0=mx[:], scalar1=-1.0, scalar2=None, op0=ALU.mult)
        P.S("activation", out=ex[:], in_=pl[:], func=AF.Exp, bias=mx[:, 0:1], accum_out=se[:])
        P.V("reciprocal", out=se[:], in_=se[:])
        P.V("tensor_scalar", out=aff[:], in0=ex[:], scalar1=se[:, 0:1], scalar2=None, op0=ALU.mult)
        P.dma("sync", aff_scr[r0:r0 + 128, :], aff[:])


def stage_moe(P, G, lyr, w1_d, w3_d, w2_d, mod_d, tric_d, xmid_scr, h2_scr, aff_scr, xl_scr, xc_scr, yl_scr, yc_scr,
              out_ap, out_row0, with_ctx, n_exp=16, n_iter=30):
    pre = f"mo{lyr}_"
    f32t = lambda n, shp: P.sb(pre + n, list(shp), F32)
    bft = lambda n, shp: P.sb(pre + n, list(shp), BF16)
    NE = 16
    NU = 32
    posIL = P.sb(pre + "posIL", [128, 16, 64], I32)
    posIC = P.sb(pre + "posIC", [128, 16, 2], I32)
    gmL = f32t("gmL", (128, 16, 64))
    gmC = f32t("gmC", (128, 16, 2))
    with P.scope():
        tric = f32t("tric", (128, 2, 128))
        P.dma("sync", tric[:], tric_d[:, :, :])
        onesF, triS = tric[:, 0, :], tric[:, 1, :]
        affL = f32t("affL", (128, 64, 16))
        affC = f32t("affC", (128, 2, 16))
        for q in range(4):
            P.dma("sync", affL[:, 16 * q:16 * q + 16, :],
                  aff_scr[256 + 2048 * q:256 + 2048 * (q + 1), :].rearrange("(n p) e -> p n e", p=128), W=[(affL, q)])
        P.dma("sync", affC[:], aff_scr[0:256, :].rearrange("(n p) e -> p n e", p=128))
        AL = f32t("AL", (128, 16, 64))
        AC = f32t("AC", (128, 16, 2))
        P.V("tensor_copy", out=AL[:], in_=affL[:].rearrange("p n e -> p e n"))
        P.V("tensor_copy", out=AC[:], in_=affC[:].rearrange("p n e -> p e n"))
        cmpL = f32t("cmpL", (128, 16, 64))
        cmpC = f32t("cmpC", (128, 16, 2))
        lo, hi, mid, cnt, ge, dl, kt = [f32t(n, (128, NU)) for n in ("lo", "hi", "mid", "cnt", "ge", "dl", "kt")]
        P.V("memset", ap=lo[:], constant=0.0, R=[], W=[lo])
        P.V("memset", ap=hi[:], constant=1.0, R=[], W=[hi])
        P.V("memset", ap=kt[:, 0:16], constant=1024.0, R=[], W=[(kt, 0)])
        P.V("memset", ap=kt[:, 16:32], constant=32.0, R=[], W=[(kt, 1)])
        ptot = P.ps(pre + "ptot", [128, NU])
        for it in range(n_iter):
            P.V("tensor_tensor", out=dl[:], in0=hi[:], in1=lo[:], op=ALU.subtract)
            P.V("scalar_tensor_tensor", out=mid[:], in0=dl[:], scalar=0.5, in1=lo[:], op0=ALU.mult, op1=ALU.add)
            P.V("tensor_tensor", out=cmpL[:], in0=AL[:], in1=mid[:, 0:16].unsqueeze(2).to_broadcast([128, 16, 64]), op=ALU.is_ge)
            P.V("tensor_reduce", out=cnt[:, 0:16], in_=cmpL[:], axis=AX.X, op=ALU.add, W=[(cnt, 0)])
            P.V("tensor_tensor", out=cmpC[:], in0=AC[:], in1=mid[:, 16:32].unsqueeze(2).to_broadcast([128, 16, 2]), op=ALU.is_ge)
            P.V("tensor_reduce", out=cnt[:, 16:32], in_=cmpC[:], axis=AX.X, op=ALU.add, W=[(cnt, 1)])
            P.T("matmul", out=ptot[:], lhsT=onesF, rhs=cnt[:], start=True, stop=True, R=[tric, cnt])
            P.V("tensor_tensor", out=ge[:], in0=ptot[:], in1=kt[:], op=ALU.is_ge)
            P.V("tensor_tensor", out=dl[:], in0=mid[:], in1=lo[:], op=ALU.subtract)
            P.V("tensor_tensor", out=dl[:], in0=dl[:], in1=ge[:], op=ALU.mult)
            P.V("tensor_tensor", out=lo[:], in0=lo[:], in1=dl[:], op=ALU.add)
            P.V("tensor_tensor", out=dl[:], in0=hi[:], in1=mid[:], op=ALU.subtract)
            P.V("tensor_tensor", out=dl[:], in0=dl[:], in1=ge[:], op=ALU.mult)
            P.V("tensor_tensor", out=hi[:], in0=mid[:], in1=dl[:], op=ALU.add)
        ones64 = f32t("ones64", (128, 64))
        P.V("memset", ap=ones64[:], constant=1.0, R=[], W=[ones64])
        pp = [P.ps(pre + f"pp{i}", [128, 512]) for i in range(2)]
        for (A, cm, th, ncol, posI, gm, sfx, KK) in ((AL, cmpL, lo[:, 0:16], 64, posIL, gmL, "L", 1024.0), (AC, cmpC, lo[:, 16:32], 2, posIC, gmC, "C", 32.0)):
            W = 16 * ncol
            ppT, ctT, csT = [f32t(n + sfx, (128, 16, ncol)) for n in ("ppT", "ctT", "csT")]
            flat = lambda tl: tl[:].rearrange("p e n -> p (e n)")
            P.V("tensor_tensor", out=cm[:], in0=A[:], in1=th.unsqueeze(2).to_broadcast([128, 16, ncol]), op=ALU.is_ge, R=[A, lo])
            P.V("tensor_tensor", out=gm[:], in0=A[:], in1=cm[:], op=ALU.mult)
            for (lhs, dst) in ((triS, ppT), (onesF, ctT)):
                for c0 in range(0, W, 512):
                    c1 = min(W, c0 + 512)
                    pq = pp[(c0 // 512) % 2]
                    P.T("matmul", out=pq[:, 0:c1 - c0], lhsT=lhs, rhs=flat(cm)[:, c0:c1], start=True, stop=True, R=[tric, cm])
                    P.S("copy", out=flat(dst)[:, c0:c1], in_=pq[:, 0:c1 - c0], W=[(dst, c0)])
            for e in range(16):
                P.V("tensor_tensor_scan", out=csT[:, e, :], data0=ones64[:, 0:ncol], data1=ctT[:, e, :], initial=0.0,
                    op0=ALU.mult, op1=ALU.add, R=[ctT, ones64], W=[(csT, e)])
            P.V("tensor_tensor", out=ppT[:], in0=ppT[:], in1=csT[:], op=ALU.add)
            P.V("tensor_tensor", out=ppT[:], in0=ppT[:], in1=ctT[:], op=ALU.subtract)
            P.V("scalar_tensor_tensor", out=ppT[:], in0=ppT[:], scalar=-KK, in1=cm[:], op0=ALU.add, op1=ALU.mult)
            P.V("tensor_scalar", out=ppT[:], in0=ppT[:], scalar1=KK, scalar2=KK, op0=ALU.add, op1=ALU.min)
            P.V("tensor_copy", out=posI[:], in_=ppT[:])
    lat_tiles = list(range(2, NTILE))
    ctx_tiles = [0, 1] if with_ctx else []
    with P.scope():
        h2ts = [bft(f"h2t{i}", (128, 1024)) for i in range(3)]
        for i, t in enumerate(ctx_tiles + lat_tiles):
            h2t = h2ts[i % 3]
            P.dma("sync", h2t[:], h2_scr[t * 128:(t + 1) * 128, :])
            for e in range(n_exp):
                if t < 2:
                    P.idma(R=[h2t, posIC], W=[xc_scr[e]], out=xc_scr[e][:, :], out_offset=bass.IndirectOffsetOnAxis(ap=posIC[:, e, t:t + 1], axis=0),
                           in_=h2t[:, :], in_offset=None)
                else:
                    P.idma(R=[h2t, posIL], W=[xl_scr[e]], out=xl_scr[e][:, :], out_offset=bass.IndirectOffsetOnAxis(ap=posIL[:, e, t - 2:t - 1], axis=0),
                           in_=h2t[:, :], in_offset=None)
    with P.scope():
        w1b, w3b = bft("w1b", (128, 8, 1536)), bft("w3b", (128, 8, 1536))
        w2b = bft("w2b", (128, 12, 1024))
        stg = [f32t(f"stg{i}", (128, 1536)) for i in range(4)]
        xins = [bft(f"xin{i}", (128, 1024)) for i in range(2)]
        NR = 1024 + (32 if with_ctx else 0)
        xT = bft("xT", (128, 8, NR))
        hidT = bft("hidT", (128, 12, NR))
        sl = [f32t(f"sl{i}", (128, 512)) for i in range(2)]
        yts = [f32t(f"yt{i}", (128, 1024)) for i in range(2)]
        pT = P.ps(pre + "pT", [128, 8, 128], BF16)
        ph = [P.ps(pre + f"ph{i}", [128, 512]) for i in range(4)]
        cast_i = [0]
        ev = 0

        def load_w(e, which):
            for (wd, wb, K8, N) in which:
                for k in range(K8):
                    s_ = stg[cast_i[0] % 4]
                    cast_i[0] += 1
                    P.dma("sync", s_[:, 0:N], wd[e, :, k, :])
                    P.G("tensor_copy", out=wb[:, k, :], in_=s_[:, 0:N], W=[(wb, k)])

        W13 = ((w1_d, w1b, 8, 1536), (w3_d, w3b, 8, 1536))
        W2 = ((w2_d, w2b, 12, 1024),)
        load_w(0, W13)
        blocks = [(xl_scr, yl_scr, b0 * 512, 512, b0 * 512) for b0 in range(2)] + ([(xc_scr, yc_scr, 0, 32, 1024)] if with_ctx else [])
        for e in range(n_exp):
            load_w(e, W2)
            ti = 0
            for (xs, ys, row0, nrow, col0) in blocks:
                for ci in range((nrow + 127) // 128):
                    rows = min(128, nrow - ci * 128)
                    xin = xins[ti % 2]
                    ti += 1
                    P.dma("sync", xin[0:rows, :], xs[e][row0 + ci * 128:row0 + ci * 128 + rows, :])
                    for k in range(8):
                        P.T("transpose", out=pT[:, k, 0:rows], in_=xin[0:rows, k * 128:(k + 1) * 128], identity=G.identB[0:rows, 0:rows])
                    c_ = col0 + ci * 128
                    P.S("copy", out=xT[:, :, c_:c_ + rows], in_=pT[:, :, 0:rows], W=[(xT, c_)])
            for (xs, ys, row0, nrow, col0) in blocks:
                for fc in range(12):
                    p1, p3 = ph[(2 * fc) % 4], ph[(2 * fc + 1) % 4]
                    for (pp_, wb) in ((p1, w1b), (p3, w3b)):
                        for k in range(8):
                            P.T("matmul", out=pp_[:, 0:nrow], lhsT=wb[:, k, fc * 128:(fc + 1) * 128], rhs=xT[:, k, col0:col0 + nrow],
                                start=(k == 0), stop=(k == 7), R=[xT, (wb, k)])
                    s_ = sl[fc % 2]
                    P.S("activation", out=s_[:, 0:nrow], in_=p1[:, 0:nrow], func=AF.Silu)
                    P.V("tensor_tensor", out=hidT[:, fc, col0:col0 + nrow], in0=s_[:, 0:nrow], in1=p3[:, 0:nrow], op=ALU.mult,
                        W=[(hidT, (fc, col0))])
            if e + 1 < n_exp:
                load_w(e + 1, W13)
            for (xs, ys, row0, nrow, col0) in blocks:
                for ci in range((nrow + 127) // 128):
                    rows = min(128, nrow - ci * 128)
                    yt = yts[ci % 2]
                    c_ = col0 + ci * 128
                    for db in range(2):
                        py = ph[ev % 4]
                        for fc in range(12):
                            P.T("matmul", out=py[0:rows, :], lhsT=hidT[:, fc, c_:c_ + rows], rhs=w2b[:, fc, db * 512:(db + 1) * 512],
                                start=(fc == 0), stop=(fc == 11), R=[hidT, (w2b, fc)])
                        if ev % 2 == 0:
                            P.S("copy", out=yt[0:rows, db * 512:(db + 1) * 512], in_=py[0:rows, :], W=[(yt, db)])
                        else:
                            P.V("tensor_copy", out=yt[0:rows, db * 512:(db + 1) * 512], in_=py[0:rows, :], W=[(yt, db)])
                        ev += 1
                    P.dma("sync", ys[e][row0 + ci * 128:row0 + ci * 128 + rows, :], yt[0:rows, :])
    with P.scope():
        gt2S = f32t("gt2S", (128, 1024))
        gt2C = f32t("gt2C", (128, 1024))
        P.dma("sync", gt2S[:], mod_d[lyr, 0, 5])
        P.dma("sync", gt2C[:], mod_d[lyr, 1, 5])
        ygs = [f32t(f"yg{i}", (128, 1024)) for i in range(4)]
        for y_ in ygs:
            P.V("memset", ap=y_[:], constant=0.0, R=[], W=[y_])
        for e in range(n_exp):
            P.dma("sync", yl_scr[e][1024:1025, :], ygs[0][0:1, :])
            if with_ctx:
                P.dma("sync", yc_scr[e][32:33, :], ygs[0][0:1, :])
        accs = [f32t(f"acc{i}", (128, 1024)) for i in range(2)]
        xms = [f32t(f"xm{i}", (128, 1024)) for i in range(2)]
        gi = 0
        for i, t in enumerate(ctx_tiles + lat_tiles):
            acc, xm = accs[i % 2], xms[i % 2]
            P.dma("sync", xm[:], xmid_scr[t * 128:(t + 1) * 128, :])
            for e in range(n_exp):
                yg = ygs[gi % 4]
                gi += 1
                if t < 2:
                    P.idma(R=[yc_scr[e], posIC], W=[yg], out=yg[:, :], out_offset=None, in_=yc_scr[e][:, :],
                           in_offset=bass.IndirectOffsetOnAxis(ap=posIC[:, e, t:t + 1], axis=0))
                    gcol = gmC[:, e, t:t + 1]
                else:
                    P.idma(R=[yl_scr[e], posIL], W=[yg], out=yg[:, :], out_offset=None, in_=yl_scr[e][:, :],
                           in_offset=bass.IndirectOffsetOnAxis(ap=posIL[:, e, t - 2:t - 1], axis=0))
                    gcol = gmL[:, e, t - 2:t - 1]
                if e == 0:
                    P.V("tensor_scalar", out=acc[:], in0=yg[:], scalar1=gcol, scalar2=None, op0=ALU.mult)
                else:
                    P.V("scalar_tensor_tensor", out=acc[:], in0=yg[:], scalar=gcol, in1=acc[:], op0=ALU.mult, op1=ALU.add)
            P.G("tensor_tensor", out=acc[:], in0=acc[:], in1=(gt2C if t < 2 else gt2S)[:], op=ALU.mult)
            P.G("tensor_tensor", out=acc[:], in0=acc[:], in1=xm[:], op=ALU.add)
            if t < 2 or out_row0 is None:
                P.dma("sync", out_ap[t * 128:(t + 1) * 128, :], acc[:])
            else:
                P.dma("sync", out_ap[t * 128 - out_row0:(t + 1) * 128 - out_row0, :], acc[:])


ODD_N = 3088


def odd_consts(inp):
    s = np.arange(64)
    earlyeq = (s[:, None] <= s[None, :]).astype(np.float32)
    MB = np.tile(np.concatenate([earlyeq, earlyeq.T], 0), (1, 4))
    ab = inp["gla_a_b"][0]
    abb = np.concatenate([np.broadcast_to(ab[0][None], (64, 256)), np.broadcast_to(ab[1][None], (64, 256))], 0)
    glc = np.stack([MB, abb], 1).astype(np.float32)
    aup = np.ascontiguousarray(inp["gla_a_up"][0].transpose(1, 0, 2)).astype(np.float32)
    nf = 16
    inv = (10000.0 ** (-np.arange(nf, dtype=np.float32) / nf)).astype(np.float32)
    t = np.arange(8192)
    ar = (t // 64).astype(np.float32)[:, None] * inv[None, :]
    ac = (t % 64).astype(np.float32)[:, None] * inv[None, :]
    cr, sr, cc, sc = np.cos(ar), np.sin(ar), np.cos(ac), np.sin(ac)
    C = np.concatenate([cr, cr, cc, cc], 1)
    S = np.concatenate([-sr, sr, -sc, sc], 1)
    Cf = np.concatenate([np.ones((256, 64), np.float32), C.astype(np.float32)], 0)
    Sf = np.concatenate([np.zeros((256, 64), np.float32), S.astype(np.float32)], 0)
    rope = np.ascontiguousarray(np.stack([Cf, Sf], 1))
    qkg = np.stack([bc128(np.tile(inp["nat_qn_g"][0], 8)), bc128(np.tile(inp["nat_kn_g"][0], 8))], 1).astype(np.float32)
    rpb = inp["nat_rpb"][0]
    kidx = np.arange(512)
    ro, cp = kidx // 64, kidx % 64
    q = np.arange(64)
    ws = np.clip(q - 8, 0, 48)
    valid = (cp[:, None] >= ws[None, :]) & (cp[:, None] < ws[None, :] + 16)
    cb = np.clip(cp[:, None] - q[None, :] + 15, 0, 30)
    bias = np.full((8, 8, 512, 64), -30000.0, np.float32)
    for pat in range(8):
        vals = rpb[:, (ro + pat)[:, None], cb]
        bias[:, pat] = np.where(valid[None], vals, -30000.0)
    natb = np.ascontiguousarray(bias.reshape(8, 8, 4, 128, 64).transpose(0, 3, 1, 2, 4))
    return dict(glc=np.ascontiguousarray(glc), aup=aup, rope=rope, qkg=np.ascontiguousarray(qkg), natb=natb,
                glng=bc128(np.tile(inp["gla_ln_g"][0], 4)).reshape(128, 1, 512).astype(np.float32),
                od_w_in=kmajor(inp["od_w_in"][0]), od_w_out=kmajor(inp["od_w_out"][0]))


def stage_gla(P, G, p_scr, of_scr, or_scr, glc_d, aup_d, rope_d, cumM_d, sel_d, nsteps=132):
    NH = 4
    f32t = lambda n, shp=(128, 512): P.sb("gl_" + n, list(shp), F32)
    bft = lambda n, shp=(128, 512): P.sb("gl_" + n, list(shp), BF16)
    glc = f32t("glc", (128, 2, 256))
    aup = f32t("aup", (16, 2, 256))
    cumM = f32t("cumM", (128, 128))
    sel = f32t("sel", (128, 2))
    P.dma("sync", glc[:], glc_d[:, :, :])
    P.dma("sync", aup[:], aup_d[:, :, :])
    P.dma("sync", cumM[:], cumM_d[:, :])
    P.dma("sync", sel[:], sel_d[:, :])
    MB, abb = glc[:, 0, :], glc[:, 1, :]
    qkvs = [f32t(f"qkv{i}", (128, 1024)) for i in range(2)]
    gas = [f32t(f"ga{i}", (128, 16)) for i in range(2)]
    ropes = [f32t(f"rope{i}", (128, 2, 64)) for i in range(2)]
    xs, xr, xr2 = f32t("xs"), f32t("xr"), f32t("xr2")
    gaT = f32t("gaT", (16, 128))
    xg, ex, sp = f32t("xg", (128, 256)), f32t("ex", (128, 256)), f32t("sp", (128, 256))
    E, Einv = f32t("E", (128, 256)), f32t("Einv", (128, 256))
    qt_t, kt_b = bft("qt_t", (128, 256)), bft("kt_b", (128, 256))
    vb = bft("vb")
    qT, kT = bft("qT", (64, 4, 128)), bft("kT", (64, 4, 128))
    Bqk = bft("Bqk", (128, 256))
    ya, y_sb = f32t("ya"), f32t("y_sb")
    Qf, tmpq = f32t("Qf", (64, 1024)), f32t("tmpq", (64, 1024))
    Qb = bft("Qb", (64, 1024))
    cC = f32t("cC", (64, 8))
    P.V("memset", ap=Qf[:], constant=0.0, R=[], W=[Qf])
    P.V("memset", ap=Qb[:], constant=0.0, R=[], W=[Qb])
    pf = [P.ps(f"glpf{i}", [128, 512]) for i in range(6)]
    pb = [P.ps(f"glpb{i}", [64, 4, 128], BF16) for i in range(2)]
    pfi = [0]

    def nxt():
        t = pf[pfi[0] % 6]
        pfi[0] += 1
        return t

    for n in range(nsteps):
        m0 = n
        m1 = (3 - n) if n < 4 else (135 - n)
        qkv, ga, rope = qkvs[n % 2], gas[n % 2], ropes[n % 2]
        for d, m in ((0, m0), (1, m1)):
            P.dma("sync", qkv[64 * d:64 * d + 64, :], p_scr[64 * m:64 * m + 64, 1536:2560], W=[(qkv, d)])
            P.dma("sync", ga[64 * d:64 * d + 64, :], p_scr[64 * m:64 * m + 64, 3072:3088], W=[(ga, d)])
            P.dma("sync", rope[64 * d:64 * d + 64, :, :], rope_d[64 * m:64 * m + 64, :, :], W=[(rope, d)])
        x4 = qkv[:, 0:512].rearrange("p (a two c) -> p a two c", two=2, c=16)
        s4 = xs[:].rearrange("p (a two c) -> p a two c", two=2, c=16)
        P.V("tensor_copy", out=s4[:, :, 0, :], in_=x4[:, :, 1, :], R=[qkv], W=[(xs, 0)])
        P.G("tensor_copy", out=s4[:, :, 1, :], in_=x4[:, :, 0, :], R=[qkv], W=[(xs, 1)])
        v8 = lambda ap: ap.rearrange("p (h j) -> p h j", h=8)
        Cb = rope[:, 0, :].unsqueeze(1).to_broadcast([128, 8, 64])
        Sb = rope[:, 1, :].unsqueeze(1).to_broadcast([128, 8, 64])
        P.V("tensor_tensor", out=v8(xr[:]), in0=v8(qkv[:, 0:512]), in1=Cb, op=ALU.mult, R=[qkv, rope], W=[xr])
        P.V("tensor_tensor", out=v8(xr2[:]), in0=v8(xs[:]), in1=Sb, op=ALU.mult, R=[xs, rope], W=[xr2])
        P.G("tensor_tensor", out=xr[:], in0=xr[:], in1=xr2[:], op=ALU.add)
        pt = nxt()
        P.T("transpose", out=pt[0:16, 0:128], in_=ga[:], identity=G.identF[:])
        P.V("tensor_copy", out=gaT[:], in_=pt[0:16, 0:128])
        pg = nxt()
        for d in range(2):
            P.T("matmul", out=pg[64 * d:64 * d + 64, 0:256], lhsT=gaT[:, 64 * d:64 * d + 64], rhs=aup[:, d, :], start=True, stop=True)
        P.V("tensor_tensor", out=xg[:], in0=pg[:, 0:256], in1=abb, op=ALU.add, R=[pg, glc])
        P.S("activation", out=ex[:], in_=xg[:], func=AF.Exp, scale=-1.0)
        P.S("activation", out=sp[:], in_=ex[:], func=AF.Ln, bias=G.oneb[:, 0:1])
        pL = nxt()
        P.T("matmul", out=pL[:, 0:256], lhsT=cumM[:], rhs=sp[:], start=True, stop=True)
        P.S("activation", out=E[:], in_=pL[:, 0:256], func=AF.Exp, scale=-1.0 / 16)
        P.S("activation", out=Einv[:], in_=pL[:, 0:256], func=AF.Exp, scale=1.0 / 16)
        P.V("scalar_tensor_tensor", out=qt_t[:], in0=xr[:, 0:256], scalar=0.125, in1=E[:], op0=ALU.mult, op1=ALU.mult)
        P.G("tensor_tensor", out=kt_b[:], in0=xr[:, 256:512], in1=Einv[:], op=ALU.mult)
        P.S("copy", out=vb[:], in_=qkv[:, 512:1024], R=[qkv])
        pc = nxt()
        for h in range(NH):
            P.T("matmul", out=pc[0:64, 2 * h:2 * h + 2], lhsT=E[:, 64 * h:64 * h + 64], rhs=sel[:], start=True, stop=True)
        P.V("tensor_copy", out=cC[:], in_=pc[0:64, 0:8])
        for qi, (src, dst) in enumerate(((qt_t, qT), (kt_b, kT))):
            pbt = pb[qi]
            for h in range(NH):
                P.T("transpose", out=pbt[:, h, :], in_=src[:, 64 * h:64 * h + 64], identity=G.identB[:])
            if qi == 0:
                P.S("copy", out=dst[:], in_=pbt[:])
            else:
                P.V("tensor_copy", out=dst[:], in_=pbt[:])
        p1 = nxt()
        for h in range(NH):
            for d in range(2):
                P.T("matmul", out=p1[64 * d:64 * d + 64, 64 * h:64 * h + 64], lhsT=kT[:, h, 64 * d:64 * d + 64],
                    rhs=qT[:, h, 64 * d:64 * d + 64], start=True, stop=True, R=[kT, qT], W=[p1])
        P.V("tensor_tensor", out=Bqk[:], in0=p1[:, 0:256], in1=MB, op=ALU.mult, R=[p1, glc])
        pya, pyb = nxt(), nxt()
        for h in range(NH):
            for d in range(2):
                u = 4 * d + h
                P.T("matmul", out=pya[64 * d:64 * d + 64, 128 * h:128 * h + 128], lhsT=qT[:, h, 64 * d:64 * d + 64],
                    rhs=Qb[:, 128 * u:128 * u + 128], start=True, stop=True, R=[qT, Qb], W=[pya])
                P.T("matmul", out=pyb[64 * d:64 * d + 64, 128 * h:128 * h + 128], lhsT=Bqk[64 * d:64 * d + 64, 64 * h:64 * h + 64],
                    rhs=vb[64 * d:64 * d + 64, 128 * h:128 * h + 128], start=True, stop=True, R=[Bqk, vb], W=[pyb])
        P.S("copy", out=ya[:], in_=pya[:])
        P.V("tensor_tensor", out=y_sb[:], in0=pyb[:], in1=ya[:], op=ALU.add)
        P.dma("sync", of_scr[64 * m0:64 * m0 + 64, :], y_sb[0:64, :])
        P.dma("sync", or_scr[64 * m1:64 * m1 + 64, :], y_sb[64:128, :])
        for d in range(2):
            pq = nxt()
            for h in range(NH):
                P.T("matmul", out=pq[0:64, 128 * h:128 * h + 128], lhsT=kt_b[64 * d:64 * d + 64, 64 * h:64 * h + 64],
                    rhs=vb[64 * d:64 * d + 64, 128 * h:128 * h + 128], start=True, stop=True, R=[kt_b, vb], W=[pq])
            P.V("tensor_tensor", out=tmpq[:, 512 * d:512 * d + 512], in0=pq[0:64, :], in1=Qf[:, 512 * d:512 * d + 512],
                op=ALU.add, W=[(tmpq, d)], R=[pq, Qf])
        P.V("tensor_tensor", out=Qf[:].rearrange("p (d h i) -> p d h i", d=2, h=NH), in0=tmpq[:].rearrange("p (d h i) -> p d h i", d=2, h=NH),
            in1=cC[:].rearrange("p (h d) -> p d h", d=2).unsqueeze(3).to_broadcast([64, 2, NH, 128]), op=ALU.mult)
        P.S("copy", out=Qb[:], in_=Qf[:])


def stage_nat(P, G, p_scr, nat_scr, qkg_d, natb_d, qT_scr, kT_scr, va_scr):
    f32t = lambda n, shp: P.sb("na_" + n, list(shp), F32)
    bft = lambda n, shp: P.sb("na_" + n, list(shp), BF16)
    with P.scope():
        if "nat" in NO_REORDER or "natprep" in NO_REORDER:
            P.mark_no_reorder()
        qkg = f32t("qkg", (128, 2, 512))
        P.dma("sync", qkg[:], qkg_d[:, :, :])
        pts = [f32t(f"pt{i}", (128, 1536)) for i in range(2)]
        sq = f32t("sq", (128, 1024))
        ms = f32t("ms", (128, 16))
        qkn = bft("qkn", (128, 1024))
        qkT = [bft(f"qkT{i}", (64, 16, 128)) for i in range(2)]
        vas = [bft(f"va{i}", (128, 8, 65)) for i in range(2)]
        for v_ in vas:
            P.V("memset", ap=v_[:], constant=1.0, R=[], W=[v_])
        pTs = [P.ps(f"na_pT{i}", [64, 8, 128], BF16) for i in range(2)]
        for t in range(NTILE):
            pt, va, qT_ = pts[t % 2], vas[t % 2], qkT[t % 2]
            P.dma("sync", pt[:], p_scr[t * 128:(t + 1) * 128, 0:1536])
            P.G("tensor_tensor", out=sq[:], in0=pt[:, 0:1024], in1=pt[:, 0:1024], op=ALU.mult)
            P.V("tensor_reduce", out=ms[:], in_=sq[:].rearrange("p (h j) -> p h j", h=16), axis=AX.X, op=ALU.add)
            P.S("activation", out=ms[:], in_=ms[:], func=AF.Sqrt, scale=1.0 / 64, bias=G.epsb[:, 0:1])
            P.V("reciprocal", out=ms[:], in_=ms[:])
            P.V("tensor_tensor", out=sq[:].rearrange("p (h j) -> p h j", h=16), in0=pt[:, 0:1024].rearrange("p (h j) -> p h j", h=16),
                in1=ms[:].unsqueeze(2).to_broadcast([128, 16, 64]), op=ALU.mult)
            P.V("scalar_tensor_tensor", out=qkn[:, 0:512], in0=sq[:, 0:512], scalar=0.125, in1=qkg[:, 0, :], op0=ALU.mult, op1=ALU.mult,
                W=[(qkn, 0)])
            P.G("tensor_tensor", out=qkn[:, 512:1024], in0=sq[:, 512:1024], in1=qkg[:, 1, :], op=ALU.mult, W=[(qkn, 1)])
            for half in range(2):
                for h in range(8):
                    c = half * 8 + h
                    P.T("transpose", out=pTs[half][:, h, :], in_=qkn[:, 64 * c:64 * c + 64], identity=G.identB[:], R=[qkn])
            P.S("copy", out=qT_[:, 0:8, :], in_=pTs[0][:], W=[(qT_, 0)])
            P.V("tensor_copy", out=qT_[:, 8:16, :], in_=pTs[1][:], W=[(qT_, 1)])
            P.dma("sync", qT_scr[:, :, t * 128:(t + 1) * 128].rearrange("h j t -> j h t"), qT_[:, 0:8, :])
            P.dma("sync", kT_scr[:, :, t * 128:(t + 1) * 128].rearrange("h j t -> j h t"), qT_[:, 8:16, :])
            P.S("copy", out=va[:, :, 0:64], in_=pt[:, 1024:1536].rearrange("p (h j) -> p h j", h=8))
            P.dma("sync", va_scr[t * 128:(t + 1) * 128, :, :], va[:])
    with P.scope():
        if "nat" in NO_REORDER or "natattn" in NO_REORDER:
            P.mark_no_reorder()
        qTs = [bft(f"qTh{i}", (64, T_TOK)) for i in range(2)]
        kTs = [bft(f"kTh{i}", (64, T_TOK)) for i in range(2)]
        Vas = [bft(f"Va{i}", (128, 66, 65)) for i in range(2)]
        Vbs = [bft(f"Vb{i}", (128, 65, 65)) for i in range(2)]
        nbs = [f32t(f"nb{i}", (128, 8, 4, 64)) for i in range(2)]
        sb = [f32t(f"sb{i}", (128, 4, 64)) for i in range(4)]
        pex = [bft(f"pex{i}", (128, 6, 64)) for i in range(4)]
        rec = f32t("rec", (128, 1))
        no = [f32t(f"no{i}", (128, 64)) for i in range(3)]
        recs = [f32t(f"rec{i}", (128, 1)) for i in range(3)]
        pss = [P.ps(f"na_ps{i}", [128, 6, 64]) for i in range(5)]
        pos_ = [P.ps(f"na_po{i}", [128, 65]) for i in range(3)]
        units = [(h, rp, sub) for h in range(8) for rp in range(64) for sub in range(2)]
        hbuf = {}

        def load_head(h):
            qT, kT, Va, Vb, nb = qTs[h % 2], kTs[h % 2], Vas[h % 2], Vbs[h % 2], nbs[h % 2]
            P.dma("sync", qT[:], qT_scr[h])
            P.dma("sync", kT[:], kT_scr[h])
            P.dma("sync", Va[:], va_scr[:, h, :].rearrange("(n p) c -> p n c", p=128))
            P.dma("sync", Vb[:], va_scr[64:64 + 65 * 128, h, :].rearrange("(n p) c -> p n c", p=128))
            P.dma("sync", nb[:], natb_d[h])
            hbuf[h] = (qT, kT, Va, Vb, nb)

        def geom(r):
            rs = min(max(r - 4, 0), 120)
            pat = 3 if 4 <= r <= 124 else (7 - r if r < 4 else 127 - r)
            return rs, pat, 256 + 64 * rs, 256 + 64 * r

        def emit_S(i):
            h, rp, sub = units[i]
            if h not in hbuf:
                load_head(h)
            qT, kT, Va, Vb, nb = hbuf[h]
            rs, pat, s0, q0 = geom(2 * rp + sub)
            ps = pss[i % 5]
            for blk in range(6):
                k0 = s0 + 128 * blk if blk < 4 else 128 * (blk - 4)
                P.T("matmul", out=ps[:, blk, :], lhsT=kT[:, k0:k0 + 128], rhs=qT[:, q0:q0 + 64], start=True, stop=True,
                    R=[kT, qT], W=[ps])
            s_, pe = sb[i % 4], pex[i % 4]
            P.V("tensor_tensor", out=s_[:], in0=ps[:, 0:4, :], in1=nb[:, pat, :, :], op=ALU.add, R=[ps, nb])
            P.S("activation", out=pe[:, 0:4, :], in_=s_[:], func=AF.Exp, W=[(pe, 0)])
            P.S("activation", out=pe[:, 4:6, :], in_=ps[:, 4:6, :], func=AF.Exp, W=[(pe, 1)])

        def emit_PV(i):
            h, rp, sub = units[i]
            qT, kT, Va, Vb, nb = hbuf[h]
            rs, pat, s0, q0 = geom(2 * rp + sub)
            pe = pex[i % 4]
            po = pos_[rp % 3]
            for blk in range(6):
                if blk >= 4:
                    vt = Va[:, blk - 4, :]
                elif rs % 2 == 0:
                    vt = Va[:, s0 // 128 + blk, :]
                else:
                    vt = Vb[:, (s0 - 64) // 128 + blk, :]
                P.T("matmul", out=po[64 * sub:64 * sub + 64, :], lhsT=pe[:, blk, :], rhs=vt, start=(blk == 0), stop=(blk == 5),
                    R=[pe, Va, Vb], W=[po])
            if sub == 1:
                n_ = no[rp % 3]
                rec = recs[rp % 3]
                P.V("reciprocal", out=rec[:], in_=po[:, 64:65])
                P.V("tensor_scalar", out=n_[:], in0=po[:, 0:64], scalar1=rec[:, 0:1], scalar2=None, op0=ALU.mult)
                P.dma("sync", nat_scr[256 + 128 * rp:256 + 128 * (rp + 1), 64 * h:64 * h + 64], n_[:])

        emit_S(0)
        for i in range(len(units)):
            if i + 1 < len(units):
                emit_S(i + 1)
            emit_PV(i)


def build_main_nc():
    nc, _ = build_main(upto="all", fuse_adaln=True)
    return nc


def kernel(**inp):
    inp = {k: np.asarray(v) for k, v in inp.items()}
    maps = [main_inputs(inp, None, b) for b in range(4)]
    if "main" not in _CACHE:
        _CACHE["main"] = build_main_nc()
    res = run_bass_kernel_spmd(_CACHE["main"], maps, core_ids=[0, 1, 2, 3]).results
    return np.stack([res[b]["out"] for b in range(4)], 0).astype(np.float32)


def stage_adaln(P, G, cT_d, adaw_d, adab_d, ng_d, mods_scr):
    f32t = lambda n, shp: P.sb("ad_" + n, list(shp), F32)
    cT = f32t("cT", (128, 8, 2))
    sc = f32t("sc", (128, 8, 2))
    P.dma("sync", cT[:], cT_d[:, :, :])
    P.S("activation", out=sc[:], in_=cT[:], func=AF.Silu)
    selr = f32t("selr", (2, 2, 128))
    P.dma("sync", selr[:], G.selr_d[:, :, :])
    aws = [f32t(f"aw{i}", (128, 8, 512)) for i in range(2)]
    ab = f32t("ab", (2, 6144))
    ng = f32t("ng", (2, 2, 1024))
    m = f32t("m", (2, 6144))
    bc = [f32t(f"bc{i}", (128, 1024)) for i in range(2)]
    pms = [P.ps(f"ad_pm{i}", [2, 512]) for i in range(2)]
    pbs = [P.ps(f"ad_pb{i}", [128, 512]) for i in range(4)]
    ev = 0
    for l in range(2):
        P.dma("sync", ab[:], adab_d[l, 0:2, :])
        P.dma("sync", ng[:], ng_d[l, 0:2, :, :])
        for cb in range(12):
            aw = aws[cb % 2]
            P.dma("sync", aw[:], adaw_d[l, :, :, cb * 512:(cb + 1) * 512])
            pm = pms[cb % 2]
            for k in range(8):
                P.T("matmul", out=pm[:], lhsT=sc[:, k, :], rhs=aw[:, k, :], start=(k == 0), stop=(k == 7))
            P.V("tensor_tensor", out=m[:, cb * 512:(cb + 1) * 512], in0=pm[:], in1=ab[:, cb * 512:(cb + 1) * 512], op=ALU.add,
                W=[(m, cb)])
        P.V("scalar_tensor_tensor", out=m[:, 1024:2048], in0=m[:, 1024:2048], scalar=1.0, in1=ng[:, 0, :], op0=ALU.add, op1=ALU.mult)
        P.V("scalar_tensor_tensor", out=m[:, 4096:5120], in0=m[:, 4096:5120], scalar=1.0, in1=ng[:, 1, :], op0=ALU.add, op1=ALU.mult)
        for s in range(2):
            for v in range(6):
                b_ = bc[ev % 2]
                for hb in range(2):
                    pb = pbs[(2 * ev + hb) % 4]
                    P.T("matmul", out=pb[:], lhsT=selr[:, s, :], rhs=m[:, v * 1024 + hb * 512:v * 1024 + hb * 512 + 512], start=True, stop=True)
                    if hb == 0:
                        P.S("copy", out=b_[:, 0:512], in_=pb[:], W=[(b_, 0)])
                    else:
                        P.V("tensor_copy", out=b_[:, 512:1024], in_=pb[:], W=[(b_, 1)])
                ev += 1
                P.dma("sync", mods_scr[l, s, v], b_[:])


def build_main_nc():
    nc, _ = build_main(upto="all", fuse_adaln=True)
    return nc


def kernel(**inp):
    inp = {k: np.asarray(v) for k, v in inp.items()}
    maps = [main_inputs(inp, None, b) for b in range(4)]
    if "main" not in _CACHE:
        _CACHE["main"] = build_main_nc()
    res = run_bass_kernel_spmd(_CACHE["main"], maps, core_ids=[0, 1, 2, 3]).results
    return np.stack([res[b]["out"] for b in range(4)], 0).astype(np.float32)


def stage_adaln(P, G, cT_d, adaw_d, adab_d, ng_d, mods_scr):
    f32t = lambda n, shp: P.sb("ad_" + n, list(shp), F32)
    cT = f32t("cT", (128, 8, 2))
    sc = f32t("sc", (128, 8, 2))
    rep = [f32t(f"rep{s}", (128, 8, 128)) for s in range(2)]
    P.dma("sync", cT[:], cT_d[:, :, :])
    P.S("activation", out=sc[:], in_=cT[:], func=AF.Silu)
    for s in range(2):
        P.V("tensor_copy", out=rep[s][:], in_=sc[:, :, s].unsqueeze(2).to_broadcast([128, 8, 128]))
    aws = [f32t(f"aw{i}", (128, 8, 512)) for i in range(2)]
    ab = f32t("ab", (128, 6144))
    ng = f32t("ng", (128, 2, 1024))
    ms = [f32t(f"m{s}", (128, 6144)) for s in range(2)]
    pms = [P.ps(f"ad_pm{i}", [128, 512]) for i in range(4)]
    ev = 0
    for l in range(2):
        P.dma("sync", ab[:], adab_d[l])
        P.dma("sync", ng[:], ng_d[l])
        for cb in range(12):
            aw = aws[cb % 2]
            P.dma("sync", aw[:], adaw_d[l, :, :, cb * 512:(cb + 1) * 512])
            for s in range(2):
                pm = pms[ev % 4]
                ev += 1
                for k in range(8):
                    P.T("matmul", out=pm[:], lhsT=rep[s][:, k, :], rhs=aw[:, k, :], start=(k == 0), stop=(k == 7))
                P.V("tensor_tensor", out=ms[s][:, cb * 512:(cb + 1) * 512], in0=pm[:], in1=ab[:, cb * 512:(cb + 1) * 512], op=ALU.add,
                    W=[(ms[s], cb)])
        for s in range(2):
            m = ms[s]
            P.V("scalar_tensor_tensor", out=m[:, 1024:2048], in0=m[:, 1024:2048], scalar=1.0, in1=ng[:, 0, :], op0=ALU.add, op1=ALU.mult)
            P.V("scalar_tensor_tensor", out=m[:, 4096:5120], in0=m[:, 4096:5120], scalar=1.0, in1=ng[:, 1, :], op0=ALU.add, op1=ALU.mult)
            for v in range(6):
                P.dma("sync", mods_scr[l, s, v], m[:, v * 1024:(v + 1) * 1024])
```
